# Optimizing a Trainium2 kernel written in Bass

```python
import math
import jax
import jax.numpy as jnp
from jax import lax
import numpy as np

D_MODEL = 1024
BATCH = 8
SEQ = 4096
DEPTH = 2

N_MIXERS = 4
D_MIX = D_MODEL
D_GROUP = D_MIX // N_MIXERS
HEAD_DIM = 64
N_HEADS = D_GROUP // HEAD_DIM
Q_BLOCK = 128
NEG_INF = -1e30
EPS = 1e-6

NSA_CMP_BLOCK = 32
NSA_CMP_STRIDE = 16
NSA_SEL_BLOCK = 64
NSA_TOP_N = 16
NSA_WINDOW = 512
NSA_CMP_HIDDEN = 128
NSA_FORCED_SCORE = 1e4

DIFF_QK_DIM = HEAD_DIM // 2

SSM_STATE = 64
SSM_GROUPS = 2
SSM_CHUNK = 128
SSM_XBC = D_GROUP + 2 * SSM_GROUPS * SSM_STATE
CONV_WIDTH = 4

MLSTM_CHUNK = 128

IN_SPLITS = (
    D_GROUP, HEAD_DIM, HEAD_DIM, HEAD_DIM, HEAD_DIM, HEAD_DIM, HEAD_DIM, 3 * N_HEADS, D_GROUP,
    D_GROUP, D_GROUP, D_GROUP, D_GROUP,
    D_GROUP, SSM_XBC, N_HEADS,
    2 * D_GROUP, D_GROUP, 2 * N_HEADS, D_GROUP, D_GROUP,
)
D_IN_PROJ = sum(IN_SPLITS)

kernel_name = "hybrid_parallel_nsa_diff_ssd_mlstm"


def rmsnorm(x, g):
    xf = x.astype(jnp.float32)
    y = xf * lax.rsqrt(jnp.mean(xf * xf, axis=-1, keepdims=True) + EPS)
    return (y * g).astype(x.dtype)


def causal_dwconv(x, w, b):
    width, ch = w.shape
    y = lax.conv_general_dilated(x, w[:, None, :], window_strides=(1,), padding=((width - 1, 0),),
                                 dimension_numbers=('NWC', 'WIO', 'NWC'), feature_group_count=ch)
    return y + b


def masked_softmax(s, valid):
    return jax.nn.softmax(jnp.where(valid, s, NEG_INF), axis=-1) * valid


def nsa_attention(q, kc, vc, ks, vs, kw, vw, gates, cmp_pos, ck_w1, ck_w2, cv_w1, cv_w2):
    bsz, seq, n_h, dh = q.shape
    scale = dh ** -0.5
    n_cmp = (seq - NSA_CMP_BLOCK) // NSA_CMP_STRIDE + 1
    n_sel = seq // NSA_SEL_BLOCK
    top_n = min(NSA_TOP_N, n_sel)
    n_qb = seq // Q_BLOCK
    span = NSA_WINDOW + Q_BLOCK

    cmp_idx = np.arange(n_cmp)[:, None] * NSA_CMP_STRIDE + np.arange(NSA_CMP_BLOCK)[None, :]
    cmp_end = jnp.asarray(cmp_idx[:, -1], jnp.int32)
    sel_start = np.arange(n_sel) * NSA_SEL_BLOCK
    overlap = np.clip(np.minimum(cmp_idx[:, -1:] + 1, sel_start[None, :] + NSA_SEL_BLOCK)
                      - np.maximum(cmp_idx[:, :1], sel_start[None, :]), 0, None)
    cmp_to_sel = jnp.asarray(overlap / NSA_CMP_BLOCK, jnp.float32)

    def compress(src, w1, w2):
        blk = src[:, cmp_idx] + cmp_pos
        hid = jax.nn.silu(blk.reshape(bsz, n_cmp, NSA_CMP_BLOCK * dh) @ w1)
        return hid @ w2

    k_cmp = compress(kc, ck_w1, ck_w2)
    v_cmp = compress(vc, cv_w1, cv_w2)
    ks_blk = ks.reshape(bsz, n_sel, NSA_SEL_BLOCK, dh)
    vs_blk = vs.reshape(bsz, n_sel, NSA_SEL_BLOCK, dh)
    kw_pad = jnp.pad(kw, ((0, 0), (NSA_WINDOW, 0), (0, 0)))
    vw_pad = jnp.pad(vw, ((0, 0), (NSA_WINDOW, 0), (0, 0)))
    b_idx = jnp.arange(bsz)[:, None, None]
    sel_ids = jnp.arange(n_sel)
    sel_off = jnp.arange(NSA_SEL_BLOCK)
    win_off = jnp.arange(span)

    def block(args):
        qi, q_blk, g_blk = args
        q0 = qi * Q_BLOCK
        t = q0 + jnp.arange(Q_BLOCK)
        s_c = jnp.einsum('bqhd,bnd->bhqn', q_blk, k_cmp).astype(jnp.float32) * scale
        p_c = masked_softmax(s_c, cmp_end[None, :] <= t[:, None])
        o_c = jnp.einsum('bhqn,bnd->bqhd', p_c.astype(v_cmp.dtype), v_cmp)
        imp = jnp.einsum('bhqn,ns->bqs', p_c, cmp_to_sel)
        cur = t // NSA_SEL_BLOCK
        forced = (sel_ids[None, :] == cur[:, None]) | (sel_ids[None, :] == 0)
        allowed = sel_ids[None, :] <= cur[:, None]
        imp = jnp.where(forced, NSA_FORCED_SCORE, jnp.where(allowed, imp, -1.0))
        _, top_idx = lax.top_k(imp, top_n)
        k_sel = ks_blk[b_idx, top_idx]
        v_sel = vs_blk[b_idx, top_idx]
        kpos = top_idx[..., None] * NSA_SEL_BLOCK + sel_off
        valid_s = (kpos <= t[None, :, None, None]).reshape(bsz, 1, Q_BLOCK, top_n * NSA_SEL_BLOCK)
        s_s = jnp.einsum('bqhd,bqnld->bhqnl', q_blk, k_sel).astype(jnp.float32) * scale
        p_s = masked_softmax(s_s.reshape(bsz, n_h, Q_BLOCK, top_n * NSA_SEL_BLOCK), valid_s)
        p_s = p_s.reshape(bsz, n_h, Q_BLOCK, top_n, NSA_SEL_BLOCK).astype(v_sel.dtype)
        o_s = jnp.einsum('bhqnl,bqnld->bqhd', p_s, v_sel)
        k_w = lax.dynamic_slice_in_dim(kw_pad, q0, span, axis=1)
        v_w = lax.dynamic_slice_in_dim(vw_pad, q0, span, axis=1)
        kpos_w = q0 - NSA_WINDOW + win_off
        valid_w = ((kpos_w[None, :] <= t[:, None]) & (kpos_w[None, :] > t[:, None] - NSA_WINDOW)
                   & (kpos_w[None, :] >= 0))
        s_w = jnp.einsum('bqhd,bkd->bhqk', q_blk, k_w).astype(jnp.float32) * scale
        p_w = masked_softmax(s_w, valid_w)
        o_w = jnp.einsum('bhqk,bkd->bqhd', p_w.astype(v_w.dtype), v_w)
        return g_blk[..., 0:1] * o_c + g_blk[..., 1:2] * o_s + g_blk[..., 2:3] * o_w

    q_blocks = jnp.moveaxis(q.reshape(bsz, n_qb, Q_BLOCK, n_h, dh), 1, 0)
    g_blocks = jnp.moveaxis(gates.reshape(bsz, n_qb, Q_BLOCK, n_h, 3), 1, 0)
    out = lax.map(block, (jnp.arange(n_qb), q_blocks, g_blocks))
    return jnp.moveaxis(out, 0, 1).reshape(bsz, seq, n_h, dh)


def diff_attention(q, k, v, lam, norm_g, layer_idx):
    bsz, seq, n_h = q.shape[:3]
    n_qb = seq // Q_BLOCK
    lambda_init = 0.8 - 0.6 * math.exp(-0.3 * layer_idx)
    lam = lam.astype(jnp.float32)
    lam_full = jnp.exp(jnp.sum(lam[0] * lam[1])) - jnp.exp(jnp.sum(lam[2] * lam[3])) + lambda_init
    scale = DIFF_QK_DIM ** -0.5
    kpos = jnp.arange(seq)

    def block(args):
        qi, q_blk = args
        t = qi * Q_BLOCK + jnp.arange(Q_BLOCK)
        s = jnp.einsum('bqhcd,bkhcd->bhcqk', q_blk, k).astype(jnp.float32) * scale
        p = jax.nn.softmax(jnp.where(kpos[None, :] <= t[:, None], s, NEG_INF), axis=-1)
        a = p[:, :, 0] - lam_full * p[:, :, 1]
        return jnp.einsum('bhqk,bkhd->bqhd', a.astype(v.dtype), v)

    q_blocks = jnp.moveaxis(q.reshape(bsz, n_qb, Q_BLOCK, n_h, 2, DIFF_QK_DIM), 1, 0)
    out = lax.map(block, (jnp.arange(n_qb), q_blocks))
    out = jnp.moveaxis(out, 0, 1).reshape(bsz, seq, n_h, -1)
    return rmsnorm(out, norm_g) * (1.0 - lambda_init)


def ssd_chunked(x, dt, a, bm, cm):
    bsz, seq, n_h, p = x.shape
    L = SSM_CHUNK
    nc = seq // L
    xdt = (x * dt[..., None]).reshape(bsz, nc, L, n_h, p)
    bc = bm.reshape(bsz, nc, L, n_h, SSM_STATE)
    cc = cm.reshape(bsz, nc, L, n_h, SSM_STATE)
    a_cs = jnp.cumsum((dt * a).reshape(bsz, nc, L, n_h), axis=2)
    causal = jnp.tril(jnp.ones((L, L), bool))[None, None, :, :, None]
    decay = jnp.exp(jnp.where(causal, a_cs[:, :, :, None, :] - a_cs[:, :, None, :, :], -jnp.inf))
    scores = jnp.einsum('bcthn,bcshn->bctsh', cc, bc) * decay
    y_diag = jnp.einsum('bctsh,bcshp->bcthp', scores, xdt)
    decay_end = jnp.exp(a_cs[:, :, -1:, :] - a_cs)
    states = jnp.einsum('bcshn,bcsh,bcshp->bchpn', bc, decay_end, xdt)
    chunk_decay = jnp.exp(a_cs[:, :, -1, :])

    def step(h, inp):
        st, dec = inp
        return dec[..., None, None] * h + st, h

    _, prev = lax.scan(step, jnp.zeros((bsz, n_h, p, SSM_STATE), jnp.float32),
                       (jnp.moveaxis(states, 1, 0), jnp.moveaxis(chunk_decay, 1, 0)))
    prev = jnp.moveaxis(prev, 0, 1)
    y_off = jnp.einsum('bcthn,bchpn,bcth->bcthp', cc, prev, jnp.exp(a_cs))
    return (y_diag + y_off).reshape(bsz, seq, n_h, p)


def ssd_mixer(xbc, dt_raw, z, conv_w, conv_b, dt_bias, a_log, d_skip, norm_g):
    bsz, seq, _ = xbc.shape
    xbc = jax.nn.silu(causal_dwconv(xbc, conv_w, conv_b))
    xs, bm, cm = jnp.split(xbc, [D_GROUP, D_GROUP + SSM_GROUPS * SSM_STATE], axis=-1)
    rep = N_HEADS // SSM_GROUPS
    xs = xs.reshape(bsz, seq, N_HEADS, HEAD_DIM).astype(jnp.float32)
    bm = jnp.repeat(bm.reshape(bsz, seq, SSM_GROUPS, SSM_STATE), rep, axis=2).astype(jnp.float32)
    cm = jnp.repeat(cm.reshape(bsz, seq, SSM_GROUPS, SSM_STATE), rep, axis=2).astype(jnp.float32)
    dt = jax.nn.softplus(dt_raw.astype(jnp.float32) + dt_bias.astype(jnp.float32))
    a = -jnp.exp(a_log.astype(jnp.float32))
    y = ssd_chunked(xs, dt, a, bm, cm) + d_skip.astype(jnp.float32)[:, None] * xs
    y = y.reshape(bsz, seq, D_GROUP).astype(z.dtype) * jax.nn.silu(z)
    y = rmsnorm(y.reshape(bsz, seq, SSM_GROUPS, -1), norm_g.reshape(SSM_GROUPS, -1))
    return y.reshape(bsz, seq, D_GROUP)


def mlstm_chunked(q, k, v, i_pre, f_pre):
    bsz, seq, n_h, dh = q.shape
    L = MLSTM_CHUNK
    nc = seq // L
    k = k * dh ** -0.5
    q = q.reshape(bsz, nc, L, n_h, dh)
    k = k.reshape(bsz, nc, L, n_h, dh)
    v = v.reshape(bsz, nc, L, n_h, dh)
    ig = i_pre.reshape(bsz, nc, L, n_h)
    b = jnp.cumsum(jax.nn.log_sigmoid(f_pre).reshape(bsz, nc, L, n_h), axis=2)
    causal = jnp.tril(jnp.ones((L, L), bool))[None, None, :, :, None]
    d_log = jnp.where(causal, b[:, :, :, None, :] - b[:, :, None, :, :] + ig[:, :, None, :, :], -jnp.inf)
    b_last = b[:, :, -1, :]
    g_end = b_last[:, :, None, :] - b + ig
    m_loc = jnp.max(g_end, axis=2)
    w_end = jnp.exp(g_end - m_loc[:, :, None, :])
    c_loc = jnp.einsum('bcsh,bcshd,bcshe->bchde', w_end, v, k)
    n_loc = jnp.einsum('bcsh,bcshe->bche', w_end, k)

    def step(carry, inp):
        c_st, n_st, m_st = carry
        c_l, n_l, m_l, b_l = inp
        m_new = jnp.maximum(b_l + m_st, m_l)
        a_prev = jnp.exp(b_l + m_st - m_new)
        a_loc = jnp.exp(m_l - m_new)
        c_new = a_prev[..., None, None] * c_st + a_loc[..., None, None] * c_l
        n_new = a_prev[..., None] * n_st + a_loc[..., None] * n_l
        return (c_new, n_new, m_new), (c_st, n_st, m_st)

    init = (jnp.zeros((bsz, n_h, dh, dh), jnp.float32), jnp.zeros((bsz, n_h, dh), jnp.float32),
            jnp.zeros((bsz, n_h), jnp.float32))
    xs = (jnp.moveaxis(c_loc, 1, 0), jnp.moveaxis(n_loc, 1, 0), jnp.moveaxis(m_loc, 1, 0),
          jnp.moveaxis(b_last, 1, 0))
    _, (c_prev, n_prev, m_prev) = lax.scan(step, init, xs)
    c_prev = jnp.moveaxis(c_prev, 0, 1)
    n_prev = jnp.moveaxis(n_prev, 0, 1)
    m_prev = jnp.moveaxis(m_prev, 0, 1)
    a_inter = b + m_prev[:, :, None, :]
    m_t = jnp.maximum(a_inter, jnp.max(d_log, axis=3))
    w_intra = jnp.exp(d_log - m_t[:, :, :, None, :])
    s_qk = jnp.einsum('bcthd,bcshd->bctsh', q, k) * w_intra
    w_inter = jnp.exp(a_inter - m_t)
    num = (jnp.einsum('bctsh,bcshd->bcthd', s_qk, v)
           + w_inter[..., None] * jnp.einsum('bcthe,bchde->bcthd', q, c_prev))
    den = jnp.sum(s_qk, axis=3) + w_inter * jnp.einsum('bcthe,bche->bcth', q, n_prev)
    h = num / jnp.maximum(jnp.abs(den), jnp.exp(-m_t))[..., None]
    return h.reshape(bsz, seq, n_h, dh)


def hybrid_layer(x, c_act, layer_idx, norm_g, ada_w, ada_b, w_in, w_out,
                 nsa_cmp_pos, nsa_ck_w1, nsa_ck_w2, nsa_cv_w1, nsa_cv_w2, nsa_norm_g,
                 diff_lam, diff_norm_g,
                 ssm_conv_w, ssm_conv_b, ssm_dt_bias, ssm_a_log, ssm_d, ssm_norm_g,
                 ml_conv_w, ml_conv_b, ml_if_b, ml_norm_g):
    bsz, seq, _ = x.shape
    shift, scale, gate = jnp.split(c_act @ ada_w + ada_b, 3, axis=-1)
    h = rmsnorm(x, norm_g) * (1.0 + scale[:, None, :]) + shift[:, None, :]
    offsets = [int(o) for o in np.cumsum(IN_SPLITS)[:-1]]
    (a_q, a_kc, a_vc, a_ks, a_vs, a_kw, a_vw, a_g, a_z,
     b_q, b_k, b_v, b_z,
     c_z, c_xbc, c_dt,
     d_qk, d_v, d_if, d_o, d_z) = jnp.split(h @ w_in, offsets, axis=-1)

    def heads(t):
        return t.reshape(bsz, seq, N_HEADS, -1)

    y_a = nsa_attention(heads(a_q), a_kc, a_vc, a_ks, a_vs, a_kw, a_vw,
                        jax.nn.sigmoid(a_g.reshape(bsz, seq, N_HEADS, 3)),
                        nsa_cmp_pos, nsa_ck_w1, nsa_ck_w2, nsa_cv_w1, nsa_cv_w2)
    y_a = rmsnorm(y_a, nsa_norm_g).reshape(bsz, seq, D_GROUP) * jax.nn.silu(a_z)
    y_b = diff_attention(b_q.reshape(bsz, seq, N_HEADS, 2, DIFF_QK_DIM),
                         b_k.reshape(bsz, seq, N_HEADS, 2, DIFF_QK_DIM),
                         heads(b_v), diff_lam, diff_norm_g, layer_idx)
    y_b = y_b.reshape(bsz, seq, D_GROUP) * jax.nn.silu(b_z)
    y_c = ssd_mixer(c_xbc, c_dt, c_z, ssm_conv_w, ssm_conv_b, ssm_dt_bias, ssm_a_log, ssm_d, ssm_norm_g)
    qk = jax.nn.silu(causal_dwconv(d_qk, ml_conv_w, ml_conv_b))
    m_q, m_k = jnp.split(qk, 2, axis=-1)
    i_pre, f_pre = jnp.split((d_if + ml_if_b).astype(jnp.float32), 2, axis=-1)
    h_m = mlstm_chunked(heads(m_q).astype(jnp.float32), heads(m_k).astype(jnp.float32),
                        heads(d_v).astype(jnp.float32), i_pre, f_pre).astype(x.dtype)
    h_m = jax.nn.sigmoid(heads(d_o)) * h_m
    y_d = rmsnorm(h_m, ml_norm_g).reshape(bsz, seq, D_GROUP) * jax.nn.silu(d_z)
    y = jnp.concatenate([y_a, y_b, y_c, y_d], axis=-1) @ w_out
    return x + gate[:, None, :] * y


def setup_inputs(seed: int = 0) -> dict:
    key = jax.random.key(seed)
    keys = iter(jax.random.split(key, 40))

    def nrm(shape, s):
        return s * jax.random.normal(next(keys), shape, jnp.float32)

    def unif(shape, lo, hi):
        return jax.random.uniform(next(keys), shape, jnp.float32, lo, hi)

    x = nrm((BATCH, SEQ, D_MODEL), 1.0)
    c = nrm((BATCH, D_MODEL), 1.0)
    norm_g = 1.0 + nrm((DEPTH, D_MODEL), 0.05)
    ada_w = nrm((DEPTH, D_MODEL, 3 * D_MODEL), D_MODEL ** -0.5)
    ada_b = nrm((DEPTH, 3 * D_MODEL), 0.02)
    w_in = nrm((DEPTH, D_MODEL, D_IN_PROJ), D_MODEL ** -0.5)
    w_out = nrm((DEPTH, D_MIX, D_MODEL), D_MIX ** -0.5)
    nsa_cmp_pos = nrm((DEPTH, NSA_CMP_BLOCK, HEAD_DIM), 0.1)
    cmp_in = NSA_CMP_BLOCK * HEAD_DIM
    nsa_ck_w1 = nrm((DEPTH, cmp_in, NSA_CMP_HIDDEN), cmp_in ** -0.5)
    nsa_ck_w2 = nrm((DEPTH, NSA_CMP_HIDDEN, HEAD_DIM), NSA_CMP_HIDDEN ** -0.5)
    nsa_cv_w1 = nrm((DEPTH, cmp_in, NSA_CMP_HIDDEN), cmp_in ** -0.5)
    nsa_cv_w2 = nrm((DEPTH, NSA_CMP_HIDDEN, HEAD_DIM), NSA_CMP_HIDDEN ** -0.5)
    nsa_norm_g = 1.0 + nrm((DEPTH, HEAD_DIM), 0.05)
    diff_lam = nrm((DEPTH, 4, DIFF_QK_DIM), 0.1)
    diff_norm_g = 1.0 + nrm((DEPTH, HEAD_DIM), 0.05)
    ssm_conv_w = nrm((DEPTH, CONV_WIDTH, SSM_XBC), CONV_WIDTH ** -0.5)
    ssm_conv_b = nrm((DEPTH, SSM_XBC), 0.02)
    dt0 = jnp.exp(unif((DEPTH, N_HEADS), math.log(1e-3), math.log(1e-1)))
    ssm_dt_bias = dt0 + jnp.log(-jnp.expm1(-dt0))
    ssm_a_log = jnp.log(unif((DEPTH, N_HEADS), 1.0, 16.0))
    ssm_d = 1.0 + nrm((DEPTH, N_HEADS), 0.1)
    ssm_norm_g = 1.0 + nrm((DEPTH, D_GROUP), 0.05)
    ml_conv_w = nrm((DEPTH, CONV_WIDTH, 2 * D_GROUP), CONV_WIDTH ** -0.5)
    ml_conv_b = nrm((DEPTH, 2 * D_GROUP), 0.02)
    f_bias = jnp.broadcast_to(jnp.linspace(3.0, 6.0, N_HEADS), (DEPTH, N_HEADS)) + nrm((DEPTH, N_HEADS), 0.1)
    ml_if_b = jnp.concatenate([nrm((DEPTH, N_HEADS), 0.1), f_bias], axis=-1)
    ml_norm_g = 1.0 + nrm((DEPTH, HEAD_DIM), 0.05)
    final_g = 1.0 + nrm((D_MODEL,), 0.05)
    return {'x': x, 'c': c, 'norm_g': norm_g, 'ada_w': ada_w, 'ada_b': ada_b, 'w_in': w_in, 'w_out': w_out,
            'nsa_cmp_pos': nsa_cmp_pos, 'nsa_ck_w1': nsa_ck_w1, 'nsa_ck_w2': nsa_ck_w2,
            'nsa_cv_w1': nsa_cv_w1, 'nsa_cv_w2': nsa_cv_w2, 'nsa_norm_g': nsa_norm_g,
            'diff_lam': diff_lam, 'diff_norm_g': diff_norm_g,
            'ssm_conv_w': ssm_conv_w, 'ssm_conv_b': ssm_conv_b, 'ssm_dt_bias': ssm_dt_bias,
            'ssm_a_log': ssm_a_log, 'ssm_d': ssm_d, 'ssm_norm_g': ssm_norm_g,
            'ml_conv_w': ml_conv_w, 'ml_conv_b': ml_conv_b, 'ml_if_b': ml_if_b, 'ml_norm_g': ml_norm_g,
            'final_g': final_g}


def reference(x, c, norm_g, ada_w, ada_b, w_in, w_out,
              nsa_cmp_pos, nsa_ck_w1, nsa_ck_w2, nsa_cv_w1, nsa_cv_w2, nsa_norm_g,
              diff_lam, diff_norm_g,
              ssm_conv_w, ssm_conv_b, ssm_dt_bias, ssm_a_log, ssm_d, ssm_norm_g,
              ml_conv_w, ml_conv_b, ml_if_b, ml_norm_g, final_g):
    c_act = jax.nn.silu(c)
    for l in range(DEPTH):
        x = hybrid_layer(x, c_act, l, norm_g[l], ada_w[l], ada_b[l], w_in[l], w_out[l],
                         nsa_cmp_pos[l], nsa_ck_w1[l], nsa_ck_w2[l], nsa_cv_w1[l], nsa_cv_w2[l], nsa_norm_g[l],
                         diff_lam[l], diff_norm_g[l],
                         ssm_conv_w[l], ssm_conv_b[l], ssm_dt_bias[l], ssm_a_log[l], ssm_d[l], ssm_norm_g[l],
                         ml_conv_w[l], ml_conv_b[l], ml_if_b[l], ml_norm_g[l])
    return rmsnorm(x, final_g)
```

```python
import contextlib
import math
import numpy as np
import ml_dtypes
import concourse.bass as bass
import concourse.mybir as mybir
from concourse.bass_utils import run_bass_kernel_spmd

F32 = mybir.dt.float32
BF16 = mybir.dt.bfloat16
AF = mybir.ActivationFunctionType
ALU = mybir.AluOpType
AX = mybir.AxisListType

D = 1024
NEGB = -30000.0
EPS = 1e-6

F_GROUPS = []
for h in range(4):
    F_GROUPS.append(("aq%d" % h, 0 + 64 * h, 64))
F_GROUPS += [("akc", 256, 64), ("avc", 320, 64), ("aks", 384, 64), ("akw", 512, 64)]
for h in range(4):
    F_GROUPS.append(("bq%d" % h, 908 + 64 * h, 64))
for h in range(4):
    F_GROUPS.append(("bk%d" % h, 1164 + 64 * h, 64))
F_GROUPS += [("cx0", 2188, 128), ("cx1", 2316, 128), ("cB0", 2444, 64), ("cB1", 2508, 64),
             ("cC0", 2572, 64), ("cC1", 2636, 64)]
for h in range(4):
    F_GROUPS.append(("dq%d" % h, 2704 + 64 * h, 64))
for h in range(4):
    F_GROUPS.append(("dk%d" % h, 2960 + 64 * h, 64))
F_ROW = {}
_r = 0
F_COLS = []
for (n_, c0_, w_) in F_GROUPS:
    F_ROW[n_] = (_r, w_)
    F_COLS += list(range(c0_, c0_ + w_))
    _r += w_
NF = _r
T_PARTS = [("vs", 448, 64), ("vw", 576, 64), ("ag", 640, 12), ("az", 652, 256),
           ("bv", 1420, 256), ("bz", 1676, 256), ("cz", 1932, 256), ("dt", 2700, 4),
           ("dv", 3216, 256), ("dif", 3472, 8), ("do", 3480, 256), ("dz", 3736, 256)]
T_COL = {}
_r = 0
T_COLS = []
for (n_, c0_, w_) in T_PARTS:
    T_COL[n_] = (_r, w_)
    T_COLS += list(range(c0_, c0_ + w_))
    _r += w_
NTC = _r
TS_COL = {"ag": (0, 12), "dt": (12, 4), "dif": (16, 8)}


class TT:
    def __init__(self, h, name=""):
        self.h = h
        self.w = {}
        self.r = {}
        self.name = name

    def __getitem__(self, key):
        return V(self, self.h[key])


class V:
    def __init__(self, t, ap):
        self.t = t
        self.ap = ap

    def __getitem__(self, key):
        return V(self.t, self.ap[key])

    def re(self, pat, **kw):
        return V(self.t, self.ap.rearrange(pat, **kw))

    def bc(self, shape):
        return V(self.t, self.ap.to_broadcast(list(shape)))

    def raw(self, fn):
        return V(self.t, fn(self.ap))


class Eng:
    def __init__(self, name, h, sem, key, is_pe=False):
        self.name = name
        self.h = h
        self.sem = sem
        self.key = key
        self.count = 0
        self.seen = {}
        self.is_pe = is_pe
        self.pend = False
        self.dsems = []
        self.dlast = []
        self.dnext = 0


class KB:
    def __init__(self, nc, es):
        self.nc = nc
        self.es = es
        self.sems = {}
        self.eng = {}
        for name, h, pe in (("pe", nc.tensor, True), ("act", nc.scalar, False),
                            ("dve", nc.vector, False), ("pool", nc.gpsimd, False),
                            ("sp", nc.sync, False)):
            s = es.enter_context(nc.semaphore("sem_" + name))
            self.sems[name] = s
            self.eng[name] = Eng(name, h, s, name, pe)
        for q, n in (("sp", 28), ("pool", 10), ("act", 6)):
            E = self.eng[q]
            for i in range(n):
                key = "d_%s_%d" % (q, i)
                s = es.enter_context(nc.semaphore(key))
                self.sems[key] = s
                E.dsems.append(key)
                E.dlast.append(0)
        self.bar = es.enter_context(nc.semaphore("barrier"))
        self.sems["bar"] = self.bar
        self.barcount = 0
        self.uid = 0

    def sb(self, st, shape, dt, name):
        self.uid += 1
        h = st.enter_context(self.nc.sbuf_tensor("%s_%d" % (name, self.uid), list(shape), dt))
        return TT(h, name)

    def ps(self, st, shape, dt, name):
        self.uid += 1
        esz = 4 if dt == F32 else 2
        n = 1
        for d_ in shape[1:]:
            n *= d_
        per_bank = 2048 // esz
        full = ((n + per_bank - 1) // per_bank) * per_bank
        h = st.enter_context(self.nc.psum_tensor("%s_%d" % (name, self.uid), [128, full], dt))
        ap = h[0:shape[0], 0:n]
        if len(shape) == 3:
            ap = ap.rearrange("p (a b) -> p a b", a=shape[1])
        t = TT(None, name)
        t.h = ap
        return t

    def sub(self, v):
        t = TT(None, v.t.name + "_sub")
        t.h = v.ap
        return t

    def _waits(self, E, outs, ins, disjoint=False):
        need = {}

        def add(d, own_ok):
            for k, val in d.items():
                if k == E.key and not own_ok:
                    continue
                if need.get(k, 0) < val:
                    need[k] = val
        for v in ins:
            add(v.t.w, True)
        for v in outs:
            if not disjoint:
                add(v.t.w, True)
                add(v.t.r, True)
        for k, val in need.items():
            if E.is_pe and k == E.key:
                continue
            if E.seen.get(k, 0) >= val:
                continue
            E.h.wait_ge(self.sems[k], val)
            E.seen[k] = val

    def _record(self, ev, outs, ins, disjoint=False):
        k, val = ev
        for v in ins:
            if v.t.r.get(k, 0) < val:
                v.t.r[k] = val
        for v in outs:
            if disjoint:
                v.t.w[k] = max(v.t.w.get(k, 0), val)
            else:
                v.t.w = {k: val}
                v.t.r = {}

    def op(self, eng, fn, outs, ins, inc=True, disjoint=False):
        E = self.eng[eng]
        self._waits(E, outs, ins, disjoint)
        inst = fn(E.h)
        if inc:
            E.count += 1
            inst.then_inc(E.sem, 1)
            ev = (E.key, E.count)
            E.pend = False
        else:
            ev = (E.key, E.count + 1)
            E.pend = True
        self._record(ev, outs, ins, disjoint)

    def dma(self, out, in_, q="sp", disjoint=False):
        E = self.eng[q]
        i = E.dnext % len(E.dsems)
        E.dnext += 1
        key = E.dsems[i]
        if E.dlast[i] and E.seen.get(key, 0) < E.dlast[i]:
            E.h.wait_ge(self.sems[key], E.dlast[i])
            E.seen[key] = E.dlast[i]
        self._waits(E, [out], [in_], disjoint)
        E.h.dma_start(out=out.ap, in_=in_.ap).then_inc(self.sems[key], 16)
        E.dlast[i] += 16
        self._record((key, E.dlast[i]), [out], [in_], disjoint)

    def barrier(self):
        sp = self.eng["sp"]
        assert not self.eng["pe"].pend
        for q in ("sp", "pool", "act"):
            E = self.eng[q]
            for i, key in enumerate(E.dsems):
                if E.dlast[i] and sp.seen.get(key, 0) < E.dlast[i]:
                    sp.h.wait_ge(self.sems[key], E.dlast[i])
                    sp.seen[key] = E.dlast[i]
        for n in ("pe", "act", "dve", "pool"):
            E = self.eng[n]
            if E.count and sp.seen.get(n, 0) < E.count:
                sp.h.wait_ge(E.sem, E.count)
                sp.seen[n] = E.count
        self.barcount += 1
        sp.h.sem_inc(self.bar, 1)
        for n in ("pe", "act", "dve", "pool"):
            E = self.eng[n]
            E.h.wait_ge(self.bar, self.barcount)
        for n, E in self.eng.items():
            for m, E2 in self.eng.items():
                if m != "sp":
                    E.seen[m] = E2.count
            for q in ("sp", "pool", "act"):
                Eq = self.eng[q]
                for i, key in enumerate(Eq.dsems):
                    E.seen[key] = Eq.dlast[i]

    def mm(self, out, lhsT, rhs, start=True, stop=True, inc=True):
        self.op("pe", lambda e: e.matmul(out.ap, lhsT.ap, rhs.ap, start=start, stop=stop),
                [out], [lhsT, rhs], inc=inc)

    def tr(self, out, in_, ident, inc=True):
        self.op("pe", lambda e: e.transpose(out.ap, in_.ap, ident.ap), [out], [in_, ident], inc=inc)

    def act(self, out, in_, func, bias=None, scale=1.0, accum=None):
        ins = [in_]
        outs = [out]
        kw = {}
        if isinstance(bias, V):
            ins.append(bias)
            kw["bias"] = bias.ap
        elif bias is not None:
            kw["bias"] = bias
        if isinstance(scale, V):
            ins.append(scale)
            kw["scale"] = scale.ap
        else:
            kw["scale"] = scale
        if accum is not None:
            outs.append(accum)
            kw["accum_out"] = accum.ap
        self.op("act", lambda e: e.activation(out.ap, in_.ap, func, **kw), outs, ins)

    def ts(self, eng, out, in0, s1, s2, op0, op1=None):
        ins = [in0]
        a1 = s1
        a2 = s2
        if isinstance(s1, V):
            ins.append(s1)
            a1 = s1.ap
        if isinstance(s2, V):
            ins.append(s2)
            a2 = s2.ap
        if op1 is None:
            self.op(eng, lambda e: e.tensor_scalar(out.ap, in0.ap, a1, None, op0), [out], ins)
        else:
            self.op(eng, lambda e: e.tensor_scalar(out.ap, in0.ap, a1, a2, op0, op1), [out], ins)

    def tt(self, eng, out, in0, in1, op):
        self.op(eng, lambda e: e.tensor_tensor(out.ap, in0.ap, in1.ap, op), [out], [in0, in1])

    def stt(self, eng, out, in0, s, in1, op0, op1):
        ins = [in0, in1]
        a = s
        if isinstance(s, V):
            ins.append(s)
            a = s.ap
        self.op(eng, lambda e: e.scalar_tensor_tensor(out.ap, in0.ap, a, in1.ap, op0, op1), [out], ins)

    def cp(self, eng, out, in_):
        if eng == "act":
            self.op("act", lambda e: e.copy(out.ap, in_.ap), [out], [in_])
        else:
            self.op(eng, lambda e: e.tensor_copy(out.ap, in_.ap), [out], [in_])

    def memset(self, eng, out, val):
        self.op(eng, lambda e: e.memset(out.ap, val), [out], [])

    def red(self, out, in_, op):
        self.op("dve", lambda e: e.tensor_reduce(out.ap, in_.ap, AX.X, op), [out], [in_])

    def rsqrt(self, out, in_, scale, post=1.0):
        n = out.ap.shape[0]
        self.act(out, in_, AF.Ln, bias=self.eps_col[0:n, 0:1], scale=scale)
        self.act(out, out, AF.Exp, scale=-0.5)
        if post != 1.0:
            self.ts("dve", out, out, float(post), None, ALU.mult)

    def recip(self, out, in_):
        self.op("dve", lambda e: e.reciprocal(out.ap, in_.ap), [out], [in_])


class Ring:
    def __init__(self, tiles):
        self.tiles = tiles
        self.i = 0

    def next(self):
        t = self.tiles[self.i % len(self.tiles)]
        self.i += 1
        return t


def build(S, DEPTH, debug=False, stop=99):
    NT = S // 128
    NG = S // 512
    NCMP = (S - 32) // 16 + 1
    NCC = (NCMP + 127) // 128
    nc = bass.Bass("TRN2", target_bir_lowering=False)

    def din(name, shape, dt=F32):
        return TT(nc.dram_tensor(name, list(shape), dt, kind="ExternalInput").ap(), name)

    def dscr(name, shape, dt):
        return TT(nc.dram_tensor(name, list(shape), dt, kind="Internal").ap(), name)

    x_in = din("x", [S, D])
    c_in = din("c", [D])
    norm_g = din("norm_g", [DEPTH, D])
    ada_w = din("ada_w", [DEPTH, D, 3 * D])
    ada_b = din("ada_b", [DEPTH, 3 * D])
    w_inF = din("w_inF", [DEPTH, D, NF])
    w_inT = din("w_inT", [DEPTH, D, NTC])
    w_out = din("w_out", [DEPTH, D, D])
    cmp_pos = din("nsa_cmp_pos", [DEPTH, 32, 64])
    ck_w1 = din("nsa_ck_w1", [DEPTH, 2048, 128])
    ck_w2 = din("nsa_ck_w2", [DEPTH, 128, 64])
    cv_w1 = din("nsa_cv_w1", [DEPTH, 2048, 128])
    cv_w2 = din("nsa_cv_w2", [DEPTH, 128, 64])
    nsa_ng = din("nsa_norm_g", [DEPTH, 64])
    diff_lam = din("diff_lam", [DEPTH, 128])
    diff_ng = din("diff_norm_g", [DEPTH, 64])
    ssm_cw = din("ssm_conv_w", [DEPTH, 4, 512])
    ssm_cb = din("ssm_conv_b", [DEPTH, 512])
    ssm_dtb = din("ssm_dt_bias", [DEPTH, 4])
    ssm_alog = din("ssm_a_log", [DEPTH, 4])
    ssm_d = din("ssm_d", [DEPTH, 4])
    ssm_ng = din("ssm_norm_g", [DEPTH, 256])
    ml_cw = din("ml_conv_w", [DEPTH, 4, 512])
    ml_cb = din("ml_conv_b", [DEPTH, 512])
    ml_ifb = din("ml_if_b", [DEPTH, 8])
    ml_ng = din("ml_norm_g", [DEPTH, 64])
    final_g = din("final_g", [D])
    c_identb = din("c_identb", [128, 128], BF16)
    c_identf = din("c_identf", [128, 128])
    c_tri4 = din("c_tri4", [128, 512], BF16)
    c_atri4 = din("c_atri4", [128, 512], BF16)
    c_U = din("c_U", [128, 128])
    c_mb_st = din("c_mb_st", [128, 128])
    c_mb_ts = din("c_mb_ts", [128, 128])
    c_sel127 = din("c_sel127", [128, 128])
    c_E = din("c_E", [64, S], BF16)
    c_c2s = din("c_c2s", [NCC * 128, 65], BF16)
    c_cmask = din("c_cmask", [NCC * 128, S], BF16)
    c_selmul = din("c_selmul", [S, 64])
    c_seladd = din("c_seladd", [S, 64])

    out_d = TT(nc.dram_tensor("out", [S, D], F32, kind="ExternalOutput").ap(), "out")
    xres = dscr("xres", [S, D], F32)
    if debug:
        projF = TT(nc.dram_tensor("projF", [NF, S], BF16, kind="ExternalOutput").ap(), "projF")
        projT = TT(nc.dram_tensor("projT", [S, NTC], BF16, kind="ExternalOutput").ap(), "projT")
        projTs = TT(nc.dram_tensor("projTs", [S, 24], F32, kind="ExternalOutput").ap(), "projTs")
        dbg = TT(nc.dram_tensor("dbg", [128, DEPTH * 24], F32, kind="ExternalOutput").ap(), "dbg")
    else:
        projF = dscr("projF", [NF, S], BF16)
        projT = dscr("projT", [S, NTC], BF16)
        projTs = dscr("projTs", [S, 24], F32)
    if debug:
        mix = TT(nc.dram_tensor("mix", [S, D], BF16, kind="ExternalOutput").ap(), "mix")
    else:
        mix = dscr("mix", [S, D], BF16)

    es = contextlib.ExitStack()
    with es:
        es.enter_context(nc.allow_non_contiguous_dma("small strided parameter loads"))
        es.enter_context(nc.allow_low_precision("bf16 matmul operands, fp32 accumulation"))
        kb = KB(nc, es)
        identb = kb.sb(es, [128, 128], BF16, "identb")
        identf = kb.sb(es, [128, 128], F32, "identf")
        ones_f = kb.sb(es, [128, 128], F32, "onesf")
        kb.dma(identb[:], c_identb[:, :])
        kb.dma(identf[:], c_identf[:, :])
        kb.memset("pool", ones_f[:], 1.0)
        kb.eps_col = kb.sb(es, [128, 1], F32, "epscol")
        kb.memset("pool", kb.eps_col[:], EPS)
        modc = kb.sb(es, [128, DEPTH, 24], F32, "modc")
        Acoef = kb.sb(es, [128, DEPTH, 8], F32, "Acoef")
        gate_row = kb.sb(es, [128, DEPTH, D], F32, "gate_row")
        fin_row = kb.sb(es, [128, D], F32, "fin_row")
        kb.dma(fin_row[:], V(final_g, final_g.h.partition_broadcast(128)))

        with contextlib.ExitStack() as st:
            cact = kb.sb(st, [128, 8], F32, "cact")
            kb.dma(cact[:], V(c_in, c_in.h.rearrange("(k p) -> p k", p=128)))
            csig = kb.sb(st, [128, 8], F32, "csig")
            kb.act(csig[:], cact[:], AF.Sigmoid)
            kb.tt("dve", cact[:], cact[:], csig[:], ALU.mult)
            adab = kb.sb(st, [128, 24], F32, "adab")
            ng = kb.sb(st, [128, 8], F32, "ng")
            pm = kb.ps(st, [128, 24], F32, "pm")
            pg = kb.ps(st, [128, 2, 512], F32, "pg")
            wring = Ring([kb.sb(st, [128, 8, 1024], F32, "adaw%d" % i) for i in range(2)])
            for l in range(DEPTH):
                kb.dma(adab[:], V(ada_b, ada_b.h[l].rearrange("(j p) -> p j", p=128)))
                kb.dma(ng[:], V(norm_g, norm_g.h[l].rearrange("(k p) -> p k", p=128)))
                for blk in range(3):
                    wt = wring.next()
                    for k in range(8):
                        kb.dma(wt[:, k, :], ada_w[l, k * 128:(k + 1) * 128, blk * 1024:(blk + 1) * 1024],
                               q=("sp" if k % 2 == 0 else "pool"))
                    for jj in range(8):
                        j = blk * 8 + jj
                        for k in range(8):
                            kb.mm(pm[:, j:j + 1], wt[:, k, jj * 128:(jj + 1) * 128], cact[:, k:k + 1],
                                  start=(k == 0), stop=(k == 7), inc=(k == 7))
                kb.tt("dve", modc[:, l, :], pm[:], adab[:], ALU.add)
                kb.stt("dve", Acoef[:, l, :], modc[:, l, 8:16], 1.0, ng[:], ALU.add, ALU.mult)
                for j in range(8):
                    kb.mm(pg[:, j // 4, (j % 4) * 128:(j % 4 + 1) * 128],
                          modc[:, l, 16 + j:17 + j].bc([128, 128]), identf[:], inc=(j % 4 == 3))
                kb.cp("act", gate_row[:, l, :], pg[:].re("p a b -> p (a b)"))
        kb.barrier()
        if debug:
            kb.dma(dbg[:, :], modc[:].re("p l j -> p (l j)"))
            kb.barrier()

        for l in range(DEPTH):
            if stop <= 0:
                break
            x_src = x_in if l == 0 else xres
            last = (l == DEPTH - 1)
            phase_inproj(kb, nc, l, S, x_src, modc, Acoef, w_inF, w_inT, projF, projT, projTs, identb)
            kb.barrier()
            if stop <= 1:
                break
            phase_nsa(kb, nc, l, S, NCMP, NCC, projF, projT, projTs, mix, identb, identf,
                      c_tri4, c_atri4, c_E, c_c2s, c_cmask, c_selmul, c_seladd,
                      cmp_pos, ck_w1, ck_w2, cv_w1, cv_w2, nsa_ng)
            kb.barrier()
            if stop <= 2:
                break
            phase_diff(kb, nc, l, S, projF, projT, mix, identf, c_tri4, diff_lam, diff_ng)
            kb.barrier()
            if stop <= 3:
                break
            phase_ssd(kb, nc, l, S, projF, projT, projTs, mix, identb, identf, ones_f, c_U, c_mb_st,
                      ssm_cw, ssm_cb, ssm_dtb, ssm_alog, ssm_d, ssm_ng)
            kb.barrier()
            if stop <= 4:
                break
            phase_mlstm(kb, nc, l, S, projF, projT, projTs, mix, identb, identf, ones_f, c_U, c_mb_st, c_mb_ts,
                        c_sel127, ml_cw, ml_cb, ml_ifb, ml_ng)
            kb.barrier()
            if stop <= 5:
                break
            phase_out(kb, nc, l, S, x_src, (out_d if last else xres), mix, w_out, gate_row, fin_row, identb, last)
            kb.barrier()
    return nc


def bcast_rows(t, sl, n=128):
    return V(t, sl.partition_broadcast(n))


import os
INPROJ_SUB = 9


def phase_inproj(kb, nc, l, S, x_src, modc, Acoef, w_inF, w_inT, projF, projT, projTs, identb):
    NT = S // 128
    NG = S // 512
    SUB = INPROJ_SUB
    with contextlib.ExitStack() as st:
        wF = kb.sb(st, [128, 8, NF], BF16, "wF")
        wT = kb.sb(st, [128, 8, NTC], BF16, "wT")
        stg = Ring([kb.sb(st, [128, 1024], F32, "wstg%d" % i) for i in range(3)])
        ci = 0
        for (src, dst, ncol) in ((w_inF, wF, NF), (w_inT, wT, NTC)):
            for k in range(8):
                for c0 in range(0, ncol, 1024):
                    w = min(1024, ncol - c0)
                    sg = stg.next()
                    kb.dma(sg[:, 0:w], src[l, k * 128:(k + 1) * 128, c0:c0 + w], q=("sp" if ci % 2 == 0 else "pool"))
                    kb.cp(("dve" if ci % 2 == 0 else "act"), dst[:, k, c0:c0 + w], sg[:, 0:w])
                    ci += 1
        xin = Ring([kb.sb(st, [128, 4, D], F32, "xin%d" % i) for i in range(2)])
        hT = Ring([kb.sb(st, [128, 8, 512], BF16, "hT%d" % i) for i in range(2)])
        xn = Ring([kb.sb(st, [128, D], BF16, "xn%d" % i) for i in range(2)])
        junk = kb.sb(st, [128, D], BF16, "junk")
        ss = kb.sb(st, [128, 4], F32, "ss")
        rstd = kb.sb(st, [128, 4], F32, "rstd")
        ptr = Ring([kb.ps(st, [128, 8, 128], BF16, "ptr%d" % i) for i in range(2)])
        pF = Ring([kb.ps(st, [128, 512], F32, "pF%d" % i) for i in range(2)])
        pT = Ring([kb.ps(st, [128, 512], F32, "pT%d" % i) for i in range(2)])
        fstage = Ring([kb.sb(st, [128, 512], BF16, "fst%d" % i) for i in range(3)])
        tstage = Ring([kb.sb(st, [128, NTC], BF16, "tst%d" % i) for i in range(2)])
        tsstage = Ring([kb.sb(st, [128, 24], F32, "tsst%d" % i) for i in range(2)])
        ev = 0
        for g in range(NG if SUB >= 2 else 0):
            xi = xin.next()
            kb.dma(xi[:], V(x_src, x_src.h[g * 512:(g + 1) * 512, :].rearrange("(j p) d -> p j d", p=128)))
            h = hT.next()
            for j in range(4):
                kb.act(junk[:], xi[:, j, :], AF.Square, accum=ss[:, j:j + 1])
                kb.rsqrt(rstd[:, j:j + 1], ss[:, j:j + 1], 1.0 / D)
                xb = xn.next()
                kb.ts("dve", xb[:], xi[:, j, :], rstd[:, j:j + 1], None, ALU.mult)
                pt = ptr.next()
                for k in range(8):
                    kb.tr(pt[:, k, :], xb[:, k * 128:(k + 1) * 128], identb[:], inc=(k == 7))
                for k in range(8):
                    e = "act" if j % 2 == 0 else "dve"
                    if e == "act":
                        kb.act(h[:, k, j * 128:(j + 1) * 128], pt[:, k, :], AF.Identity,
                               bias=modc[:, l, k:k + 1], scale=Acoef[:, l, k:k + 1])
                    else:
                        kb.ts("dve", h[:, k, j * 128:(j + 1) * 128], pt[:, k, :], Acoef[:, l, k:k + 1],
                              modc[:, l, k:k + 1], ALU.mult, ALU.add)
            for (name, c0, w) in (F_GROUPS if SUB >= 3 else []):
                r0, _ = F_ROW[name]
                p = pF.next()
                for k in range(8):
                    kb.mm(p[0:w, :], wF[:, k, r0:r0 + w], h[:, k, :], start=(k == 0), stop=(k == 7), inc=(k == 7))
                fs = fstage.next()
                kb.cp(("act" if ev % 2 == 0 else "dve"), fs[0:w, :], p[0:w, :])
                ev += 1
                kb.dma(projF[r0:r0 + w, g * 512:(g + 1) * 512], fs[0:w, :], q="sp", disjoint=True)
            for j in range(4 if SUB >= 4 else 0):
                ts_ = tstage.next()
                tss = tsstage.next()
                for c0 in range(0, NTC, 512):
                    w = min(512, NTC - c0)
                    p = pT.next()
                    for k in range(8):
                        kb.mm(p[:, 0:w], h[:, k, j * 128:(j + 1) * 128], wT[:, k, c0:c0 + w],
                              start=(k == 0), stop=(k == 7), inc=(k == 7))
                    e_ = ("act" if ev % 2 == 0 else "dve")
                    kb.cp(e_, ts_[:, c0:c0 + w], p[:, 0:w])
                    ev += 1
                    for nm in (("ag", "dt", "dif") if SUB >= 5 else ()):
                        tc0, tw = T_COL[nm]
                        if c0 <= tc0 < c0 + w:
                            so, _ = TS_COL[nm]
                            kb.cp(e_, tss[:, so:so + tw], p[:, tc0 - c0:tc0 - c0 + tw])
                tok = g * 512 + j * 128
                if SUB >= 6:
                    kb.dma(projT[tok:tok + 128, :], ts_[:], q="sp", disjoint=True)
                if SUB >= 7:
                    kb.dma(projTs[tok:tok + 128, :], tss[:], q="sp", disjoint=True)


def load_T(kb, dst, projT, name, S, sub=None, q="sp"):
    NT = S // 128
    c0, w = T_COL[name]
    if sub is not None:
        c0, w = c0 + sub[0], sub[1]
    step = 8
    for a in range(0, NT, step):
        b = min(NT, a + step)
        kb.dma(dst[:, a:b, :], V(projT, projT.h[a * 128:b * 128, c0:c0 + w].rearrange("(c p) n -> p c n", p=128)), q=q)


def load_Ts(kb, dst, projTs, name, S):
    c0, w = TS_COL[name]
    kb.dma(dst[:], V(projTs, projTs.h[:, c0:c0 + w].rearrange("(c p) n -> p c n", p=128)))


def head_tail(kb, st_bufs, o, ng_row, sz, mix, tok, col0, post_scale, nheads=4, hd=64):
    sq, ssq, rs, yo = st_bufs
    W = nheads * hd
    kb.tt("pool", sq[:, 0:W], o, o, ALU.mult)
    kb.red(ssq[:, 0:nheads], sq[:, 0:W].re("p (h d) -> p h d", h=nheads), ALU.add)
    kb.rsqrt(rs[:, 0:nheads], ssq[:, 0:nheads], 1.0 / hd, post_scale)
    for h in range(nheads):
        kb.ts("dve", sq[:, h * hd:(h + 1) * hd], o[:, h * hd:(h + 1) * hd], rs[:, h:h + 1], None, ALU.mult)
    kb.tt("pool", sq[:, 0:W], sq[:, 0:W], ng_row, ALU.mult)
    kb.tt("dve", yo[:, 0:W], sq[:, 0:W], sz, ALU.mult)
    kb.dma(mix[tok:tok + 128, col0:col0 + W], yo[:, 0:W], q="sp", disjoint=True)


def silu_all(kb, st, z, S, name):
    NT = S // 128
    W = 256
    sg = kb.sb(st, [128, NT, W], BF16, name + "_sg")
    kb.act(sg[:], z[:], AF.Sigmoid)
    kb.tt("pool", z[:], z[:], sg[:], ALU.mult)
    return z


def phase_nsa(kb, nc, l, S, NCMP, NCC, projF, projT, projTs, mix, identb, identf,
              c_tri4, c_atri4, c_E, c_c2s, c_cmask, c_selmul, c_seladd,
              cmp_pos, ck_w1, ck_w2, cv_w1, cv_w2, nsa_ng):
    NT = S // 128
    NCP = NCC * 128
    with contextlib.ExitStack() as st:
        q_all = kb.sb(st, [128, NT, 4, 128], BF16, "q_all")
        qtiles = [kb.sub(q_all[:, i]) for i in range(NT)]
        kcT = kb.sb(st, [64, S], BF16, "kcT")
        vcT = kb.sb(st, [64, S], BF16, "vcT")
        kwT = kb.sb(st, [64, S], BF16, "kwT")
        lsel = kb.sb(st, [128, S], BF16, "lsel")
        vs1 = kb.sb(st, [128, NT, 65], BF16, "vs1")
        vw1 = kb.sb(st, [128, NT, 65], BF16, "vw1")
        gts = kb.sb(st, [128, NT, 12], F32, "gts")
        z = kb.sb(st, [128, NT, 256], BF16, "z")
        tri4 = kb.sb(st, [128, 512], BF16, "tri4")
        atri4 = kb.sb(st, [128, 512], BF16, "atri4")
        c2s = kb.sb(st, [128, NCC, 65], BF16, "c2s")
        cmask = kb.sb(st, [128, NCC, S], BF16, "cmask")
        selmul = kb.sb(st, [128, NT, 64], F32, "selmul")
        seladd = kb.sb(st, [128, NT, 64], F32, "seladd")
        ngrow = kb.sb(st, [128, 4, 64], F32, "ngrow")
        for h in range(4):
            r0, _ = F_ROW["aq%d" % h]
            for i in range(NT):
                pass
            kb.dma(V(q_all, q_all.h[0:64, :, h, :]), V(projF, projF.h[r0:r0 + 64, :].rearrange("d (c t) -> d c t", t=128)),
                   q=("sp" if h % 2 == 0 else "pool"))
        kb.memset("pool", V(q_all, q_all.h[64:128]), 0.0)
        kb.dma(kcT[:], projF[F_ROW["akc"][0]:F_ROW["akc"][0] + 64, :])
        kb.dma(vcT[:], projF[F_ROW["avc"][0]:F_ROW["avc"][0] + 64, :], q="pool")
        kb.dma(kwT[:], projF[F_ROW["akw"][0]:F_ROW["akw"][0] + 64, :])
        kb.dma(lsel[0:64, :], projF[F_ROW["aks"][0]:F_ROW["aks"][0] + 64, :], q="pool")
        kb.dma(lsel[64:128, :], c_E[:, :])
        kb.memset("pool", vs1[:, :, 64:65], 1.0)
        kb.memset("pool", vw1[:, :, 64:65], 1.0)
        load_T(kb, V(vs1, vs1.h[:, :, 0:64]), projT, "vs", S)
        load_T(kb, V(vw1, vw1.h[:, :, 0:64]), projT, "vw", S, q="pool")
        load_Ts(kb, gts, projTs, "ag", S)
        load_T(kb, z, projT, "az", S)
        kb.dma(tri4[:], c_tri4[:, :])
        kb.dma(atri4[:], c_atri4[:, :])
        kb.dma(c2s[:], V(c_c2s, c_c2s.h.rearrange("(c p) n -> p c n", p=128)))
        for cc in range(NCC):
            kb.dma(cmask[:, cc, :], c_cmask[cc * 128:(cc + 1) * 128, :], q=("sp" if cc == 0 else "pool"))
        kb.dma(selmul[:], V(c_selmul, c_selmul.h.rearrange("(c p) n -> p c n", p=128)))
        kb.dma(seladd[:], V(c_seladd, c_seladd.h.rearrange("(c p) n -> p c n", p=128)), q="pool")
        kb.dma(ngrow[:, 0, :], bcast_rows(nsa_ng, nsa_ng.h[l]))
        for h in range(1, 4):
            kb.cp("pool", ngrow[:, h, :], ngrow[:, 0, :])
        kb.act(gts[:], gts[:], AF.Sigmoid)

        kcmpT = kb.sb(st, [64, NCP], BF16, "kcmpT")
        vcmp1 = kb.sb(st, [128, NCC, 65], BF16, "vcmp1")
        kb.memset("pool", vcmp1[:, :, 64:65], 1.0)
        with contextlib.ExitStack() as s2:
            sz = silu_all(kb, s2, z, S, "az")
            posT = kb.sb(s2, [64, 32], F32, "posT")
            posTb = kb.sb(s2, [64, 32], BF16, "posTb")
            kb.dma(posT[:], V(cmp_pos, cmp_pos.h[l].rearrange("l d -> d l")))
            kb.cp("dve", posTb[:], posT[:])
            w1s = kb.sb(s2, [64, 16, 128], F32, "w1s")
            w1b = kb.sb(s2, [64, 32, 128], BF16, "w1b")
            w2s = kb.sb(s2, [128, 64], F32, "w2s")
            w2b = kb.sb(s2, [128, 64], BF16, "w2b")
            hid = kb.sb(s2, [128, NCP], BF16, "hid")
            tpre = kb.sb(s2, [128, NCP], F32, "tpre")
            sgm = kb.sb(s2, [128, NCP], F32, "sgm")
            cst = kb.sb(s2, [128, 1], F32, "cst")
            pc = kb.ps(s2, [128, 1], F32, "pc")
            ph = kb.ps(s2, [128, NCP], F32, "ph")
            po = kb.ps(s2, [128, NCP], F32, "po")
            for which, (w1d, w2d, srcT) in enumerate(((ck_w1, ck_w2, kcT), (cv_w1, cv_w2, vcT))):
                for hf in range(2):
                    kb.dma(w1s[:], V(w1d, w1d.h[l, hf * 1024:(hf + 1) * 1024, :].rearrange("(l d) h -> d l h", d=64)))
                    kb.cp("dve", w1b[:, hf * 16:(hf + 1) * 16, :], w1s[:])
                kb.dma(w2s[:], w2d[l])
                kb.cp("dve", w2b[:], w2s[:])
                for li in range(32):
                    kb.mm(pc[:], w1b[:, li, :], posTb[:, li:li + 1], start=(li == 0), stop=(li == 31), inc=(li == 31))
                kb.cp("dve", cst[:], pc[:])
                for li in range(32):
                    kb.mm(ph[:, 0:NCMP], w1b[:, li, :], V(srcT, srcT.h[:, li:li + 16 * (NCMP - 1) + 1:16]),
                          start=(li == 0), stop=(li == 31), inc=(li == 31))
                kb.memset("pool", hid[:], 0.0)
                kb.ts("dve", tpre[:, 0:NCMP], ph[:, 0:NCMP], cst[:, 0:1], None, ALU.add)
                kb.act(sgm[:, 0:NCMP], tpre[:, 0:NCMP], AF.Sigmoid)
                kb.tt("dve", hid[:, 0:NCMP], tpre[:, 0:NCMP], sgm[:, 0:NCMP], ALU.mult)
                if which == 0:
                    kb.mm(po[0:64, :], w2b[:], hid[:])
                    kb.cp("dve", kcmpT[:], po[0:64, :])
                else:
                    for cc in range(NCC):
                        kb.mm(po[:, cc * 64:(cc + 1) * 64], hid[:, cc * 128:(cc + 1) * 128], w2b[:], inc=(cc == NCC - 1))
                    kb.cp("dve", V(vcmp1, vcmp1.h[:, :, 0:64]), po[:, 0:NCC * 64].re("p (c d) -> p c d", c=NCC))
        kb.barrier()

        psc = Ring([kb.ps(st, [128, 512], F32, "psc%d" % i) for i in range(2)])
        pacc = [kb.ps(st, [65, 512], F32, "pacc%d" % i) for i in range(3)]
        pmisc = kb.ps(st, [128, 512], F32, "pmisc")
        ptl = kb.ps(st, [128, 6, 65], F32, "ptl")
        Pr = Ring([kb.sb(st, [128, 512], BF16, "P%d" % i) for i in range(4)])
        Pc = [kb.sb(st, [128, 512], BF16, "Pc%d" % i) for i in range(NCC)]
        oT = kb.sb(st, [65, 3, 512], F32, "oT")
        rdc = kb.sb(st, [128, 4], F32, "rdc")
        imp = kb.sb(st, [128, 64], F32, "imp")
        imp2 = kb.sb(st, [128, 64], F32, "imp2")
        m8 = kb.sb(st, [128, 16], F32, "m8")
        selpad = kb.sb(st, [128, 128], F32, "selpad")
        kb.memset("pool", selpad[:], 0.0)
        tl = kb.sb(st, [128, 12, 65], F32, "tl")
        rden = kb.sb(st, [128, 12], F32, "rden")
        fco = kb.sb(st, [128, 12], F32, "fco")
        o = kb.sb(st, [128, 256], F32, "o")
        bufs = (kb.sb(st, [128, 256], F32, "sq"), kb.sb(st, [128, 4], F32, "ssq"),
                kb.sb(st, [128, 4], F32, "rs"), kb.sb(st, [128, 256], BF16, "yo"))

        def expmask(p, dstP, mask):
            kb.act(dstP, p, AF.Exp, scale=0.125)
            if mask is not None:
                kb.tt("pool", dstP, dstP, mask, ALU.mult)

        for qi in range(NT):
            qt = qtiles[qi]
            rq = V(qt, qt.h[0:64].rearrange("d h t -> d (h t)"))
            rqs = V(qt, qt.h.rearrange("d h t -> d (h t)"))
            q0 = qi * 128
            ncv = min(NCC, (8 * qi + 6) // 128 + 1)
            for cc in range(ncv):
                p = psc.next()
                kb.mm(p[:], kcmpT[:, cc * 128:(cc + 1) * 128], rq)
                cm = V(cmask, cmask.h[:, cc, q0:q0 + 128].unsqueeze(1).to_broadcast([128, 4, 128]))
                kb.act(Pc[cc][:], p[:], AF.Exp, scale=0.125)
                kb.tt("pool", Pc[cc][:].re("p (h t) -> p h t", h=4), Pc[cc][:].re("p (h t) -> p h t", h=4), cm, ALU.mult)
            for cc in range(ncv):
                kb.mm(pacc[0][:], vcmp1[:, cc, :], Pc[cc][:], start=(cc == 0), stop=(cc == ncv - 1), inc=(cc == ncv - 1))
            for h in range(4):
                for cc in range(ncv):
                    kb.mm(pmisc[:, h * 65:(h + 1) * 65], Pc[cc][:, h * 128:(h + 1) * 128], c2s[:, cc, :],
                          start=(cc == 0), stop=(cc == ncv - 1), inc=(h == 3 and cc == ncv - 1))
            pim = pmisc[:, 0:260].re("p (h n) -> p h n", h=4)
            kb.ts("dve", rdc[:], pim[:, :, 64], 1e-30, None, ALU.max)
            kb.recip(rdc[:], rdc[:])
            kb.ts("dve", imp[:], pim[:, 0, 0:64], rdc[:, 0:1], None, ALU.mult)
            for h in range(1, 4):
                kb.stt("dve", imp[:], pim[:, h, 0:64], rdc[:, h:h + 1], imp[:], ALU.mult, ALU.add)
            kb.tt("dve", imp[:], imp[:], selmul[:, qi, :], ALU.mult)
            kb.tt("dve", imp[:], imp[:], seladd[:, qi, :], ALU.add)
            kb.op("dve", lambda e: e.max(out=m8[:, 0:8].ap, in_=imp[:].ap), [m8[:]], [imp[:]])
            kb.op("dve", lambda e: e.match_replace(out=imp2[:].ap, in_to_replace=m8[:, 0:8].ap,
                                                   in_values=imp[:].ap, imm_value=-3.0), [imp2[:]], [m8[:], imp[:]])
            kb.op("dve", lambda e: e.max(out=m8[:, 8:16].ap, in_=imp2[:].ap), [m8[:]], [imp2[:]])
            kb.ts("dve", selpad[:, 64:128], imp[:], m8[:, 15:16], -1.0, ALU.is_ge, ALU.add)
            kb.tr(pmisc[:, 260:388], selpad[:], identf[:])
            kb.cp("act", V(qt, qt.h[64:128]), V(pmisc, pmisc.h[64:128, 260:388].unsqueeze(1).to_broadcast([64, 4, 128])))
            wl = [kc for kc in range(qi - 4, qi + 1) if kc >= 0]
            for kc in wl:
                p = psc.next()
                kb.mm(p[:], kwT[:, kc * 128:(kc + 1) * 128], rq)
                P = Pr.next()
                expmask(p[:], P[:], tri4[:] if kc == qi else (atri4[:] if kc == qi - 4 else None))
                kb.mm(pacc[2][:], vw1[:, kc, :], P[:], start=(kc == wl[0]), stop=(kc == qi), inc=True)
            for kc in range(qi + 1):
                p = psc.next()
                kb.mm(p[:], lsel[:, kc * 128:(kc + 1) * 128], rqs)
                P = Pr.next()
                expmask(p[:], P[:], tri4[:] if kc == qi else None)
                kb.mm(pacc[1][:], vs1[:, kc, :], P[:], start=(kc == 0), stop=(kc == qi), inc=True)
            for b in range(3):
                kb.cp(("act" if b != 1 else "dve"), oT[:, b, :], pacc[b][:])
            for half in range(2):
                for i in range(6):
                    idx = half * 6 + i
                    b, h = idx // 4, idx % 4
                    kb.tr(ptl[:, i, :], oT[:, b, h * 128:(h + 1) * 128], identf[0:65, 0:65], inc=(i == 5))
                kb.cp("act", tl[:, half * 6:(half + 1) * 6, :], ptl[:])
            kb.ts("dve", rden[:], tl[:, :, 64], 1e-30, None, ALU.max)
            kb.recip(rden[:], rden[:])
            kb.tt("dve", fco[:].re("p (b h) -> p b h", b=3), rden[:].re("p (b h) -> p b h", b=3),
                  V(gts, gts.h[:, qi, :].rearrange("p (h b) -> p b h", b=3)), ALU.mult)
            for h in range(4):
                kb.ts("dve", o[:, h * 64:(h + 1) * 64], tl[:, h, 0:64], fco[:, h:h + 1], None, ALU.mult)
                for b in (1, 2):
                    kb.stt("dve", o[:, h * 64:(h + 1) * 64], tl[:, b * 4 + h, 0:64], fco[:, b * 4 + h:b * 4 + h + 1],
                           o[:, h * 64:(h + 1) * 64], ALU.mult, ALU.add)
            head_tail(kb, bufs, o[:], ngrow[:].re("p h d -> p (h d)"), sz[:, qi, :], mix, q0, 0, 1.0)


def phase_diff(kb, nc, l, S, projF, projT, mix, identf, c_tri4, diff_lam, diff_ng):
    NT = S // 128
    lambda_init = 0.8 - 0.6 * math.exp(-0.3 * l)
    sc = 32 ** -0.5
    with contextlib.ExitStack() as st:
        qT = kb.sb(st, [64, 4, S], BF16, "qT")
        kT = kb.sb(st, [64, 4, S], BF16, "kT")
        v1 = kb.sb(st, [128, NT, 4, 65], BF16, "v1")
        z = kb.sb(st, [128, NT, 256], BF16, "z")
        tri4 = kb.sb(st, [128, 512], BF16, "tri4")
        ngrow = kb.sb(st, [128, 4, 64], F32, "ngrow")
        lam = kb.sb(st, [128, 128], F32, "lam")
        lp = kb.sb(st, [128, 64], F32, "lp")
        ls = kb.sb(st, [128, 2], F32, "ls")
        nlam = kb.sb(st, [128, 1], F32, "nlam")
        s2 = contextlib.ExitStack()
        vtmp = kb.sb(s2, [128, NT, 256], BF16, "vtmp")
        for h in range(4):
            kb.dma(qT[:, h, :], projF[F_ROW["bq%d" % h][0]:F_ROW["bq%d" % h][0] + 64, :], q="sp")
            kb.dma(kT[:, h, :], projF[F_ROW["bk%d" % h][0]:F_ROW["bk%d" % h][0] + 64, :], q="pool")
        load_T(kb, vtmp, projT, "bv", S)
        load_T(kb, z, projT, "bz", S, q="pool")
        kb.dma(tri4[:], c_tri4[:, :])
        kb.dma(ngrow[:, 0, :], bcast_rows(diff_ng, diff_ng.h[l]))
        kb.dma(lam[:], bcast_rows(diff_lam, diff_lam.h[l]))
        for h in range(1, 4):
            kb.cp("pool", ngrow[:, h, :], ngrow[:, 0, :])
        kb.memset("pool", V(v1, v1.h[:, :, :, 64:65]), 1.0)
        kb.cp("pool", V(v1, v1.h[:, :, :, 0:64]), vtmp[:].re("p c (h d) -> p c h d", h=4))
        lv = lam[:].re("p (a b d) -> p a b d", a=2, b=2)
        kb.tt("dve", lp[:].re("p (a d) -> p a d", a=2), lv[:, :, 0, :], lv[:, :, 1, :], ALU.mult)
        kb.red(ls[:], lp[:].re("p (a d) -> p a d", a=2), ALU.add)
        kb.act(ls[:], ls[:], AF.Exp)
        kb.tt("dve", nlam[:], ls[:, 1:2], ls[:, 0:1], ALU.subtract)
        kb.ts("dve", nlam[:], nlam[:], -lambda_init, None, ALU.add)
        sz = silu_all(kb, s2, z, S, "bz")
        s2.close()
        kb.barrier()

        psc = Ring([kb.ps(st, [128, 512], F32, "psc%d" % i) for i in range(3)])
        pacc = [kb.ps(st, [65, 512], F32, "pacc%d" % i) for i in range(2)]
        ptl = Ring([kb.ps(st, [128, 4, 65], F32, "ptl%d" % i) for i in range(2)])
        Pr = Ring([kb.sb(st, [128, 512], BF16, "P%d" % i) for i in range(4)])
        oT = kb.sb(st, [65, 2, 512], F32, "oT")
        tl = kb.sb(st, [128, 8, 65], F32, "tl")
        rden = kb.sb(st, [128, 8], F32, "rden")
        o1 = kb.sb(st, [128, 64], F32, "o1")
        o = kb.sb(st, [128, 256], F32, "o")
        bufs = (kb.sb(st, [128, 256], F32, "sq"), kb.sb(st, [128, 4], F32, "ssq"),
                kb.sb(st, [128, 4], F32, "rs"), kb.sb(st, [128, 256], BF16, "yo"))
        qm = Ring([kb.sb(st, [64, 4, 2, 128], BF16, "qm%d" % i) for i in range(2)])
        for t_ in qm.tiles:
            kb.memset("pool", t_[:], 0.0)
        for qi in range(NT):
            q0 = qi * 128
            qmt = qm.next()
            kb.cp("pool", V(qmt, qmt.h[0:32, :, 0, :]), qT[0:32, :, q0:q0 + 128])
            kb.cp("pool", V(qmt, qmt.h[32:64, :, 1, :]), qT[32:64, :, q0:q0 + 128])
            for hp in range(2):
                for kc in range(qi + 1):
                    p = psc.next()
                    for hh in range(2):
                        h = hp * 2 + hh
                        kb.mm(p[:, hh * 256:(hh + 1) * 256], kT[:, h, kc * 128:(kc + 1) * 128],
                              V(qmt, qmt.h[:, h].rearrange("d c t -> d (c t)")), inc=(hh == 1))
                    P = Pr.next()
                    kb.act(P[:], p[:], AF.Exp, scale=sc)
                    if kc == qi:
                        kb.tt("pool", P[:], P[:], tri4[:], ALU.mult)
                    for hh in range(2):
                        h = hp * 2 + hh
                        kb.mm(pacc[hp][:, hh * 256:(hh + 1) * 256], v1[:, kc, h, :], P[:, hh * 256:(hh + 1) * 256],
                              start=(kc == 0 and hh == 0), stop=(kc == qi and hh == 1), inc=(hh == 1))
                kb.cp(("act" if hp == 0 else "dve"), oT[:, hp, :], pacc[hp][:])
            for half in range(2):
                pt = ptl.next()
                for i in range(4):
                    idx = half * 4 + i
                    kb.tr(pt[:, i, :], oT[:, half, i * 128:(i + 1) * 128], identf[0:65, 0:65], inc=(i == 3))
                kb.cp("act", tl[:, half * 4:(half + 1) * 4, :], pt[:])
            kb.ts("dve", rden[:], tl[:, :, 64], 1e-30, None, ALU.max)
            kb.recip(rden[:], rden[:])
            kb.ts("dve", V(rden, rden.h[:, 1:8:2]), V(rden, rden.h[:, 1:8:2]), nlam[:, 0:1], None, ALU.mult)
            for h in range(4):
                kb.ts("dve", o1[:], tl[:, 2 * h, 0:64], rden[:, 2 * h:2 * h + 1], None, ALU.mult)
                kb.stt("dve", o[:, h * 64:(h + 1) * 64], tl[:, 2 * h + 1, 0:64], rden[:, 2 * h + 1:2 * h + 2],
                       o1[:], ALU.mult, ALU.add)
            head_tail(kb, bufs, o[:], ngrow[:].re("p h d -> p (h d)"), sz[:, qi, :], mix, q0, 256, 1.0 - lambda_init)


def conv_silu(kb, eng, dst, src, acc, wcol, bcol, S, rows):
    kb.ts(eng, acc[0:rows, :], src, wcol[:, 3:4], bcol, ALU.mult, ALU.add)
    for k in range(3):
        sh = 3 - k
        kb.stt("dve", acc[0:rows, sh:S], src[:, 0:S - sh], wcol[:, k:k + 1], acc[0:rows, sh:S], ALU.mult, ALU.add)
    kb.act(dst, acc[0:rows, :], AF.Silu)


def phase_ssd(kb, nc, l, S, projF, projT, projTs, mix, identb, identf, ones_f, c_U, c_mb_st,
              ssm_cw, ssm_cb, ssm_dtb, ssm_alog, ssm_d, ssm_ng):
    NT = S // 128
    with contextlib.ExitStack() as st:
        U = kb.sb(st, [128, 128], F32, "U")
        mbst = kb.sb(st, [128, 128], F32, "mbst")
        kb.dma(U[:], c_U[:, :])
        kb.dma(mbst[:], c_mb_st[:, :])
        xT = kb.sb(st, [128, 2, S], BF16, "xT")
        BT = kb.sb(st, [64, 2, S], BF16, "BT")
        CT = kb.sb(st, [64, 2, S], BF16, "CT")
        xB = kb.sb(st, [128, NT, 384], BF16, "xB")
        z = kb.sb(st, [128, NT, 256], BF16, "z")
        dtr = kb.sb(st, [128, NT, 4], F32, "dtr")
        with contextlib.ExitStack() as s2:
            raw = Ring([kb.sb(s2, [128, S], BF16, "raw%d" % i) for i in range(2)])
            acc = Ring([kb.sb(s2, [128, S], F32, "acc%d" % i) for i in range(2)])
            wc = kb.sb(s2, [128, 6, 4], F32, "wc")
            bc_ = kb.sb(s2, [128, 6], F32, "bc")
            specs = [("cx0", 0, 128, xT, 0), ("cx1", 128, 128, xT, 1), ("cB0", 256, 64, BT, 0), ("cB1", 320, 64, BT, 1),
                     ("cC0", 384, 64, CT, 0), ("cC1", 448, 64, CT, 1)]
            for i, (nm, ch0, rows, dst, di) in enumerate(specs):
                kb.dma(wc[0:rows, i, :], V(ssm_cw, ssm_cw.h[l, :, ch0:ch0 + rows].rearrange("k c -> c k")))
                kb.dma(bc_[0:rows, i:i + 1], V(ssm_cb, ssm_cb.h[l, ch0:ch0 + rows].rearrange("(c o) -> c o", o=1)))
            for i, (nm, ch0, rows, dst, di) in enumerate(specs):
                r = raw.next()
                a = acc.next()
                r0 = F_ROW[nm][0]
                kb.dma(r[0:rows, :], projF[r0:r0 + rows, :], q=("sp" if i % 2 == 0 else "pool"))
                conv_silu(kb, ("dve" if i % 2 == 0 else "pool"), dst[0:rows, di, :], r[0:rows, :], a,
                          wc[0:rows, i, :], bc_[0:rows, i:i + 1], S, rows)
            load_T(kb, z, projT, "cz", S)
            sz = silu_all(kb, s2, z, S, "cz")
        kb.barrier()
        load_Ts(kb, dtr, projTs, "dt", S)
        dtb = kb.sb(st, [128, 4], F32, "dtb")
        aneg = kb.sb(st, [128, 4], F32, "aneg")
        dsk = kb.sb(st, [128, 4], F32, "dsk")
        ngrow = kb.sb(st, [128, 256], F32, "ngrow")
        kb.dma(dtb[:], bcast_rows(ssm_dtb, ssm_dtb.h[l]))
        kb.dma(aneg[:], bcast_rows(ssm_alog, ssm_alog.h[l]))
        kb.dma(dsk[:], bcast_rows(ssm_d, ssm_d.h[l]))
        kb.dma(ngrow[:], bcast_rows(ssm_ng, ssm_ng.h[l]))
        kb.act(aneg[:], aneg[:], AF.Exp)
        kb.ts("dve", aneg[:], aneg[:], -1.0, None, ALU.mult)
        dt = kb.sb(st, [128, NT, 4], F32, "dt")
        adt = kb.sb(st, [128, NT, 4], F32, "adt")
        kb.tt("dve", dt[:], dtr[:], V(dtb, dtb.h[:, :].unsqueeze(1).to_broadcast([128, NT, 4])), ALU.add)
        kb.act(dt[:], dt[:], AF.Exp)
        kb.act(dt[:], dt[:], AF.Ln, bias=1.0)
        kb.tt("dve", adt[:], dt[:], V(aneg, aneg.h[:, :].unsqueeze(1).to_broadcast([128, NT, 4])), ALU.mult)
        acs = kb.sb(st, [128, NT, 4], F32, "acs")
        alast = kb.sb(st, [128, NT, 4], F32, "alast")
        ea = kb.sb(st, [128, NT, 4], F32, "ea")
        de = kb.sb(st, [128, NT, 4], F32, "de")
        cd = kb.sb(st, [128, NT, 4], F32, "cd")
        with contextlib.ExitStack() as s2:
            pa = kb.ps(s2, [128, NT * 4], F32, "pa")
            pb = kb.ps(s2, [128, NT * 4], F32, "pb")
            kb.mm(pa[:], U[:], adt[:].re("p c h -> p (c h)"))
            kb.mm(pb[:], ones_f[:], adt[:].re("p c h -> p (c h)"))
            kb.cp("dve", acs[:].re("p c h -> p (c h)"), pa[:])
            kb.cp("dve", alast[:].re("p c h -> p (c h)"), pb[:])
        kb.barrier()
        kb.act(ea[:], acs[:], AF.Exp)
        kb.act(cd[:], alast[:], AF.Exp)
        kb.tt("dve", de[:], alast[:], acs[:], ALU.subtract)
        kb.act(de[:], de[:], AF.Exp)
        with contextlib.ExitStack() as s2:
            ptx = Ring([kb.ps(s2, [128, 384], BF16, "ptx%d" % i) for i in range(2)])
            for c in range(NT):
                pt = ptx.next()
                kb.tr(pt[:, 0:128], xT[:, 0, c * 128:(c + 1) * 128], identb[:], inc=False)
                kb.tr(pt[:, 128:256], xT[:, 1, c * 128:(c + 1) * 128], identb[:], inc=False)
                kb.tr(pt[:, 256:320], BT[:, 0, c * 128:(c + 1) * 128], identb[0:64, 0:64], inc=False)
                kb.tr(pt[:, 320:384], BT[:, 1, c * 128:(c + 1) * 128], identb[0:64, 0:64], inc=True)
                kb.cp(("act" if c % 2 == 0 else "dve"), xB[:, c, :], pt[:])
        kb.barrier()
        pR = kb.ps(st, [128, 4, 128], F32, "pR")
        pS = kb.ps(st, [128, 2, 128], F32, "pS")
        pY = kb.ps(st, [128, 256], F32, "pY")
        pO = kb.ps(st, [128, 256], F32, "pO")
        pN = kb.ps(st, [64, 4, 64], F32, "pN")
        arg = kb.sb(st, [128, 4, 128], F32, "arg")
        dec = kb.sb(st, [128, 4, 128], F32, "dec")
        GT = kb.sb(st, [128, 4, 128], BF16, "GT")
        xdt = kb.sb(st, [128, 4, 64], BF16, "xdt")
        xdw = kb.sb(st, [128, 4, 64], BF16, "xdw")
        stf = kb.sb(st, [64, 4, 64], F32, "stf")
        stb = kb.sb(st, [64, 4, 64], BF16, "stb")
        yd = kb.sb(st, [128, 256], F32, "yd")
        y = kb.sb(st, [128, 256], F32, "y")
        kb.memset("pool", stf[:], 0.0)
        kb.memset("pool", stb[:], 0.0)
        bufs = (kb.sb(st, [128, 256], F32, "sq"), kb.sb(st, [128, 4], F32, "ssq"),
                kb.sb(st, [128, 4], F32, "rs"), kb.sb(st, [128, 256], BF16, "yo"))
        for c in range(NT):
            t0 = c * 128
            xc = V(xB, xB.h[:, c, 0:256].rearrange("p (h d) -> p h d", h=4))
            for h in range(4):
                kb.mm(pR[:, h, :], adt[:, c, h:h + 1].bc([128, 128]), U[:], inc=(h == 3))
            for h in range(4):
                kb.stt("dve", arg[:, h, :], pR[:, h, :], acs[:, c, h:h + 1], mbst[:], ALU.subtract, ALU.add)
            kb.act(dec[:], arg[:], AF.Exp)
            for g in range(2):
                kb.mm(pS[:, g, :], BT[:, g, t0:t0 + 128], CT[:, g, t0:t0 + 128], inc=(g == 1))
            for h in range(4):
                kb.tt("dve", GT[:, h, :], pS[:, h // 2, :], dec[:, h, :], ALU.mult)
            kb.tt("pool", xdt[:], xc, V(dt, dt.h[:, c, :].unsqueeze(2).to_broadcast([128, 4, 64])), ALU.mult)
            kb.tt("pool", xdw[:], xdt[:], V(de, de.h[:, c, :].unsqueeze(2).to_broadcast([128, 4, 64])), ALU.mult)
            for h in range(4):
                kb.mm(pY[:, h * 64:(h + 1) * 64], GT[:, h, :], xdt[:, h, :], inc=(h == 3))
            for h in range(4):
                kb.mm(pO[:, h * 64:(h + 1) * 64], CT[:, h // 2, t0:t0 + 128], stb[:, h, :], inc=(h == 3))
            kb.cp("act", yd[:], pY[:])
            for h in range(4):
                hs = slice(h * 64, (h + 1) * 64)
                kb.stt("dve", y[:, hs], pO[:, hs], ea[:, c, h:h + 1], yd[:, hs], ALU.mult, ALU.add)
                kb.stt("dve", y[:, hs], xc[:, h, :], dsk[:, h:h + 1], y[:, hs], ALU.mult, ALU.add)
            for h in range(4):
                kb.mm(pN[:, h, :], xB[:, c, 256 + 64 * (h // 2):256 + 64 * (h // 2) + 64], xdw[:, h, :], inc=(h == 3))
            for h in range(4):
                kb.stt("dve", stf[:, h, :], stf[:, h, :], cd[0:64, c, h:h + 1], pN[:, h, :], ALU.mult, ALU.add)
            kb.cp("act", stb[:], stf[:])
            kb.tt("pool", y[:], y[:], sz[:, c, :], ALU.mult)
            head_tail(kb, bufs, y[:], ngrow[:], ones_f[:, 0:1].bc([128, 256]), mix, t0, 512, 1.0, nheads=2, hd=128)


def phase_mlstm(kb, nc, l, S, projF, projT, projTs, mix, identb, identf, ones_f, c_U, c_mb_st, c_mb_ts,
                c_sel127, ml_cw, ml_cb, ml_ifb, ml_ng):
    NT = S // 128
    with contextlib.ExitStack() as st:
        U = kb.sb(st, [128, 128], F32, "U")
        mbst = kb.sb(st, [128, 4, 128], F32, "mbst")
        mbts = kb.sb(st, [128, 4, 128], F32, "mbts")
        sel127 = kb.sb(st, [128, 128], F32, "sel127")
        kb.dma(U[:], c_U[:, :])
        kb.dma(sel127[:], c_sel127[:, :])
        for h in range(4):
            kb.dma(mbst[:, h, :], c_mb_st[:, :])
            kb.dma(mbts[:, h, :], c_mb_ts[:, :], q="pool")
        qT = kb.sb(st, [64, 4, S], BF16, "qT")
        kT = kb.sb(st, [64, 4, S], BF16, "kT")
        kTl = kb.sb(st, [128, NT, 4, 64], BF16, "kTl")
        v1 = kb.sb(st, [128, NT, 4, 65], BF16, "v1")
        z = kb.sb(st, [128, NT, 256], BF16, "z")
        og = kb.sb(st, [128, NT, 256], BF16, "og")
        ifr = kb.sb(st, [128, NT, 8], F32, "ifr")
        with contextlib.ExitStack() as s2:
            raw = Ring([kb.sb(s2, [64, S], BF16, "raw%d" % i) for i in range(2)])
            acc = Ring([kb.sb(s2, [64, S], F32, "acc%d" % i) for i in range(2)])
            wc = kb.sb(s2, [64, 8, 4], F32, "wc")
            bc_ = kb.sb(s2, [64, 8], F32, "bc")
            for i in range(8):
                ch0 = i * 64
                kb.dma(wc[:, i, :], V(ml_cw, ml_cw.h[l, :, ch0:ch0 + 64].rearrange("k c -> c k")))
                kb.dma(bc_[:, i:i + 1], V(ml_cb, ml_cb.h[l, ch0:ch0 + 64].rearrange("(c o) -> c o", o=1)))
            for i in range(8):
                nm = ("dq%d" % i) if i < 4 else ("dk%d" % (i - 4))
                dst = qT if i < 4 else kT
                r = raw.next()
                a = acc.next()
                r0 = F_ROW[nm][0]
                kb.dma(r[:], projF[r0:r0 + 64, :], q=("sp" if i % 2 == 0 else "pool"))
                conv_silu(kb, ("dve" if i % 2 == 0 else "pool"), dst[:, i % 4, :], r[:], a, wc[:, i, :], bc_[:, i:i + 1], S, 64)
        kb.barrier()
        with contextlib.ExitStack() as s2:
            vtmp = kb.sb(s2, [128, NT, 256], BF16, "vtmp")
            load_T(kb, vtmp, projT, "dv", S)
            load_T(kb, z, projT, "dz", S, q="pool")
            load_T(kb, og, projT, "do", S)
            kb.memset("pool", V(v1, v1.h[:, :, :, 64:65]), 1.0)
            kb.cp("pool", V(v1, v1.h[:, :, :, 0:64]), vtmp[:].re("p c (h d) -> p c h d", h=4))
            sz = silu_all(kb, s2, z, S, "dz")
            kb.act(og[:], og[:], AF.Sigmoid)
        kb.barrier()
        load_Ts(kb, ifr, projTs, "dif", S)
        ifb = kb.sb(st, [128, 8], F32, "ifb")
        ngrow = kb.sb(st, [128, 4, 64], F32, "ngrow")
        kb.dma(ifb[:], bcast_rows(ml_ifb, ml_ifb.h[l]))
        kb.dma(ngrow[:, 0, :], bcast_rows(ml_ng, ml_ng.h[l]))
        for h in range(1, 4):
            kb.cp("pool", ngrow[:, h, :], ngrow[:, 0, :])
        kb.tt("dve", ifr[:], ifr[:], V(ifb, ifb.h[:, :].unsqueeze(1).to_broadcast([128, NT, 8])), ALU.add)
        ig = V(ifr, ifr.h[:, :, 0:4])
        lf = kb.sb(st, [128, NT, 4], F32, "lf")
        kb.act(lf[:], V(ifr, ifr.h[:, :, 4:8]), AF.Exp, scale=-1.0)
        kb.act(lf[:], lf[:], AF.Ln, bias=1.0)
        kb.ts("dve", lf[:], lf[:], -1.0, None, ALU.mult)
        b = kb.sb(st, [128, NT, 4], F32, "b")
        blast = kb.sb(st, [128, NT, 4], F32, "blast")
        u = kb.sb(st, [128, NT, 4], F32, "u")
        with contextlib.ExitStack() as s2:
            pa = kb.ps(s2, [128, NT * 4], F32, "pa")
            pb = kb.ps(s2, [128, NT * 4], F32, "pb")
            kb.mm(pa[:], U[:], lf[:].re("p c h -> p (c h)"))
            kb.mm(pb[:], ones_f[:], lf[:].re("p c h -> p (c h)"))
            kb.cp("dve", b[:].re("p c h -> p (c h)"), pa[:])
            kb.cp("dve", blast[:].re("p c h -> p (c h)"), pb[:])
        kb.barrier()
        kb.tt("dve", u[:], ig, b[:], ALU.subtract)
        with contextlib.ExitStack() as s2:
            ptk = Ring([kb.ps(s2, [128, 4, 64], BF16, "ptk%d" % i) for i in range(2)])
            for c in range(NT):
                pt = ptk.next()
                for h in range(4):
                    kb.tr(pt[:, h, :], kT[:, h, c * 128:(c + 1) * 128], identb[0:64, 0:64], inc=(h == 3))
                kb.cp(("act" if c % 2 == 0 else "dve"), kTl[:, c, :, :], pt[:])
        kb.barrier()
        pM = kb.ps(st, [128, 4, 128], F32, "pM")
        pW = kb.ps(st, [128, 4, 128], F32, "pW")
        pSC = kb.ps(st, [128, 4, 128], F32, "pSC")
        pND = kb.ps(st, [128, 4, 65], F32, "pND")
        pIN = kb.ps(st, [128, 4, 65], F32, "pIN")
        pL = kb.ps(st, [64, 4, 65], F32, "pL")
        pU = kb.ps(st, [128, 4], F32, "pU")
        cmx = kb.sb(st, [128, 4], F32, "cmx")
        umax = kb.sb(st, [128, 4], F32, "umax")
        mprev = kb.sb(st, [128, 4], F32, "mprev")
        tmp = kb.sb(st, [128, 4], F32, "tmp")
        ntmp = kb.sb(st, [128, 4], F32, "ntmp")
        mt = kb.sb(st, [128, 4], F32, "mt")
        emt = kb.sb(st, [128, 4], F32, "emt")
        wint = kb.sb(st, [128, 4], F32, "wint")
        wend = kb.sb(st, [128, 4], F32, "wend")
        mm_ = kb.sb(st, [128, 4], F32, "mm")
        aprev = kb.sb(st, [128, 4], F32, "aprev")
        aloc = kb.sb(st, [128, 4], F32, "aloc")
        wT = kb.sb(st, [128, 4, 128], F32, "wT")
        sqk = kb.sb(st, [128, 4, 128], BF16, "sqk")
        nds = kb.sb(st, [128, 4, 65], F32, "nds")
        nd = kb.sb(st, [128, 4, 65], F32, "nd")
        dn = kb.sb(st, [128, 4], F32, "dn")
        hh_ = kb.sb(st, [128, 256], F32, "hh")
        kw = kb.sb(st, [128, 4, 64], BF16, "kw")
        cnf = kb.sb(st, [64, 4, 65], F32, "cnf")
        cnb = kb.sb(st, [64, 4, 65], BF16, "cnb")
        ltmp = kb.sb(st, [64, 4, 65], F32, "ltmp")
        kb.memset("pool", cnf[:], 0.0)
        kb.memset("pool", cnb[:], 0.0)
        kb.memset("pool", mprev[:], 0.0)
        bufs = (kb.sb(st, [128, 256], F32, "sq"), kb.sb(st, [128, 4], F32, "ssq"),
                kb.sb(st, [128, 4], F32, "rs"), kb.sb(st, [128, 256], BF16, "yo"))
        for c in range(NT):
            t0 = c * 128
            for h in range(4):
                kb.mm(pM[:, h, :], u[:, c, h:h + 1].bc([128, 128]), identf[:], start=(h == 0), stop=False, inc=False)
            kb.mm(pM[:].re("p h s -> p (h s)"), identf[:], mbts[:].re("p h s -> p (h s)"), start=False, stop=True)
            kb.red(cmx[:], pM[:], ALU.max)
            kb.mm(pU[:], sel127[:], cmx[:])
            kb.cp("dve", umax[:], pU[:])
            kb.tt("dve", tmp[:], cmx[:], mprev[:], ALU.max)
            kb.ts("dve", ntmp[:], tmp[:], -1.0, None, ALU.mult)
            kb.tt("dve", mt[:], tmp[:], b[:, c, :], ALU.add)
            kb.act(emt[:], mt[:], AF.Exp, scale=-1.0)
            kb.tt("dve", wint[:], mprev[:], tmp[:], ALU.subtract)
            kb.act(wint[:], wint[:], AF.Exp)
            for h in range(4):
                kb.mm(pW[:, h, :], ntmp[:, h:h + 1].bc([128, 128]), identf[:], start=(h == 0), stop=False, inc=False)
            kb.mm(pW[:].re("p h s -> p (h s)"), identf[:], mbst[:].re("p h s -> p (h s)"), start=False, stop=True)
            for h in range(4):
                kb.act(wT[:, h, :], pW[:, h, :], AF.Exp, bias=u[:, c, h:h + 1])
            for h in range(4):
                kb.mm(pSC[:, h, :], kT[:, h, t0:t0 + 128], qT[:, h, t0:t0 + 128], inc=(h == 3))
            kb.stt("dve", sqk[:], pSC[:], 0.125, wT[:], ALU.mult, ALU.mult)
            for h in range(4):
                kb.mm(pND[:, h, :], sqk[:, h, :], v1[:, c, h, :], inc=(h == 3))
            for h in range(4):
                kb.mm(pIN[:, h, :], qT[:, h, t0:t0 + 128], cnb[:, h, :], inc=(h == 3))
            kb.cp("act", nds[:], pND[:])
            for h in range(4):
                kb.stt("dve", nd[:, h, :], pIN[:, h, :], wint[:, h:h + 1], nds[:, h, :], ALU.mult, ALU.add)
            kb.ts("dve", dn[:], nd[:, :, 64], -1.0, None, ALU.mult)
            kb.tt("dve", dn[:], dn[:], nd[:, :, 64], ALU.max)
            kb.tt("dve", dn[:], dn[:], emt[:], ALU.max)
            kb.recip(dn[:], dn[:])
            for h in range(4):
                kb.ts("dve", hh_[:, h * 64:(h + 1) * 64], nd[:, h, 0:64], dn[:, h:h + 1], None, ALU.mult)
            kb.tt("pool", hh_[:], hh_[:], og[:, c, :], ALU.mult)
            kb.tt("dve", wend[:], u[:, c, :], umax[:], ALU.subtract)
            kb.act(wend[:], wend[:], AF.Exp)
            kb.ts("dve", wend[:], wend[:], 0.125, None, ALU.mult)
            kb.tt("dve", mm_[:], mprev[:], umax[:], ALU.max)
            kb.tt("dve", aprev[:], mprev[:], mm_[:], ALU.subtract)
            kb.act(aprev[:], aprev[:], AF.Exp)
            kb.tt("dve", aloc[:], umax[:], mm_[:], ALU.subtract)
            kb.act(aloc[:], aloc[:], AF.Exp)
            kb.tt("pool", kw[:], kTl[:, c, :, :], V(wend, wend.h[:, :].unsqueeze(2).to_broadcast([128, 4, 64])), ALU.mult)
            for h in range(4):
                kb.mm(pL[:, h, :], kw[:, h, :], v1[:, c, h, :], inc=(h == 3))
            for h in range(4):
                kb.ts("dve", ltmp[:, h, :], pL[:, h, :], aloc[0:64, h:h + 1], None, ALU.mult)
                kb.stt("dve", cnf[:, h, :], cnf[:, h, :], aprev[0:64, h:h + 1], ltmp[:, h, :], ALU.mult, ALU.add)
            kb.cp("act", cnb[:], cnf[:])
            kb.tt("dve", mprev[:], mm_[:], blast[:, c, :], ALU.add)
            head_tail(kb, bufs, hh_[:], ngrow[:].re("p h d -> p (h d)"), sz[:, c, :], mix, t0, 768, 1.0)


def phase_out(kb, nc, l, S, x_src, x_dst, mix, w_out, gate_row, fin_row, identb, last):
    NT = S // 128
    with contextlib.ExitStack() as st:
        wo = kb.sb(st, [128, 8, D], BF16, "wo")
        stg = Ring([kb.sb(st, [128, D], F32, "wstg%d" % i) for i in range(2)])
        for k in range(8):
            sg = stg.next()
            kb.dma(sg[:], w_out[l, k * 128:(k + 1) * 128, :], q=("sp" if k % 2 == 0 else "pool"))
            kb.cp(("dve" if k % 2 == 0 else "act"), wo[:, k, :], sg[:])
        mixr = Ring([kb.sb(st, [128, D], BF16, "mixt%d" % i) for i in range(2)])
        xr = Ring([kb.sb(st, [128, D], F32, "xt%d" % i) for i in range(2)])
        mT = Ring([kb.sb(st, [128, 8, 128], BF16, "mT%d" % i) for i in range(2)])
        yr = Ring([kb.sb(st, [128, D], F32, "y%d" % i) for i in range(2)])
        junk = kb.sb(st, [128, D], BF16, "junk")
        ss = kb.sb(st, [128, 1], F32, "ss")
        ptr = Ring([kb.ps(st, [128, 8, 128], BF16, "ptr%d" % i) for i in range(2)])
        py = Ring([kb.ps(st, [128, 512], F32, "py%d" % i) for i in range(4)])
        for t in range(NT):
            t0 = t * 128
            mt_ = mixr.next()
            xt = xr.next()
            kb.dma(mt_[:], mix[t0:t0 + 128, :])
            kb.dma(xt[:], x_src[t0:t0 + 128, :], q="pool")
            pt = ptr.next()
            for k in range(8):
                kb.tr(pt[:, k, :], mt_[:, k * 128:(k + 1) * 128], identb[:], inc=(k == 7))
            m = mT.next()
            kb.cp("act", m[:], pt[:])
            y = yr.next()
            for half in range(2):
                p = py.next()
                for k in range(8):
                    kb.mm(p[:], m[:, k, :], wo[:, k, half * 512:(half + 1) * 512], start=(k == 0), stop=(k == 7), inc=(k == 7))
                hs = slice(half * 512, (half + 1) * 512)
                kb.tt("dve", y[:, hs], p[:], gate_row[:, l, hs], ALU.mult)
                kb.tt("pool", y[:, hs], y[:, hs], xt[:, hs], ALU.add)
            if last:
                kb.act(junk[:], y[:], AF.Square, accum=ss[:])
                kb.rsqrt(ss[:], ss[:], 1.0 / D)
                kb.stt("dve", y[:], y[:], ss[:, 0:1], fin_row[:], ALU.mult, ALU.mult)
            kb.dma(x_dst[t0:t0 + 128, :], y[:], q="sp", disjoint=True)


def make_consts(S):
    NCMP = (S - 32) // 16 + 1
    NCC = (NCMP + 127) // 128
    bf = ml_dtypes.bfloat16
    k = np.arange(128)
    tri = (k[:, None] <= k[None, :]).astype(np.float32)
    c = {}
    c["c_identb"] = np.eye(128, dtype=np.float32).astype(bf)
    c["c_identf"] = np.eye(128, dtype=np.float32)
    c["c_tri4"] = np.tile(tri, (1, 4)).astype(bf)
    c["c_atri4"] = np.tile(1.0 - tri, (1, 4)).astype(bf)
    c["c_U"] = tri.copy()
    c["c_mb_st"] = ((1.0 - tri) * NEGB).astype(np.float32)
    c["c_mb_ts"] = np.ascontiguousarray(c["c_mb_st"].T)
    s127 = np.zeros((128, 128), np.float32)
    s127[127, :] = 1.0
    c["c_sel127"] = s127
    E = np.zeros((64, S), np.float32)
    keys = np.arange(S)
    E[keys // 64 % 64, keys] = 30000.0 * (keys // 64 < 64)
    c["c_E"] = E.astype(bf)
    n_sel = S // 64
    cmp_idx = np.arange(NCMP)[:, None] * 16 + np.arange(32)[None, :]
    sel_start = np.arange(n_sel) * 64
    overlap = np.clip(np.minimum(cmp_idx[:, -1:] + 1, sel_start[None, :] + 64)
                      - np.maximum(cmp_idx[:, :1], sel_start[None, :]), 0, None)
    c2s = np.zeros((NCC * 128, 65), np.float32)
    c2s[:NCMP, :n_sel] = overlap / 32.0
    c2s[:NCMP, 64] = 1.0
    c["c_c2s"] = c2s.astype(bf)
    cm = np.zeros((NCC * 128, S), np.float32)
    cm[:NCMP] = (cmp_idx[:, -1][:, None] <= keys[None, :]).astype(np.float32)
    c["c_cmask"] = cm.astype(bf)
    t = np.arange(S)
    cur = t // 64
    sid = np.arange(64)
    forced = (sid[None, :] == cur[:, None]) | (sid[None, :] == 0)
    allowed = (sid[None, :] <= cur[:, None]) & (sid[None, :] < n_sel)
    c["c_selmul"] = (allowed & ~forced).astype(np.float32)
    c["c_seladd"] = np.where(forced, 1e4, np.where(allowed, 0.0, -1.0)).astype(np.float32)
    return c


_NC_CACHE = {}


def run(inputs, S, DEPTH, ncores, debug=False, stop=99):
    key = (S, DEPTH, debug, stop)
    if key not in _NC_CACHE:
        _NC_CACHE[key] = build(S, DEPTH, debug, stop)
    nc = _NC_CACHE[key]
    consts = make_consts(S)
    f32 = np.float32
    w_in = np.asarray(inputs["w_in"], f32)
    shared = dict(consts)
    shared["w_inF"] = np.ascontiguousarray(w_in[:, :, F_COLS])
    shared["w_inT"] = np.ascontiguousarray(w_in[:, :, T_COLS])
    for nm in ("norm_g", "ada_w", "ada_b", "w_out", "nsa_cmp_pos", "nsa_ck_w1", "nsa_ck_w2", "nsa_cv_w1",
               "nsa_cv_w2", "nsa_norm_g", "diff_norm_g", "ssm_conv_w", "ssm_conv_b", "ssm_dt_bias",
               "ssm_a_log", "ssm_d", "ssm_norm_g", "ml_conv_w", "ml_conv_b", "ml_if_b", "ml_norm_g", "final_g"):
        shared[nm] = np.ascontiguousarray(np.asarray(inputs[nm], f32))
    shared["diff_lam"] = np.ascontiguousarray(np.asarray(inputs["diff_lam"], f32).reshape(DEPTH, 128))
    x = np.asarray(inputs["x"], f32)
    c = np.asarray(inputs["c"], f32)
    in_maps = []
    for i in range(ncores):
        m = dict(shared)
        m["x"] = np.ascontiguousarray(x[i])
        m["c"] = np.ascontiguousarray(c[i])
        in_maps.append(m)
    res = run_bass_kernel_spmd(nc, in_maps, core_ids=list(range(ncores)))
    return res


def kernel(**inputs):
    res = run(inputs, 4096, 2, 8)
    return np.stack([np.asarray(r["out"], np.float32) for r in res.results], axis=0)
```

```python
import contextlib
import math
import numpy as np
import ml_dtypes
import concourse.bass as bass
import concourse.mybir as mybir
from concourse.bass_utils import run_bass_kernel_spmd

F32 = mybir.dt.float32
BF16 = mybir.dt.bfloat16
AF = mybir.ActivationFunctionType
ALU = mybir.AluOpType
AX = mybir.AxisListType

D = 1024
NEGB = -30000.0
EPS = 1e-6

F_GROUPS = []
for h in range(4):
    F_GROUPS.append(("aq%d" % h, 0 + 64 * h, 64))
F_GROUPS += [("akc", 256, 64), ("avc", 320, 64), ("aks", 384, 64), ("akw", 512, 64)]
for h in range(4):
    F_GROUPS.append(("bq%d" % h, 908 + 64 * h, 64))
for h in range(4):
    F_GROUPS.append(("bk%d" % h, 1164 + 64 * h, 64))
F_GROUPS += [("cx0", 2188, 128), ("cx1", 2316, 128), ("cB0", 2444, 64), ("cB1", 2508, 64),
             ("cC0", 2572, 64), ("cC1", 2636, 64)]
for h in range(4):
    F_GROUPS.append(("dq%d" % h, 2704 + 64 * h, 64))
for h in range(4):
    F_GROUPS.append(("dk%d" % h, 2960 + 64 * h, 64))
F_ROW = {}
_r = 0
F_COLS = []
for (n_, c0_, w_) in F_GROUPS:
    F_ROW[n_] = (_r, w_)
    F_COLS += list(range(c0_, c0_ + w_))
    _r += w_
NF = _r
T_PARTS = [("vs", 448, 64), ("vw", 576, 64), ("ag", 640, 12), ("az", 652, 256),
           ("bv", 1420, 256), ("bz", 1676, 256), ("cz", 1932, 256), ("dt", 2700, 4),
           ("dv", 3216, 256), ("dif", 3472, 8), ("do", 3480, 256), ("dz", 3736, 256)]
T_COL = {}
_r = 0
T_COLS = []
for (n_, c0_, w_) in T_PARTS:
    T_COL[n_] = (_r, w_)
    T_COLS += list(range(c0_, c0_ + w_))
    _r += w_
NTC = _r
TS_COL = {"ag": (0, 12), "dt": (12, 4), "dif": (16, 8)}


class TT:
    def __init__(self, h, name=""):
        self.h = h
        self.w = {}
        self.r = {}
        self.name = name

    def __getitem__(self, key):
        return V(self, self.h[key])


class V:
    def __init__(self, t, ap):
        self.t = t
        self.ap = ap

    def __getitem__(self, key):
        return V(self.t, self.ap[key])

    def re(self, pat, **kw):
        return V(self.t, self.ap.rearrange(pat, **kw))

    def bc(self, shape):
        return V(self.t, self.ap.to_broadcast(list(shape)))

    def raw(self, fn):
        return V(self.t, fn(self.ap))


class Eng:
    def __init__(self, name, h, sem, key, is_pe=False):
        self.name = name
        self.h = h
        self.sem = sem
        self.key = key
        self.count = 0
        self.seen = {}
        self.is_pe = is_pe
        self.pend = False
        self.dsems = []
        self.dlast = []
        self.dnext = 0


class KB:
    def __init__(self, nc, es):
        self.nc = nc
        self.es = es
        self.sems = {}
        self.eng = {}
        for name, h, pe in (("pe", nc.tensor, True), ("act", nc.scalar, False),
                            ("dve", nc.vector, False), ("pool", nc.gpsimd, False),
                            ("sp", nc.sync, False)):
            s = es.enter_context(nc.semaphore("sem_" + name))
            self.sems[name] = s
            self.eng[name] = Eng(name, h, s, name, pe)
        for q, n in (("sp", 28), ("pool", 10), ("act", 6)):
            E = self.eng[q]
            for i in range(n):
                key = "d_%s_%d" % (q, i)
                s = es.enter_context(nc.semaphore(key))
                self.sems[key] = s
                E.dsems.append(key)
                E.dlast.append(0)
        self.bar = es.enter_context(nc.semaphore("barrier"))
        self.sems["bar"] = self.bar
        self.barcount = 0
        self.uid = 0

    def sb(self, st, shape, dt, name):
        self.uid += 1
        h = st.enter_context(self.nc.sbuf_tensor("%s_%d" % (name, self.uid), list(shape), dt))
        return TT(h, name)

    def ps(self, st, shape, dt, name):
        self.uid += 1
        esz = 4 if dt == F32 else 2
        n = 1
        for d_ in shape[1:]:
            n *= d_
        per_bank = 2048 // esz
        full = ((n + per_bank - 1) // per_bank) * per_bank
        h = st.enter_context(self.nc.psum_tensor("%s_%d" % (name, self.uid), [128, full], dt))
        ap = h[0:shape[0], 0:n]
        if len(shape) == 3:
            ap = ap.rearrange("p (a b) -> p a b", a=shape[1])
        t = TT(None, name)
        t.h = ap
        return t

    def sub(self, v):
        t = TT(None, v.t.name + "_sub")
        t.h = v.ap
        return t

    def _waits(self, E, outs, ins, disjoint=False):
        need = {}

        def add(d, own_ok):
            for k, val in d.items():
                if k == E.key and not own_ok:
                    continue
                if need.get(k, 0) < val:
                    need[k] = val
        for v in ins:
            add(v.t.w, True)
        for v in outs:
            if not disjoint:
                add(v.t.w, True)
                add(v.t.r, True)
        for k, val in need.items():
            if E.is_pe and k == E.key:
                continue
            if E.seen.get(k, 0) >= val:
                continue
            E.h.wait_ge(self.sems[k], val)
            E.seen[k] = val

    def _record(self, ev, outs, ins, disjoint=False):
        k, val = ev
        for v in ins:
            if v.t.r.get(k, 0) < val:
                v.t.r[k] = val
        for v in outs:
            if disjoint:
                v.t.w[k] = max(v.t.w.get(k, 0), val)
            else:
                v.t.w = {k: val}
                v.t.r = {}

    def op(self, eng, fn, outs, ins, inc=True, disjoint=False):
        E = self.eng[eng]
        self._waits(E, outs, ins, disjoint)
        inst = fn(E.h)
        if inc:
            E.count += 1
            inst.then_inc(E.sem, 1)
            ev = (E.key, E.count)
            E.pend = False
        else:
            ev = (E.key, E.count + 1)
            E.pend = True
        self._record(ev, outs, ins, disjoint)

    def dma(self, out, in_, q="sp", disjoint=False):
        E = self.eng[q]
        i = E.dnext % len(E.dsems)
        E.dnext += 1
        key = E.dsems[i]
        if E.dlast[i] and E.seen.get(key, 0) < E.dlast[i]:
            E.h.wait_ge(self.sems[key], E.dlast[i])
            E.seen[key] = E.dlast[i]
        self._waits(E, [out], [in_], disjoint)
        E.h.dma_start(out=out.ap, in_=in_.ap).then_inc(self.sems[key], 16)
        E.dlast[i] += 16
        self._record((key, E.dlast[i]), [out], [in_], disjoint)

    def barrier(self):
        sp = self.eng["sp"]
        assert not self.eng["pe"].pend
        for q in ("sp", "pool", "act"):
            E = self.eng[q]
            for i, key in enumerate(E.dsems):
                if E.dlast[i] and sp.seen.get(key, 0) < E.dlast[i]:
                    sp.h.wait_ge(self.sems[key], E.dlast[i])
                    sp.seen[key] = E.dlast[i]
        for n in ("pe", "act", "dve", "pool"):
            E = self.eng[n]
            if E.count and sp.seen.get(n, 0) < E.count:
                sp.h.wait_ge(E.sem, E.count)
                sp.seen[n] = E.count
        self.barcount += 1
        sp.h.sem_inc(self.bar, 1)
        for n in ("pe", "act", "dve", "pool"):
            E = self.eng[n]
            E.h.wait_ge(self.bar, self.barcount)
        for n, E in self.eng.items():
            for m, E2 in self.eng.items():
                if m != "sp":
                    E.seen[m] = E2.count
            for q in ("sp", "pool", "act"):
                Eq = self.eng[q]
                for i, key in enumerate(Eq.dsems):
                    E.seen[key] = Eq.dlast[i]

    def mm(self, out, lhsT, rhs, start=True, stop=True, inc=True):
        self.op("pe", lambda e: e.matmul(out.ap, lhsT.ap, rhs.ap, start=start, stop=stop),
                [out], [lhsT, rhs], inc=inc)

    def tr(self, out, in_, ident, inc=True):
        self.op("pe", lambda e: e.transpose(out.ap, in_.ap, ident.ap), [out], [in_, ident], inc=inc)

    def act(self, out, in_, func, bias=None, scale=1.0, accum=None):
        ins = [in_]
        outs = [out]
        kw = {}
        if isinstance(bias, V):
            ins.append(bias)
            kw["bias"] = bias.ap
        elif bias is not None:
            kw["bias"] = bias
        if isinstance(scale, V):
            ins.append(scale)
            kw["scale"] = scale.ap
        else:
            kw["scale"] = scale
        if accum is not None:
            outs.append(accum)
            kw["accum_out"] = accum.ap
        self.op("act", lambda e: e.activation(out.ap, in_.ap, func, **kw), outs, ins)

    def ts(self, eng, out, in0, s1, s2, op0, op1=None):
        ins = [in0]
        a1 = s1
        a2 = s2
        if isinstance(s1, V):
            ins.append(s1)
            a1 = s1.ap
        if isinstance(s2, V):
            ins.append(s2)
            a2 = s2.ap
        if op1 is None:
            self.op(eng, lambda e: e.tensor_scalar(out.ap, in0.ap, a1, None, op0), [out], ins)
        else:
            self.op(eng, lambda e: e.tensor_scalar(out.ap, in0.ap, a1, a2, op0, op1), [out], ins)

    def tt(self, eng, out, in0, in1, op):
        self.op(eng, lambda e: e.tensor_tensor(out.ap, in0.ap, in1.ap, op), [out], [in0, in1])

    def stt(self, eng, out, in0, s, in1, op0, op1):
        ins = [in0, in1]
        a = s
        if isinstance(s, V):
            ins.append(s)
            a = s.ap
        self.op(eng, lambda e: e.scalar_tensor_tensor(out.ap, in0.ap, a, in1.ap, op0, op1), [out], ins)

    def cp(self, eng, out, in_):
        if eng == "act":
            self.op("act", lambda e: e.copy(out.ap, in_.ap), [out], [in_])
        else:
            self.op(eng, lambda e: e.tensor_copy(out.ap, in_.ap), [out], [in_])

    def memset(self, eng, out, val):
        self.op(eng, lambda e: e.memset(out.ap, val), [out], [])

    def red(self, out, in_, op):
        self.op("dve", lambda e: e.tensor_reduce(out.ap, in_.ap, AX.X, op), [out], [in_])

    def rsqrt(self, out, in_, scale, post=1.0):
        n = out.ap.shape[0]
        self.act(out, in_, AF.Ln, bias=self.eps_col[0:n, 0:1], scale=scale)
        self.act(out, out, AF.Exp, scale=-0.5)
        if post != 1.0:
            self.ts("dve", out, out, float(post), None, ALU.mult)

    def recip(self, out, in_):
        self.op("dve", lambda e: e.reciprocal(out.ap, in_.ap), [out], [in_])


class Deferred:
    def __init__(self):
        self.q = []

    def push(self, fn, delay):
        e = [delay, fn]
        self.q.append(e)
        return e

    def force(self, e):
        for i, x in enumerate(self.q):
            if x is e:
                del self.q[i]
                e[1]()
                return

    def step(self):
        ready = [e for e in self.q if e[0] <= 0]
        self.q = [e for e in self.q if e[0] > 0]
        for e in self.q:
            e[0] -= 1
        for e in ready:
            e[1]()

    def flush(self):
        while self.q:
            self.step()


class Ring:
    def __init__(self, tiles):
        self.tiles = tiles
        self.i = 0

    def next(self):
        t = self.tiles[self.i % len(self.tiles)]
        self.i += 1
        return t


def build(S, DEPTH, debug=False, stop=99):
    NT = S // 128
    NG = S // 512
    NCMP = (S - 32) // 16 + 1
    NCC = (NCMP + 127) // 128
    nc = bass.Bass("TRN2", target_bir_lowering=False)

    def din(name, shape, dt=F32):
        return TT(nc.dram_tensor(name, list(shape), dt, kind="ExternalInput").ap(), name)

    def dscr(name, shape, dt):
        return TT(nc.dram_tensor(name, list(shape), dt, kind="Internal").ap(), name)

    x_in = din("x", [S, D])
    c_in = din("c", [D])
    norm_g = din("norm_g", [DEPTH, D])
    ada_w = din("ada_w", [DEPTH, D, 3 * D])
    ada_b = din("ada_b", [DEPTH, 3 * D])
    w_inF = din("w_inF", [DEPTH, D, NF])
    w_inT = din("w_inT", [DEPTH, D, NTC])
    w_out = din("w_out", [DEPTH, D, D])
    cmp_pos = din("nsa_cmp_pos", [DEPTH, 32, 64])
    ck_w1 = din("nsa_ck_w1", [DEPTH, 2048, 128])
    ck_w2 = din("nsa_ck_w2", [DEPTH, 128, 64])
    cv_w1 = din("nsa_cv_w1", [DEPTH, 2048, 128])
    cv_w2 = din("nsa_cv_w2", [DEPTH, 128, 64])
    nsa_ng = din("nsa_norm_g", [DEPTH, 64])
    diff_lam = din("diff_lam", [DEPTH, 128])
    diff_ng = din("diff_norm_g", [DEPTH, 64])
    ssm_cw = din("ssm_conv_w", [DEPTH, 4, 512])
    ssm_cb = din("ssm_conv_b", [DEPTH, 512])
    ssm_dtb = din("ssm_dt_bias", [DEPTH, 4])
    ssm_alog = din("ssm_a_log", [DEPTH, 4])
    ssm_d = din("ssm_d", [DEPTH, 4])
    ssm_ng = din("ssm_norm_g", [DEPTH, 256])
    ml_cw = din("ml_conv_w", [DEPTH, 4, 512])
    ml_cb = din("ml_conv_b", [DEPTH, 512])
    ml_ifb = din("ml_if_b", [DEPTH, 8])
    ml_ng = din("ml_norm_g", [DEPTH, 64])
    final_g = din("final_g", [D])
    c_identb = din("c_identb", [128, 128], BF16)
    c_identf = din("c_identf", [128, 128])
    c_tri4 = din("c_tri4", [128, 512], BF16)
    c_atri4 = din("c_atri4", [128, 512], BF16)
    c_U = din("c_U", [128, 128])
    c_mb_st = din("c_mb_st", [128, 128])
    c_mb_ts = din("c_mb_ts", [128, 128])
    c_sel127 = din("c_sel127", [128, 128])
    c_E = din("c_E", [64, S], BF16)
    c_c2s = din("c_c2s", [NCC * 128, 65], BF16)
    c_cmask = din("c_cmask", [NCC * 128, S], BF16)
    c_selmul = din("c_selmul", [S, 64])
    c_seladd = din("c_seladd", [S, 64])

    out_d = TT(nc.dram_tensor("out", [S, D], F32, kind="ExternalOutput").ap(), "out")
    xres = dscr("xres", [S, D], F32)
    if debug:
        projF = TT(nc.dram_tensor("projF", [NF, S], BF16, kind="ExternalOutput").ap(), "projF")
        projT = TT(nc.dram_tensor("projT", [S, NTC], BF16, kind="ExternalOutput").ap(), "projT")
        projTs = TT(nc.dram_tensor("projTs", [S, 24], F32, kind="ExternalOutput").ap(), "projTs")
        dbg = TT(nc.dram_tensor("dbg", [128, DEPTH * 24], F32, kind="ExternalOutput").ap(), "dbg")
    else:
        projF = dscr("projF", [NF, S], BF16)
        projT = dscr("projT", [S, NTC], BF16)
        projTs = dscr("projTs", [S, 24], F32)
    if debug:
        mix = TT(nc.dram_tensor("mix", [S, D], BF16, kind="ExternalOutput").ap(), "mix")
    else:
        mix = dscr("mix", [S, D], BF16)

    es = contextlib.ExitStack()
    with es:
        es.enter_context(nc.allow_non_contiguous_dma("small strided parameter loads"))
        es.enter_context(nc.allow_low_precision("bf16 matmul operands, fp32 accumulation"))
        kb = KB(nc, es)
        identb = kb.sb(es, [128, 128], BF16, "identb")
        identf = kb.sb(es, [128, 128], F32, "identf")
        ones_f = kb.sb(es, [128, 128], F32, "onesf")
        kb.dma(identb[:], c_identb[:, :])
        kb.dma(identf[:], c_identf[:, :])
        kb.memset("pool", ones_f[:], 1.0)
        kb.eps_col = kb.sb(es, [128, 1], F32, "epscol")
        kb.memset("pool", kb.eps_col[:], EPS)
        modc = kb.sb(es, [128, DEPTH, 24], F32, "modc")
        Acoef = kb.sb(es, [128, DEPTH, 8], F32, "Acoef")
        gate_row = kb.sb(es, [128, DEPTH, D], F32, "gate_row")
        fin_row = kb.sb(es, [128, D], F32, "fin_row")
        kb.dma(fin_row[:], V(final_g, final_g.h.partition_broadcast(128)))

        with contextlib.ExitStack() as st:
            cact = kb.sb(st, [128, 8], F32, "cact")
            kb.dma(cact[:], V(c_in, c_in.h.rearrange("(k p) -> p k", p=128)))
            csig = kb.sb(st, [128, 8], F32, "csig")
            kb.act(csig[:], cact[:], AF.Sigmoid)
            kb.tt("dve", cact[:], cact[:], csig[:], ALU.mult)
            adab = kb.sb(st, [128, 24], F32, "adab")
            ng = kb.sb(st, [128, 8], F32, "ng")
            pm = kb.ps(st, [128, 24], F32, "pm")
            pg = kb.ps(st, [128, 2, 512], F32, "pg")
            wring = Ring([kb.sb(st, [128, 8, 1024], F32, "adaw%d" % i) for i in range(2)])
            for l in range(DEPTH):
                kb.dma(adab[:], V(ada_b, ada_b.h[l].rearrange("(j p) -> p j", p=128)))
                kb.dma(ng[:], V(norm_g, norm_g.h[l].rearrange("(k p) -> p k", p=128)))
                for blk in range(3):
                    wt = wring.next()
                    for k in range(8):
                        kb.dma(wt[:, k, :], ada_w[l, k * 128:(k + 1) * 128, blk * 1024:(blk + 1) * 1024],
                               q=("sp" if k % 2 == 0 else "pool"))
                    for jj in range(8):
                        j = blk * 8 + jj
                        for k in range(8):
                            kb.mm(pm[:, j:j + 1], wt[:, k, jj * 128:(jj + 1) * 128], cact[:, k:k + 1],
                                  start=(k == 0), stop=(k == 7), inc=(k == 7))
                kb.tt("dve", modc[:, l, :], pm[:], adab[:], ALU.add)
                kb.stt("dve", Acoef[:, l, :], modc[:, l, 8:16], 1.0, ng[:], ALU.add, ALU.mult)
                for j in range(8):
                    kb.mm(pg[:, j // 4, (j % 4) * 128:(j % 4 + 1) * 128],
                          modc[:, l, 16 + j:17 + j].bc([128, 128]), identf[:], inc=(j % 4 == 3))
                kb.cp("act", gate_row[:, l, :], pg[:].re("p a b -> p (a b)"))
        kb.barrier()
        if debug:
            kb.dma(dbg[:, :], modc[:].re("p l j -> p (l j)"))
            kb.barrier()

        for l in range(DEPTH):
            if stop <= 0:
                break
            x_src = x_in if l == 0 else xres
            last = (l == DEPTH - 1)
            phase_inproj(kb, nc, l, S, x_src, modc, Acoef, w_inF, w_inT, projF, projT, projTs, identb)
            kb.barrier()
            if stop <= 1:
                break
            phase_nsa(kb, nc, l, S, NCMP, NCC, projF, projT, projTs, mix, identb, identf,
                      c_tri4, c_atri4, c_E, c_c2s, c_cmask, c_selmul, c_seladd,
                      cmp_pos, ck_w1, ck_w2, cv_w1, cv_w2, nsa_ng)
            kb.barrier()
            if stop <= 2:
                break
            phase_diff(kb, nc, l, S, projF, projT, mix, identf, c_tri4, diff_lam, diff_ng)
            kb.barrier()
            if stop <= 3:
                break
            phase_ssd(kb, nc, l, S, projF, projT, projTs, mix, identb, identf, ones_f, c_U, c_mb_st,
                      ssm_cw, ssm_cb, ssm_dtb, ssm_alog, ssm_d, ssm_ng)
            kb.barrier()
            if stop <= 4:
                break
            phase_mlstm(kb, nc, l, S, projF, projT, projTs, mix, identb, identf, ones_f, c_U, c_mb_st, c_mb_ts,
                        c_sel127, ml_cw, ml_cb, ml_ifb, ml_ng)
            kb.barrier()
            if stop <= 5:
                break
            phase_out(kb, nc, l, S, x_src, (out_d if last else xres), mix, w_out, gate_row, fin_row, identb, last)
            kb.barrier()
    return nc


def bcast_rows(t, sl, n=128):
    return V(t, sl.partition_broadcast(n))


import os
INPROJ_SUB = 9


def phase_inproj(kb, nc, l, S, x_src, modc, Acoef, w_inF, w_inT, projF, projT, projTs, identb):
    NT = S // 128
    NG = S // 512
    SUB = INPROJ_SUB
    with contextlib.ExitStack() as st:
        wF = kb.sb(st, [128, 8, NF], BF16, "wF")
        wT = kb.sb(st, [128, 8, NTC], BF16, "wT")
        stg = Ring([kb.sb(st, [128, 1024], F32, "wstg%d" % i) for i in range(3)])
        ci = 0
        for (src, dst, ncol) in ((w_inF, wF, NF), (w_inT, wT, NTC)):
            for k in range(8):
                for c0 in range(0, ncol, 1024):
                    w = min(1024, ncol - c0)
                    sg = stg.next()
                    kb.dma(sg[:, 0:w], src[l, k * 128:(k + 1) * 128, c0:c0 + w], q=("sp" if ci % 2 == 0 else "pool"))
                    kb.cp(("dve" if ci % 2 == 0 else "act"), dst[:, k, c0:c0 + w], sg[:, 0:w])
                    ci += 1
        xin = Ring([kb.sb(st, [128, 4, D], F32, "xin%d" % i) for i in range(2)])
        hT = Ring([kb.sb(st, [128, 8, 512], BF16, "hT%d" % i) for i in range(2)])
        xn = Ring([kb.sb(st, [128, D], BF16, "xn%d" % i) for i in range(2)])
        junk = kb.sb(st, [128, D], BF16, "junk")
        ss = kb.sb(st, [128, 4], F32, "ss")
        rstd = kb.sb(st, [128, 4], F32, "rstd")
        ptr = Ring([kb.ps(st, [128, 8, 128], BF16, "ptr%d" % i) for i in range(2)])
        pF = Ring([kb.ps(st, [128, 512], F32, "pF%d" % i) for i in range(2)])
        pT = Ring([kb.ps(st, [128, 512], F32, "pT%d" % i) for i in range(2)])
        fstage = Ring([kb.sb(st, [128, 512], BF16, "fst%d" % i) for i in range(3)])
        tstage = Ring([kb.sb(st, [128, NTC], BF16, "tst%d" % i) for i in range(2)])
        tsstage = Ring([kb.sb(st, [128, 24], F32, "tsst%d" % i) for i in range(2)])
        ev = 0
        for g in range(NG if SUB >= 2 else 0):
            xi = xin.next()
            kb.dma(xi[:], V(x_src, x_src.h[g * 512:(g + 1) * 512, :].rearrange("(j p) d -> p j d", p=128)))
            h = hT.next()
            for j in range(4):
                kb.act(junk[:], xi[:, j, :], AF.Square, accum=ss[:, j:j + 1])
                kb.rsqrt(rstd[:, j:j + 1], ss[:, j:j + 1], 1.0 / D)
                xb = xn.next()
                kb.ts("dve", xb[:], xi[:, j, :], rstd[:, j:j + 1], None, ALU.mult)
                pt = ptr.next()
                for k in range(8):
                    kb.tr(pt[:, k, :], xb[:, k * 128:(k + 1) * 128], identb[:], inc=(k == 7))
                for k in range(8):
                    e = "act" if j % 2 == 0 else "dve"
                    if e == "act":
                        kb.act(h[:, k, j * 128:(j + 1) * 128], pt[:, k, :], AF.Identity,
                               bias=modc[:, l, k:k + 1], scale=Acoef[:, l, k:k + 1])
                    else:
                        kb.ts("dve", h[:, k, j * 128:(j + 1) * 128], pt[:, k, :], Acoef[:, l, k:k + 1],
                              modc[:, l, k:k + 1], ALU.mult, ALU.add)
            for (name, c0, w) in (F_GROUPS if SUB >= 3 else []):
                r0, _ = F_ROW[name]
                p = pF.next()
                for k in range(8):
                    kb.mm(p[0:w, :], wF[:, k, r0:r0 + w], h[:, k, :], start=(k == 0), stop=(k == 7), inc=(k == 7))
                fs = fstage.next()
                kb.cp(("act" if ev % 2 == 0 else "dve"), fs[0:w, :], p[0:w, :])
                ev += 1
                kb.dma(projF[r0:r0 + w, g * 512:(g + 1) * 512], fs[0:w, :], q="sp", disjoint=True)
            for j in range(4 if SUB >= 4 else 0):
                ts_ = tstage.next()
                tss = tsstage.next()
                for c0 in range(0, NTC, 512):
                    w = min(512, NTC - c0)
                    p = pT.next()
                    for k in range(8):
                        kb.mm(p[:, 0:w], h[:, k, j * 128:(j + 1) * 128], wT[:, k, c0:c0 + w],
                              start=(k == 0), stop=(k == 7), inc=(k == 7))
                    e_ = ("act" if ev % 2 == 0 else "dve")
                    kb.cp(e_, ts_[:, c0:c0 + w], p[:, 0:w])
                    ev += 1
                    for nm in (("ag", "dt", "dif") if SUB >= 5 else ()):
                        tc0, tw = T_COL[nm]
                        if c0 <= tc0 < c0 + w:
                            so, _ = TS_COL[nm]
                            kb.cp(e_, tss[:, so:so + tw], p[:, tc0 - c0:tc0 - c0 + tw])
                tok = g * 512 + j * 128
                if SUB >= 6:
                    kb.dma(projT[tok:tok + 128, :], ts_[:], q="sp", disjoint=True)
                if SUB >= 7:
                    kb.dma(projTs[tok:tok + 128, :], tss[:], q="sp", disjoint=True)


def load_T(kb, dst, projT, name, S, sub=None, q="sp"):
    NT = S // 128
    c0, w = T_COL[name]
    if sub is not None:
        c0, w = c0 + sub[0], sub[1]
    step = 8
    for a in range(0, NT, step):
        b = min(NT, a + step)
        kb.dma(dst[:, a:b, :], V(projT, projT.h[a * 128:b * 128, c0:c0 + w].rearrange("(c p) n -> p c n", p=128)), q=q)


def load_Ts(kb, dst, projTs, name, S):
    c0, w = TS_COL[name]
    kb.dma(dst[:], V(projTs, projTs.h[:, c0:c0 + w].rearrange("(c p) n -> p c n", p=128)))


def head_tail(kb, st_bufs, o, ng_row, sz, mix, tok, col0, post_scale, nheads=4, hd=64):
    sq, ssq, rs, yo = st_bufs
    W = nheads * hd
    kb.tt("pool", sq[:, 0:W], o, o, ALU.mult)
    kb.red(ssq[:, 0:nheads], sq[:, 0:W].re("p (h d) -> p h d", h=nheads), ALU.add)
    kb.rsqrt(rs[:, 0:nheads], ssq[:, 0:nheads], 1.0 / hd, post_scale)
    for h in range(nheads):
        kb.ts("dve", sq[:, h * hd:(h + 1) * hd], o[:, h * hd:(h + 1) * hd], rs[:, h:h + 1], None, ALU.mult)
    kb.tt("pool", sq[:, 0:W], sq[:, 0:W], ng_row, ALU.mult)
    kb.tt("dve", yo[:, 0:W], sq[:, 0:W], sz, ALU.mult)
    kb.dma(mix[tok:tok + 128, col0:col0 + W], yo[:, 0:W], q="sp", disjoint=True)


def silu_all(kb, st, z, S, name):
    NT = S // 128
    W = 256
    sg = kb.sb(st, [128, NT, W], BF16, name + "_sg")
    kb.act(sg[:], z[:], AF.Sigmoid)
    kb.tt("pool", z[:], z[:], sg[:], ALU.mult)
    return z


def phase_nsa(kb, nc, l, S, NCMP, NCC, projF, projT, projTs, mix, identb, identf,
              c_tri4, c_atri4, c_E, c_c2s, c_cmask, c_selmul, c_seladd,
              cmp_pos, ck_w1, ck_w2, cv_w1, cv_w2, nsa_ng):
    NT = S // 128
    NCP = NCC * 128
    with contextlib.ExitStack() as st:
        q_all = kb.sb(st, [128, NT, 4, 128], BF16, "q_all")
        qtiles = [kb.sub(q_all[:, i]) for i in range(NT)]
        kcT = kb.sb(st, [64, S], BF16, "kcT")
        vcT = kb.sb(st, [64, S], BF16, "vcT")
        kwT = kb.sb(st, [64, S], BF16, "kwT")
        lsel = kb.sb(st, [128, S], BF16, "lsel")
        vs1 = kb.sb(st, [128, NT, 65], BF16, "vs1")
        vw1 = kb.sb(st, [128, NT, 65], BF16, "vw1")
        gts = kb.sb(st, [128, NT, 12], F32, "gts")
        z = kb.sb(st, [128, NT, 256], BF16, "z")
        tri4 = kb.sb(st, [128, 512], BF16, "tri4")
        atri4 = kb.sb(st, [128, 512], BF16, "atri4")
        c2s = kb.sb(st, [128, NCC, 65], BF16, "c2s")
        cmask = kb.sb(st, [128, NCC, S], BF16, "cmask")
        selmul = kb.sb(st, [128, NT, 64], F32, "selmul")
        seladd = kb.sb(st, [128, NT, 64], F32, "seladd")
        ngrow = kb.sb(st, [128, 4, 64], F32, "ngrow")
        for h in range(4):
            r0, _ = F_ROW["aq%d" % h]
            for i in range(NT):
                pass
            kb.dma(V(q_all, q_all.h[0:64, :, h, :]), V(projF, projF.h[r0:r0 + 64, :].rearrange("d (c t) -> d c t", t=128)),
                   q=("sp" if h % 2 == 0 else "pool"))
        kb.memset("pool", V(q_all, q_all.h[64:128]), 0.0)
        kb.dma(kcT[:], projF[F_ROW["akc"][0]:F_ROW["akc"][0] + 64, :])
        kb.dma(vcT[:], projF[F_ROW["avc"][0]:F_ROW["avc"][0] + 64, :], q="pool")
        kb.dma(kwT[:], projF[F_ROW["akw"][0]:F_ROW["akw"][0] + 64, :])
        kb.dma(lsel[0:64, :], projF[F_ROW["aks"][0]:F_ROW["aks"][0] + 64, :], q="pool")
        kb.dma(lsel[64:128, :], c_E[:, :])
        kb.memset("pool", vs1[:, :, 64:65], 1.0)
        kb.memset("pool", vw1[:, :, 64:65], 1.0)
        load_T(kb, V(vs1, vs1.h[:, :, 0:64]), projT, "vs", S)
        load_T(kb, V(vw1, vw1.h[:, :, 0:64]), projT, "vw", S, q="pool")
        load_Ts(kb, gts, projTs, "ag", S)
        load_T(kb, z, projT, "az", S)
        kb.dma(tri4[:], c_tri4[:, :])
        kb.dma(atri4[:], c_atri4[:, :])
        kb.dma(c2s[:], V(c_c2s, c_c2s.h.rearrange("(c p) n -> p c n", p=128)))
        for cc in range(NCC):
            kb.dma(cmask[:, cc, :], c_cmask[cc * 128:(cc + 1) * 128, :], q=("sp" if cc == 0 else "pool"))
        kb.dma(selmul[:], V(c_selmul, c_selmul.h.rearrange("(c p) n -> p c n", p=128)))
        kb.dma(seladd[:], V(c_seladd, c_seladd.h.rearrange("(c p) n -> p c n", p=128)), q="pool")
        kb.dma(ngrow[:, 0, :], bcast_rows(nsa_ng, nsa_ng.h[l]))
        for h in range(1, 4):
            kb.cp("pool", ngrow[:, h, :], ngrow[:, 0, :])
        kb.act(gts[:], gts[:], AF.Sigmoid)

        kcmpT = kb.sb(st, [64, NCP], BF16, "kcmpT")
        vcmp1 = kb.sb(st, [128, NCC, 65], BF16, "vcmp1")
        kb.memset("pool", vcmp1[:, :, 64:65], 1.0)
        with contextlib.ExitStack() as s2:
            sz = silu_all(kb, s2, z, S, "az")
            posT = kb.sb(s2, [64, 32], F32, "posT")
            posTb = kb.sb(s2, [64, 32], BF16, "posTb")
            kb.dma(posT[:], V(cmp_pos, cmp_pos.h[l].rearrange("l d -> d l")))
            kb.cp("dve", posTb[:], posT[:])
            w1s = kb.sb(s2, [64, 16, 128], F32, "w1s")
            w1b = kb.sb(s2, [64, 32, 128], BF16, "w1b")
            w2s = kb.sb(s2, [128, 64], F32, "w2s")
            w2b = kb.sb(s2, [128, 64], BF16, "w2b")
            hid = kb.sb(s2, [128, NCP], BF16, "hid")
            tpre = kb.sb(s2, [128, NCP], F32, "tpre")
            sgm = kb.sb(s2, [128, NCP], F32, "sgm")
            cst = kb.sb(s2, [128, 1], F32, "cst")
            pc = kb.ps(s2, [128, 1], F32, "pc")
            ph = kb.ps(s2, [128, NCP], F32, "ph")
            po = kb.ps(s2, [128, NCP], F32, "po")
            for which, (w1d, w2d, srcT) in enumerate(((ck_w1, ck_w2, kcT), (cv_w1, cv_w2, vcT))):
                for hf in range(2):
                    kb.dma(w1s[:], V(w1d, w1d.h[l, hf * 1024:(hf + 1) * 1024, :].rearrange("(l d) h -> d l h", d=64)))
                    kb.cp("dve", w1b[:, hf * 16:(hf + 1) * 16, :], w1s[:])
                kb.dma(w2s[:], w2d[l])
                kb.cp("dve", w2b[:], w2s[:])
                for li in range(32):
                    kb.mm(pc[:], w1b[:, li, :], posTb[:, li:li + 1], start=(li == 0), stop=(li == 31), inc=(li == 31))
                kb.cp("dve", cst[:], pc[:])
                for li in range(32):
                    kb.mm(ph[:, 0:NCMP], w1b[:, li, :], V(srcT, srcT.h[:, li:li + 16 * (NCMP - 1) + 1:16]),
                          start=(li == 0), stop=(li == 31), inc=(li == 31))
                kb.memset("pool", hid[:], 0.0)
                kb.ts("dve", tpre[:, 0:NCMP], ph[:, 0:NCMP], cst[:, 0:1], None, ALU.add)
                kb.act(sgm[:, 0:NCMP], tpre[:, 0:NCMP], AF.Sigmoid)
                kb.tt("dve", hid[:, 0:NCMP], tpre[:, 0:NCMP], sgm[:, 0:NCMP], ALU.mult)
                if which == 0:
                    kb.mm(po[0:64, :], w2b[:], hid[:])
                    kb.cp("dve", kcmpT[:], po[0:64, :])
                else:
                    for cc in range(NCC):
                        kb.mm(po[:, cc * 64:(cc + 1) * 64], hid[:, cc * 128:(cc + 1) * 128], w2b[:], inc=(cc == NCC - 1))
                    kb.cp("dve", V(vcmp1, vcmp1.h[:, :, 0:64]), po[:, 0:NCC * 64].re("p (c d) -> p c d", c=NCC))
        kb.barrier()

        psc = Ring([kb.ps(st, [128, 512], F32, "psc%d" % i) for i in range(3)])
        pacc = [kb.ps(st, [65, 512], F32, "pacc%d" % i) for i in range(3)]
        pmisc = kb.ps(st, [128, 512], F32, "pmisc")
        ptl = kb.ps(st, [128, 6, 65], F32, "ptl")
        Pr = Ring([kb.sb(st, [128, 512], BF16, "P%d" % i) for i in range(4)])
        Pc = [kb.sb(st, [128, 512], BF16, "Pc%d" % i) for i in range(NCC)]
        oTs = [kb.sb(st, [65, 3, 512], F32, "oT%d" % i) for i in range(2)]
        rdc = kb.sb(st, [128, 4], F32, "rdc")
        imp = kb.sb(st, [128, 64], F32, "imp")
        imp2 = kb.sb(st, [128, 64], F32, "imp2")
        m8 = kb.sb(st, [128, 16], F32, "m8")
        selpad = kb.sb(st, [128, 128], F32, "selpad")
        kb.memset("pool", selpad[:], 0.0)
        tl = kb.sb(st, [128, 12, 65], F32, "tl")
        rden = kb.sb(st, [128, 12], F32, "rden")
        fco = kb.sb(st, [128, 12], F32, "fco")
        o = kb.sb(st, [128, 256], F32, "o")
        bufs = (kb.sb(st, [128, 256], F32, "sq"), kb.sb(st, [128, 4], F32, "ssq"),
                kb.sb(st, [128, 4], F32, "rs"), kb.sb(st, [128, 256], BF16, "yo"))

        def select1(qi, ncv):
            for h in range(4):
                for cc in range(ncv):
                    kb.mm(pmisc[:, h * 65:(h + 1) * 65], Pc[cc][:, h * 128:(h + 1) * 128], c2s[:, cc, :],
                          start=(cc == 0), stop=(cc == ncv - 1), inc=(h == 3 and cc == ncv - 1))
            pim = pmisc[:, 0:260].re("p (h n) -> p h n", h=4)
            kb.ts("dve", rdc[:], pim[:, :, 64], 1e-30, None, ALU.max)
            kb.recip(rdc[:], rdc[:])
            kb.ts("dve", imp[:], pim[:, 0, 0:64], rdc[:, 0:1], None, ALU.mult)
            for h in range(1, 4):
                kb.stt("dve", imp[:], pim[:, h, 0:64], rdc[:, h:h + 1], imp[:], ALU.mult, ALU.add)
            kb.tt("dve", imp[:], imp[:], selmul[:, qi, :], ALU.mult)
            kb.tt("dve", imp[:], imp[:], seladd[:, qi, :], ALU.add)
            kb.op("dve", lambda e: e.max(out=m8[:, 0:8].ap, in_=imp[:].ap), [m8[:]], [imp[:]])
            kb.op("dve", lambda e: e.match_replace(out=imp2[:].ap, in_to_replace=m8[:, 0:8].ap,
                                                   in_values=imp[:].ap, imm_value=-3.0), [imp2[:]], [m8[:], imp[:]])
            kb.op("dve", lambda e: e.max(out=m8[:, 8:16].ap, in_=imp2[:].ap), [m8[:]], [imp2[:]])
            kb.ts("dve", selpad[:, 64:128], imp[:], m8[:, 15:16], -1.0, ALU.is_ge, ALU.add)

        def select2(qi):
            qt = qtiles[qi]
            kb.tr(pmisc[:, 260:388], selpad[:], identf[:])
            kb.cp("dve", V(qt, qt.h[64:128]), V(pmisc, pmisc.h[64:128, 260:388].unsqueeze(1).to_broadcast([64, 4, 128])))

        def tail(qi):
            q0 = qi * 128
            oT = oTs[qi % 2]
            for half in range(2):
                for i in range(6):
                    idx = half * 6 + i
                    b, h = idx // 4, idx % 4
                    kb.tr(ptl[:, i, :], oT[:, b, h * 128:(h + 1) * 128], identf[0:65, 0:65], inc=(i == 5))
                kb.cp("dve", tl[:, half * 6:(half + 1) * 6, :], ptl[:])
            kb.ts("dve", rden[:], tl[:, :, 64], 1e-30, None, ALU.max)
            kb.recip(rden[:], rden[:])
            kb.tt("dve", fco[:].re("p (b h) -> p b h", b=3), rden[:].re("p (b h) -> p b h", b=3),
                  V(gts, gts.h[:, qi, :].rearrange("p (h b) -> p b h", b=3)), ALU.mult)
            for h in range(4):
                kb.ts("dve", o[:, h * 64:(h + 1) * 64], tl[:, h, 0:64], fco[:, h:h + 1], None, ALU.mult)
                for b in (1, 2):
                    kb.stt("dve", o[:, h * 64:(h + 1) * 64], tl[:, b * 4 + h, 0:64], fco[:, b * 4 + h:b * 4 + h + 1],
                           o[:, h * 64:(h + 1) * 64], ALU.mult, ALU.add)
            head_tail(kb, bufs, o[:], ngrow[:].re("p h d -> p (h d)"), sz[:, qi, :], mix, q0, 0, 1.0)

        dq = Deferred()
        for qi in range(NT):
            qt = qtiles[qi]
            rq = V(qt, qt.h[0:64].rearrange("d h t -> d (h t)"))
            rqs = V(qt, qt.h.rearrange("d h t -> d (h t)"))
            q0 = qi * 128
            ncv = min(NCC, (8 * qi + 6) // 128 + 1)
            wl = [kc for kc in range(qi - 4, qi + 1) if kc >= 0]
            items = [("c", cc) for cc in range(ncv)] + [("w", kc) for kc in wl] + [("s", kc) for kc in range(qi + 1)]
            sel2 = [None]
            for (kind, kc) in items:
                p = psc.next()
                if kind == "c":
                    kb.mm(p[:], kcmpT[:, kc * 128:(kc + 1) * 128], rq)
                    P = Pc[kc]
                    kb.act(P[:], p[:], AF.Exp, scale=0.125)
                    cm = V(cmask, cmask.h[:, kc, q0:q0 + 128].unsqueeze(1).to_broadcast([128, 4, 128]))
                    kb.tt("pool", P[:].re("p (h t) -> p h t", h=4), P[:].re("p (h t) -> p h t", h=4), cm, ALU.mult)
                elif kind == "w":
                    kb.mm(p[:], kwT[:, kc * 128:(kc + 1) * 128], rq)
                    P = Pr.next()
                    kb.act(P[:], p[:], AF.Exp, scale=0.125)
                    if kc == qi:
                        kb.tt("pool", P[:], P[:], tri4[:], ALU.mult)
                    elif kc == qi - 4:
                        kb.tt("pool", P[:], P[:], atri4[:], ALU.mult)
                else:
                    if kc == 0 and sel2[0] is not None:
                        dq.force(sel2[0])
                    kb.mm(p[:], lsel[:, kc * 128:(kc + 1) * 128], rqs)
                    P = Pr.next()
                    kb.act(P[:], p[:], AF.Exp, scale=0.125)
                    if kc == qi:
                        kb.tt("pool", P[:], P[:], tri4[:], ALU.mult)

                def pv(kind=kind, kc=kc, P=P, qi=qi, ncv=ncv, wl=wl, sel2=sel2):
                    if kind == "c":
                        kb.mm(pacc[0][:], vcmp1[:, kc, :], P[:], start=(kc == 0), stop=(kc == ncv - 1), inc=True)
                        if kc == ncv - 1:
                            select1(qi, ncv)
                            sel2[0] = dq.push(lambda qi=qi: select2(qi), 3)
                    elif kind == "w":
                        kb.mm(pacc[2][:], vw1[:, kc, :], P[:], start=(kc == wl[0]), stop=(kc == qi), inc=True)
                    else:
                        kb.mm(pacc[1][:], vs1[:, kc, :], P[:], start=(kc == 0), stop=(kc == qi), inc=True)
                        if kc == qi:
                            for b in range(3):
                                kb.cp("dve", oTs[qi % 2][:, b, :], pacc[b][:])
                            dq.push(lambda qi=qi: tail(qi), 2)
                dq.step()
                dq.push(pv, 0)
        dq.flush()


def phase_diff(kb, nc, l, S, projF, projT, mix, identf, c_tri4, diff_lam, diff_ng):
    NT = S // 128
    lambda_init = 0.8 - 0.6 * math.exp(-0.3 * l)
    sc = 32 ** -0.5
    with contextlib.ExitStack() as st:
        qT = kb.sb(st, [64, 4, S], BF16, "qT")
        kT = kb.sb(st, [64, 4, S], BF16, "kT")
        v1 = kb.sb(st, [128, NT, 4, 65], BF16, "v1")
        z = kb.sb(st, [128, NT, 256], BF16, "z")
        tri4 = kb.sb(st, [128, 512], BF16, "tri4")
        ngrow = kb.sb(st, [128, 4, 64], F32, "ngrow")
        lam = kb.sb(st, [128, 128], F32, "lam")
        lp = kb.sb(st, [128, 64], F32, "lp")
        ls = kb.sb(st, [128, 2], F32, "ls")
        nlam = kb.sb(st, [128, 1], F32, "nlam")
        s2 = contextlib.ExitStack()
        vtmp = kb.sb(s2, [128, NT, 256], BF16, "vtmp")
        for h in range(4):
            kb.dma(qT[:, h, :], projF[F_ROW["bq%d" % h][0]:F_ROW["bq%d" % h][0] + 64, :], q="sp")
            kb.dma(kT[:, h, :], projF[F_ROW["bk%d" % h][0]:F_ROW["bk%d" % h][0] + 64, :], q="pool")
        load_T(kb, vtmp, projT, "bv", S)
        load_T(kb, z, projT, "bz", S, q="pool")
        kb.dma(tri4[:], c_tri4[:, :])
        kb.dma(ngrow[:, 0, :], bcast_rows(diff_ng, diff_ng.h[l]))
        kb.dma(lam[:], bcast_rows(diff_lam, diff_lam.h[l]))
        for h in range(1, 4):
            kb.cp("pool", ngrow[:, h, :], ngrow[:, 0, :])
        kb.memset("pool", V(v1, v1.h[:, :, :, 64:65]), 1.0)
        kb.cp("pool", V(v1, v1.h[:, :, :, 0:64]), vtmp[:].re("p c (h d) -> p c h d", h=4))
        lv = lam[:].re("p (a b d) -> p a b d", a=2, b=2)
        kb.tt("dve", lp[:].re("p (a d) -> p a d", a=2), lv[:, :, 0, :], lv[:, :, 1, :], ALU.mult)
        kb.red(ls[:], lp[:].re("p (a d) -> p a d", a=2), ALU.add)
        kb.act(ls[:], ls[:], AF.Exp)
        kb.tt("dve", nlam[:], ls[:, 1:2], ls[:, 0:1], ALU.subtract)
        kb.ts("dve", nlam[:], nlam[:], -lambda_init, None, ALU.add)
        sz = silu_all(kb, s2, z, S, "bz")
        s2.close()
        kb.barrier()

        psc = Ring([kb.ps(st, [128, 512], F32, "psc%d" % i) for i in range(2)])
        paccs = [[kb.ps(st, [65, 512], F32, "pacc%d_%d" % (hp, par)) for par in range(2)] for hp in range(2)]
        ptl = Ring([kb.ps(st, [128, 4, 65], F32, "ptl%d" % i) for i in range(2)])
        Pr = Ring([kb.sb(st, [128, 512], BF16, "P%d" % i) for i in range(4)])
        oTs = [kb.sb(st, [65, 2, 512], F32, "oT%d" % i) for i in range(2)]
        tls = [kb.sb(st, [128, 8, 65], F32, "tl%d" % i) for i in range(2)]
        rden = kb.sb(st, [128, 8], F32, "rden")
        o1 = kb.sb(st, [128, 64], F32, "o1")
        o = kb.sb(st, [128, 256], F32, "o")
        bufs = (kb.sb(st, [128, 256], F32, "sq"), kb.sb(st, [128, 4], F32, "ssq"),
                kb.sb(st, [128, 4], F32, "rs"), kb.sb(st, [128, 256], BF16, "yo"))
        qm = Ring([kb.sb(st, [64, 4, 2, 128], BF16, "qm%d" % i) for i in range(2)])
        for t_ in qm.tiles:
            kb.memset("pool", t_[:], 0.0)

        def tail(qi):
            q0 = qi * 128
            oT = oTs[qi % 2]
            tl = tls[qi % 2]
            for half in range(2):
                pt = ptl.next()
                for i in range(4):
                    kb.tr(pt[:, i, :], oT[:, half, i * 128:(i + 1) * 128], identf[0:65, 0:65], inc=(i == 3))
                kb.cp("dve", tl[:, half * 4:(half + 1) * 4, :], pt[:])
            kb.ts("dve", rden[:], tl[:, :, 64], 1e-30, None, ALU.max)
            kb.recip(rden[:], rden[:])
            kb.ts("dve", V(rden, rden.h[:, 1:8:2]), V(rden, rden.h[:, 1:8:2]), nlam[:, 0:1], None, ALU.mult)
            for h in range(4):
                kb.ts("dve", o1[:], tl[:, 2 * h, 0:64], rden[:, 2 * h:2 * h + 1], None, ALU.mult)
                kb.stt("dve", o[:, h * 64:(h + 1) * 64], tl[:, 2 * h + 1, 0:64], rden[:, 2 * h + 1:2 * h + 2],
                       o1[:], ALU.mult, ALU.add)
            head_tail(kb, bufs, o[:], ngrow[:].re("p h d -> p (h d)"), sz[:, qi, :], mix, q0, 256, 1.0 - lambda_init)

        dq = Deferred()
        for qi in range(NT):
            q0 = qi * 128
            qmt = qm.next()
            kb.cp("pool", V(qmt, qmt.h[0:32, :, 0, :]), qT[0:32, :, q0:q0 + 128])
            kb.cp("pool", V(qmt, qmt.h[32:64, :, 1, :]), qT[32:64, :, q0:q0 + 128])
            for hp in range(2):
                for kc in range(qi + 1):
                    p = psc.next()
                    for hh in range(2):
                        h = hp * 2 + hh
                        kb.mm(p[:, hh * 256:(hh + 1) * 256], kT[:, h, kc * 128:(kc + 1) * 128],
                              V(qmt, qmt.h[:, h].rearrange("d c t -> d (c t)")), inc=(hh == 1))
                    P = Pr.next()
                    kb.act(P[:], p[:], AF.Exp, scale=sc)
                    if kc == qi:
                        kb.tt("pool", P[:], P[:], tri4[:], ALU.mult)

                    def pv(qi=qi, hp=hp, kc=kc, P=P):
                        pa = paccs[hp][qi % 2]
                        for hh in range(2):
                            h = hp * 2 + hh
                            kb.mm(pa[:, hh * 256:(hh + 1) * 256], v1[:, kc, h, :], P[:, hh * 256:(hh + 1) * 256],
                                  start=(kc == 0 and hh == 0), stop=(kc == qi and hh == 1), inc=(hh == 1))
                        if kc == qi:
                            kb.cp("dve", oTs[qi % 2][:, hp, :], pa[:])
                            if hp == 1:
                                dq.push(lambda qi=qi: tail(qi), 2)
                    dq.step()
                    dq.push(pv, 0)
        dq.flush()


def conv_silu(kb, eng, dst, src, acc, wcol, bcol, S, rows):
    kb.ts(eng, acc[0:rows, :], src, wcol[:, 3:4], bcol, ALU.mult, ALU.add)
    for k in range(3):
        sh = 3 - k
        kb.stt("dve", acc[0:rows, sh:S], src[:, 0:S - sh], wcol[:, k:k + 1], acc[0:rows, sh:S], ALU.mult, ALU.add)
    kb.act(dst, acc[0:rows, :], AF.Silu)


def phase_ssd(kb, nc, l, S, projF, projT, projTs, mix, identb, identf, ones_f, c_U, c_mb_st,
              ssm_cw, ssm_cb, ssm_dtb, ssm_alog, ssm_d, ssm_ng):
    NT = S // 128
    with contextlib.ExitStack() as st:
        U = kb.sb(st, [128, 128], F32, "U")
        mbst = kb.sb(st, [128, 128], F32, "mbst")
        kb.dma(U[:], c_U[:, :])
        kb.dma(mbst[:], c_mb_st[:, :])
        xT = kb.sb(st, [128, 2, S], BF16, "xT")
        BT = kb.sb(st, [64, 2, S], BF16, "BT")
        CT = kb.sb(st, [64, 2, S], BF16, "CT")
        xB = kb.sb(st, [128, NT, 384], BF16, "xB")
        z = kb.sb(st, [128, NT, 256], BF16, "z")
        dtr = kb.sb(st, [128, NT, 4], F32, "dtr")
        with contextlib.ExitStack() as s2:
            raw = Ring([kb.sb(s2, [128, S], BF16, "raw%d" % i) for i in range(2)])
            acc = Ring([kb.sb(s2, [128, S], F32, "acc%d" % i) for i in range(2)])
            wc = kb.sb(s2, [128, 6, 4], F32, "wc")
            bc_ = kb.sb(s2, [128, 6], F32, "bc")
            specs = [("cx0", 0, 128, xT, 0), ("cx1", 128, 128, xT, 1), ("cB0", 256, 64, BT, 0), ("cB1", 320, 64, BT, 1),
                     ("cC0", 384, 64, CT, 0), ("cC1", 448, 64, CT, 1)]
            for i, (nm, ch0, rows, dst, di) in enumerate(specs):
                kb.dma(wc[0:rows, i, :], V(ssm_cw, ssm_cw.h[l, :, ch0:ch0 + rows].rearrange("k c -> c k")))
                kb.dma(bc_[0:rows, i:i + 1], V(ssm_cb, ssm_cb.h[l, ch0:ch0 + rows].rearrange("(c o) -> c o", o=1)))
            for i, (nm, ch0, rows, dst, di) in enumerate(specs):
                r = raw.next()
                a = acc.next()
                r0 = F_ROW[nm][0]
                kb.dma(r[0:rows, :], projF[r0:r0 + rows, :], q=("sp" if i % 2 == 0 else "pool"))
                conv_silu(kb, ("dve" if i % 2 == 0 else "pool"), dst[0:rows, di, :], r[0:rows, :], a,
                          wc[0:rows, i, :], bc_[0:rows, i:i + 1], S, rows)
            load_T(kb, z, projT, "cz", S)
            sz = silu_all(kb, s2, z, S, "cz")
        kb.barrier()
        load_Ts(kb, dtr, projTs, "dt", S)
        dtb = kb.sb(st, [128, 4], F32, "dtb")
        aneg = kb.sb(st, [128, 4], F32, "aneg")
        dsk = kb.sb(st, [128, 4], F32, "dsk")
        ngrow = kb.sb(st, [128, 256], F32, "ngrow")
        kb.dma(dtb[:], bcast_rows(ssm_dtb, ssm_dtb.h[l]))
        kb.dma(aneg[:], bcast_rows(ssm_alog, ssm_alog.h[l]))
        kb.dma(dsk[:], bcast_rows(ssm_d, ssm_d.h[l]))
        kb.dma(ngrow[:], bcast_rows(ssm_ng, ssm_ng.h[l]))
        kb.act(aneg[:], aneg[:], AF.Exp)
        kb.ts("dve", aneg[:], aneg[:], -1.0, None, ALU.mult)
        dt = kb.sb(st, [128, NT, 4], F32, "dt")
        adt = kb.sb(st, [128, NT, 4], F32, "adt")
        kb.tt("dve", dt[:], dtr[:], V(dtb, dtb.h[:, :].unsqueeze(1).to_broadcast([128, NT, 4])), ALU.add)
        kb.act(dt[:], dt[:], AF.Exp)
        kb.act(dt[:], dt[:], AF.Ln, bias=1.0)
        kb.tt("dve", adt[:], dt[:], V(aneg, aneg.h[:, :].unsqueeze(1).to_broadcast([128, NT, 4])), ALU.mult)
        acs = kb.sb(st, [128, NT, 4], F32, "acs")
        alast = kb.sb(st, [128, NT, 4], F32, "alast")
        ea = kb.sb(st, [128, NT, 4], F32, "ea")
        de = kb.sb(st, [128, NT, 4], F32, "de")
        cd = kb.sb(st, [128, NT, 4], F32, "cd")
        with contextlib.ExitStack() as s2:
            pa = kb.ps(s2, [128, NT * 4], F32, "pa")
            pb = kb.ps(s2, [128, NT * 4], F32, "pb")
            kb.mm(pa[:], U[:], adt[:].re("p c h -> p (c h)"))
            kb.mm(pb[:], ones_f[:], adt[:].re("p c h -> p (c h)"))
            kb.cp("dve", acs[:].re("p c h -> p (c h)"), pa[:])
            kb.cp("dve", alast[:].re("p c h -> p (c h)"), pb[:])
        kb.barrier()
        kb.act(ea[:], acs[:], AF.Exp)
        kb.act(cd[:], alast[:], AF.Exp)
        kb.tt("dve", de[:], alast[:], acs[:], ALU.subtract)
        kb.act(de[:], de[:], AF.Exp)
        with contextlib.ExitStack() as s2:
            ptx = Ring([kb.ps(s2, [128, 384], BF16, "ptx%d" % i) for i in range(2)])
            for c in range(NT):
                pt = ptx.next()
                kb.tr(pt[:, 0:128], xT[:, 0, c * 128:(c + 1) * 128], identb[:], inc=False)
                kb.tr(pt[:, 128:256], xT[:, 1, c * 128:(c + 1) * 128], identb[:], inc=False)
                kb.tr(pt[:, 256:320], BT[:, 0, c * 128:(c + 1) * 128], identb[0:64, 0:64], inc=False)
                kb.tr(pt[:, 320:384], BT[:, 1, c * 128:(c + 1) * 128], identb[0:64, 0:64], inc=True)
                kb.cp(("act" if c % 2 == 0 else "dve"), xB[:, c, :], pt[:])
        kb.barrier()
        pR = kb.ps(st, [128, 4, 128], F32, "pR")
        pS = kb.ps(st, [128, 2, 128], F32, "pS")
        pY = kb.ps(st, [128, 256], F32, "pY")
        pO = kb.ps(st, [128, 256], F32, "pO")
        pN = kb.ps(st, [64, 4, 64], F32, "pN")
        arg = kb.sb(st, [128, 4, 128], F32, "arg")
        dec = kb.sb(st, [128, 4, 128], F32, "dec")
        GT = kb.sb(st, [128, 4, 128], BF16, "GT")
        xdt = kb.sb(st, [128, 4, 64], BF16, "xdt")
        xdw = kb.sb(st, [128, 4, 64], BF16, "xdw")
        stf = kb.sb(st, [64, 4, 64], F32, "stf")
        stb = kb.sb(st, [64, 4, 64], BF16, "stb")
        yd = kb.sb(st, [128, 256], F32, "yd")
        y = kb.sb(st, [128, 256], F32, "y")
        kb.memset("pool", stf[:], 0.0)
        kb.memset("pool", stb[:], 0.0)
        bufs = (kb.sb(st, [128, 256], F32, "sq"), kb.sb(st, [128, 4], F32, "ssq"),
                kb.sb(st, [128, 4], F32, "rs"), kb.sb(st, [128, 256], BF16, "yo"))
        for c in range(NT):
            t0 = c * 128
            xc = V(xB, xB.h[:, c, 0:256].rearrange("p (h d) -> p h d", h=4))
            for h in range(4):
                kb.mm(pR[:, h, :], adt[:, c, h:h + 1].bc([128, 128]), U[:], inc=(h == 3))
            for h in range(4):
                kb.stt("dve", arg[:, h, :], pR[:, h, :], acs[:, c, h:h + 1], mbst[:], ALU.subtract, ALU.add)
            kb.act(dec[:], arg[:], AF.Exp)
            for g in range(2):
                kb.mm(pS[:, g, :], BT[:, g, t0:t0 + 128], CT[:, g, t0:t0 + 128], inc=(g == 1))
            for h in range(4):
                kb.tt("dve", GT[:, h, :], pS[:, h // 2, :], dec[:, h, :], ALU.mult)
            kb.tt("pool", xdt[:], xc, V(dt, dt.h[:, c, :].unsqueeze(2).to_broadcast([128, 4, 64])), ALU.mult)
            kb.tt("pool", xdw[:], xdt[:], V(de, de.h[:, c, :].unsqueeze(2).to_broadcast([128, 4, 64])), ALU.mult)
            for h in range(4):
                kb.mm(pY[:, h * 64:(h + 1) * 64], GT[:, h, :], xdt[:, h, :], inc=(h == 3))
            for h in range(4):
                kb.mm(pO[:, h * 64:(h + 1) * 64], CT[:, h // 2, t0:t0 + 128], stb[:, h, :], inc=(h == 3))
            kb.cp("act", yd[:], pY[:])
            for h in range(4):
                hs = slice(h * 64, (h + 1) * 64)
                kb.stt("dve", y[:, hs], pO[:, hs], ea[:, c, h:h + 1], yd[:, hs], ALU.mult, ALU.add)
                kb.stt("dve", y[:, hs], xc[:, h, :], dsk[:, h:h + 1], y[:, hs], ALU.mult, ALU.add)
            for h in range(4):
                kb.mm(pN[:, h, :], xB[:, c, 256 + 64 * (h // 2):256 + 64 * (h // 2) + 64], xdw[:, h, :], inc=(h == 3))
            for h in range(4):
                kb.stt("dve", stf[:, h, :], stf[:, h, :], cd[0:64, c, h:h + 1], pN[:, h, :], ALU.mult, ALU.add)
            kb.cp("act", stb[:], stf[:])
            kb.tt("pool", y[:], y[:], sz[:, c, :], ALU.mult)
            head_tail(kb, bufs, y[:], ngrow[:], ones_f[:, 0:1].bc([128, 256]), mix, t0, 512, 1.0, nheads=2, hd=128)


def phase_mlstm(kb, nc, l, S, projF, projT, projTs, mix, identb, identf, ones_f, c_U, c_mb_st, c_mb_ts,
                c_sel127, ml_cw, ml_cb, ml_ifb, ml_ng):
    NT = S // 128
    with contextlib.ExitStack() as st:
        U = kb.sb(st, [128, 128], F32, "U")
        mbst = kb.sb(st, [128, 4, 128], F32, "mbst")
        mbts = kb.sb(st, [128, 4, 128], F32, "mbts")
        sel127 = kb.sb(st, [128, 128], F32, "sel127")
        kb.dma(U[:], c_U[:, :])
        kb.dma(sel127[:], c_sel127[:, :])
        for h in range(4):
            kb.dma(mbst[:, h, :], c_mb_st[:, :])
            kb.dma(mbts[:, h, :], c_mb_ts[:, :], q="pool")
        qT = kb.sb(st, [64, 4, S], BF16, "qT")
        kT = kb.sb(st, [64, 4, S], BF16, "kT")
        kTl = kb.sb(st, [128, NT, 4, 64], BF16, "kTl")
        v1 = kb.sb(st, [128, NT, 4, 65], BF16, "v1")
        z = kb.sb(st, [128, NT, 256], BF16, "z")
        og = kb.sb(st, [128, NT, 256], BF16, "og")
        ifr = kb.sb(st, [128, NT, 8], F32, "ifr")
        with contextlib.ExitStack() as s2:
            raw = Ring([kb.sb(s2, [64, S], BF16, "raw%d" % i) for i in range(2)])
            acc = Ring([kb.sb(s2, [64, S], F32, "acc%d" % i) for i in range(2)])
            wc = kb.sb(s2, [64, 8, 4], F32, "wc")
            bc_ = kb.sb(s2, [64, 8], F32, "bc")
            for i in range(8):
                ch0 = i * 64
                kb.dma(wc[:, i, :], V(ml_cw, ml_cw.h[l, :, ch0:ch0 + 64].rearrange("k c -> c k")))
                kb.dma(bc_[:, i:i + 1], V(ml_cb, ml_cb.h[l, ch0:ch0 + 64].rearrange("(c o) -> c o", o=1)))
            for i in range(8):
                nm = ("dq%d" % i) if i < 4 else ("dk%d" % (i - 4))
                dst = qT if i < 4 else kT
                r = raw.next()
                a = acc.next()
                r0 = F_ROW[nm][0]
                kb.dma(r[:], projF[r0:r0 + 64, :], q=("sp" if i % 2 == 0 else "pool"))
                conv_silu(kb, ("dve" if i % 2 == 0 else "pool"), dst[:, i % 4, :], r[:], a, wc[:, i, :], bc_[:, i:i + 1], S, 64)
        kb.barrier()
        with contextlib.ExitStack() as s2:
            vtmp = kb.sb(s2, [128, NT, 256], BF16, "vtmp")
            load_T(kb, vtmp, projT, "dv", S)
            load_T(kb, z, projT, "dz", S, q="pool")
            load_T(kb, og, projT, "do", S)
            kb.memset("pool", V(v1, v1.h[:, :, :, 64:65]), 1.0)
            kb.cp("pool", V(v1, v1.h[:, :, :, 0:64]), vtmp[:].re("p c (h d) -> p c h d", h=4))
            sz = silu_all(kb, s2, z, S, "dz")
            kb.act(og[:], og[:], AF.Sigmoid)
        kb.barrier()
        load_Ts(kb, ifr, projTs, "dif", S)
        ifb = kb.sb(st, [128, 8], F32, "ifb")
        ngrow = kb.sb(st, [128, 4, 64], F32, "ngrow")
        kb.dma(ifb[:], bcast_rows(ml_ifb, ml_ifb.h[l]))
        kb.dma(ngrow[:, 0, :], bcast_rows(ml_ng, ml_ng.h[l]))
        for h in range(1, 4):
            kb.cp("pool", ngrow[:, h, :], ngrow[:, 0, :])
        kb.tt("dve", ifr[:], ifr[:], V(ifb, ifb.h[:, :].unsqueeze(1).to_broadcast([128, NT, 8])), ALU.add)
        ig = V(ifr, ifr.h[:, :, 0:4])
        lf = kb.sb(st, [128, NT, 4], F32, "lf")
        kb.act(lf[:], V(ifr, ifr.h[:, :, 4:8]), AF.Exp, scale=-1.0)
        kb.act(lf[:], lf[:], AF.Ln, bias=1.0)
        kb.ts("dve", lf[:], lf[:], -1.0, None, ALU.mult)
        b = kb.sb(st, [128, NT, 4], F32, "b")
        blast = kb.sb(st, [128, NT, 4], F32, "blast")
        u = kb.sb(st, [128, NT, 4], F32, "u")
        with contextlib.ExitStack() as s2:
            pa = kb.ps(s2, [128, NT * 4], F32, "pa")
            pb = kb.ps(s2, [128, NT * 4], F32, "pb")
            kb.mm(pa[:], U[:], lf[:].re("p c h -> p (c h)"))
            kb.mm(pb[:], ones_f[:], lf[:].re("p c h -> p (c h)"))
            kb.cp("dve", b[:].re("p c h -> p (c h)"), pa[:])
            kb.cp("dve", blast[:].re("p c h -> p (c h)"), pb[:])
        kb.barrier()
        kb.tt("dve", u[:], ig, b[:], ALU.subtract)
        with contextlib.ExitStack() as s2:
            ptk = Ring([kb.ps(s2, [128, 4, 64], BF16, "ptk%d" % i) for i in range(2)])
            for c in range(NT):
                pt = ptk.next()
                for h in range(4):
                    kb.tr(pt[:, h, :], kT[:, h, c * 128:(c + 1) * 128], identb[0:64, 0:64], inc=(h == 3))
                kb.cp(("act" if c % 2 == 0 else "dve"), kTl[:, c, :, :], pt[:])
        kb.barrier()
        pM = kb.ps(st, [128, 4, 128], F32, "pM")
        pW = kb.ps(st, [128, 4, 128], F32, "pW")
        pSC = kb.ps(st, [128, 4, 128], F32, "pSC")
        pND = kb.ps(st, [128, 4, 65], F32, "pND")
        pIN = kb.ps(st, [128, 4, 65], F32, "pIN")
        pL = kb.ps(st, [64, 4, 65], F32, "pL")
        pU = kb.ps(st, [128, 4], F32, "pU")
        cmx = kb.sb(st, [128, 4], F32, "cmx")
        umax = kb.sb(st, [128, 4], F32, "umax")
        mprev = kb.sb(st, [128, 4], F32, "mprev")
        tmp = kb.sb(st, [128, 4], F32, "tmp")
        ntmp = kb.sb(st, [128, 4], F32, "ntmp")
        mt = kb.sb(st, [128, 4], F32, "mt")
        emt = kb.sb(st, [128, 4], F32, "emt")
        wint = kb.sb(st, [128, 4], F32, "wint")
        wend = kb.sb(st, [128, 4], F32, "wend")
        mm_ = kb.sb(st, [128, 4], F32, "mm")
        aprev = kb.sb(st, [128, 4], F32, "aprev")
        aloc = kb.sb(st, [128, 4], F32, "aloc")
        wT = kb.sb(st, [128, 4, 128], F32, "wT")
        sqk = kb.sb(st, [128, 4, 128], BF16, "sqk")
        nds = kb.sb(st, [128, 4, 65], F32, "nds")
        nd = kb.sb(st, [128, 4, 65], F32, "nd")
        dn = kb.sb(st, [128, 4], F32, "dn")
        hh_ = kb.sb(st, [128, 256], F32, "hh")
        kw = kb.sb(st, [128, 4, 64], BF16, "kw")
        cnf = kb.sb(st, [64, 4, 65], F32, "cnf")
        cnb = kb.sb(st, [64, 4, 65], BF16, "cnb")
        ltmp = kb.sb(st, [64, 4, 65], F32, "ltmp")
        kb.memset("pool", cnf[:], 0.0)
        kb.memset("pool", cnb[:], 0.0)
        kb.memset("pool", mprev[:], 0.0)
        bufs = (kb.sb(st, [128, 256], F32, "sq"), kb.sb(st, [128, 4], F32, "ssq"),
                kb.sb(st, [128, 4], F32, "rs"), kb.sb(st, [128, 256], BF16, "yo"))
        for c in range(NT):
            t0 = c * 128
            for h in range(4):
                kb.mm(pM[:, h, :], u[:, c, h:h + 1].bc([128, 128]), identf[:], start=(h == 0), stop=False, inc=False)
            kb.mm(pM[:].re("p h s -> p (h s)"), identf[:], mbts[:].re("p h s -> p (h s)"), start=False, stop=True)
            kb.red(cmx[:], pM[:], ALU.max)
            kb.mm(pU[:], sel127[:], cmx[:])
            kb.cp("dve", umax[:], pU[:])
            kb.tt("dve", tmp[:], cmx[:], mprev[:], ALU.max)
            kb.ts("dve", ntmp[:], tmp[:], -1.0, None, ALU.mult)
            kb.tt("dve", mt[:], tmp[:], b[:, c, :], ALU.add)
            kb.act(emt[:], mt[:], AF.Exp, scale=-1.0)
            kb.tt("dve", wint[:], mprev[:], tmp[:], ALU.subtract)
            kb.act(wint[:], wint[:], AF.Exp)
            for h in range(4):
                kb.mm(pW[:, h, :], ntmp[:, h:h + 1].bc([128, 128]), identf[:], start=(h == 0), stop=False, inc=False)
            kb.mm(pW[:].re("p h s -> p (h s)"), identf[:], mbst[:].re("p h s -> p (h s)"), start=False, stop=True)
            for h in range(4):
                kb.act(wT[:, h, :], pW[:, h, :], AF.Exp, bias=u[:, c, h:h + 1])
            for h in range(4):
                kb.mm(pSC[:, h, :], kT[:, h, t0:t0 + 128], qT[:, h, t0:t0 + 128], inc=(h == 3))
            kb.stt("dve", sqk[:], pSC[:], 0.125, wT[:], ALU.mult, ALU.mult)
            for h in range(4):
                kb.mm(pND[:, h, :], sqk[:, h, :], v1[:, c, h, :], inc=(h == 3))
            for h in range(4):
                kb.mm(pIN[:, h, :], qT[:, h, t0:t0 + 128], cnb[:, h, :], inc=(h == 3))
            kb.cp("act", nds[:], pND[:])
            for h in range(4):
                kb.stt("dve", nd[:, h, :], pIN[:, h, :], wint[:, h:h + 1], nds[:, h, :], ALU.mult, ALU.add)
            kb.ts("dve", dn[:], nd[:, :, 64], -1.0, None, ALU.mult)
            kb.tt("dve", dn[:], dn[:], nd[:, :, 64], ALU.max)
            kb.tt("dve", dn[:], dn[:], emt[:], ALU.max)
            kb.recip(dn[:], dn[:])
            for h in range(4):
                kb.ts("dve", hh_[:, h * 64:(h + 1) * 64], nd[:, h, 0:64], dn[:, h:h + 1], None, ALU.mult)
            kb.tt("pool", hh_[:], hh_[:], og[:, c, :], ALU.mult)
            kb.tt("dve", wend[:], u[:, c, :], umax[:], ALU.subtract)
            kb.act(wend[:], wend[:], AF.Exp)
            kb.ts("dve", wend[:], wend[:], 0.125, None, ALU.mult)
            kb.tt("dve", mm_[:], mprev[:], umax[:], ALU.max)
            kb.tt("dve", aprev[:], mprev[:], mm_[:], ALU.subtract)
            kb.act(aprev[:], aprev[:], AF.Exp)
            kb.tt("dve", aloc[:], umax[:], mm_[:], ALU.subtract)
            kb.act(aloc[:], aloc[:], AF.Exp)
            kb.tt("pool", kw[:], kTl[:, c, :, :], V(wend, wend.h[:, :].unsqueeze(2).to_broadcast([128, 4, 64])), ALU.mult)
            for h in range(4):
                kb.mm(pL[:, h, :], kw[:, h, :], v1[:, c, h, :], inc=(h == 3))
            for h in range(4):
                kb.ts("dve", ltmp[:, h, :], pL[:, h, :], aloc[0:64, h:h + 1], None, ALU.mult)
                kb.stt("dve", cnf[:, h, :], cnf[:, h, :], aprev[0:64, h:h + 1], ltmp[:, h, :], ALU.mult, ALU.add)
            kb.cp("act", cnb[:], cnf[:])
            kb.tt("dve", mprev[:], mm_[:], blast[:, c, :], ALU.add)
            head_tail(kb, bufs, hh_[:], ngrow[:].re("p h d -> p (h d)"), sz[:, c, :], mix, t0, 768, 1.0)


def phase_out(kb, nc, l, S, x_src, x_dst, mix, w_out, gate_row, fin_row, identb, last):
    NT = S // 128
    with contextlib.ExitStack() as st:
        wo = kb.sb(st, [128, 8, D], BF16, "wo")
        stg = Ring([kb.sb(st, [128, D], F32, "wstg%d" % i) for i in range(2)])
        for k in range(8):
            sg = stg.next()
            kb.dma(sg[:], w_out[l, k * 128:(k + 1) * 128, :], q=("sp" if k % 2 == 0 else "pool"))
            kb.cp(("dve" if k % 2 == 0 else "act"), wo[:, k, :], sg[:])
        mixr = Ring([kb.sb(st, [128, D], BF16, "mixt%d" % i) for i in range(2)])
        xr = Ring([kb.sb(st, [128, D], F32, "xt%d" % i) for i in range(2)])
        mT = Ring([kb.sb(st, [128, 8, 128], BF16, "mT%d" % i) for i in range(2)])
        yr = Ring([kb.sb(st, [128, D], F32, "y%d" % i) for i in range(2)])
        junk = kb.sb(st, [128, D], BF16, "junk")
        ss = kb.sb(st, [128, 1], F32, "ss")
        ptr = Ring([kb.ps(st, [128, 8, 128], BF16, "ptr%d" % i) for i in range(2)])
        py = Ring([kb.ps(st, [128, 512], F32, "py%d" % i) for i in range(4)])
        for t in range(NT):
            t0 = t * 128
            mt_ = mixr.next()
            xt = xr.next()
            kb.dma(mt_[:], mix[t0:t0 + 128, :])
            kb.dma(xt[:], x_src[t0:t0 + 128, :], q="pool")
            pt = ptr.next()
            for k in range(8):
                kb.tr(pt[:, k, :], mt_[:, k * 128:(k + 1) * 128], identb[:], inc=(k == 7))
            m = mT.next()
            kb.cp("act", m[:], pt[:])
            y = yr.next()
            for half in range(2):
                p = py.next()
                for k in range(8):
                    kb.mm(p[:], m[:, k, :], wo[:, k, half * 512:(half + 1) * 512], start=(k == 0), stop=(k == 7), inc=(k == 7))
                hs = slice(half * 512, (half + 1) * 512)
                kb.tt("dve", y[:, hs], p[:], gate_row[:, l, hs], ALU.mult)
                kb.tt("pool", y[:, hs], y[:, hs], xt[:, hs], ALU.add)
            if last:
                kb.act(junk[:], y[:], AF.Square, accum=ss[:])
                kb.rsqrt(ss[:], ss[:], 1.0 / D)
                kb.stt("dve", y[:], y[:], ss[:, 0:1], fin_row[:], ALU.mult, ALU.mult)
            kb.dma(x_dst[t0:t0 + 128, :], y[:], q="sp", disjoint=True)


def make_consts(S):
    NCMP = (S - 32) // 16 + 1
    NCC = (NCMP + 127) // 128
    bf = ml_dtypes.bfloat16
    k = np.arange(128)
    tri = (k[:, None] <= k[None, :]).astype(np.float32)
    c = {}
    c["c_identb"] = np.eye(128, dtype=np.float32).astype(bf)
    c["c_identf"] = np.eye(128, dtype=np.float32)
    c["c_tri4"] = np.tile(tri, (1, 4)).astype(bf)
    c["c_atri4"] = np.tile(1.0 - tri, (1, 4)).astype(bf)
    c["c_U"] = tri.copy()
    c["c_mb_st"] = ((1.0 - tri) * NEGB).astype(np.float32)
    c["c_mb_ts"] = np.ascontiguousarray(c["c_mb_st"].T)
    s127 = np.zeros((128, 128), np.float32)
    s127[127, :] = 1.0
    c["c_sel127"] = s127
    E = np.zeros((64, S), np.float32)
    keys = np.arange(S)
    E[keys // 64 % 64, keys] = 30000.0 * (keys // 64 < 64)
    c["c_E"] = E.astype(bf)
    n_sel = S // 64
    cmp_idx = np.arange(NCMP)[:, None] * 16 + np.arange(32)[None, :]
    sel_start = np.arange(n_sel) * 64
    overlap = np.clip(np.minimum(cmp_idx[:, -1:] + 1, sel_start[None, :] + 64)
                      - np.maximum(cmp_idx[:, :1], sel_start[None, :]), 0, None)
    c2s = np.zeros((NCC * 128, 65), np.float32)
    c2s[:NCMP, :n_sel] = overlap / 32.0
    c2s[:NCMP, 64] = 1.0
    c["c_c2s"] = c2s.astype(bf)
    cm = np.zeros((NCC * 128, S), np.float32)
    cm[:NCMP] = (cmp_idx[:, -1][:, None] <= keys[None, :]).astype(np.float32)
    c["c_cmask"] = cm.astype(bf)
    t = np.arange(S)
    cur = t // 64
    sid = np.arange(64)
    forced = (sid[None, :] == cur[:, None]) | (sid[None, :] == 0)
    allowed = (sid[None, :] <= cur[:, None]) & (sid[None, :] < n_sel)
    c["c_selmul"] = (allowed & ~forced).astype(np.float32)
    c["c_seladd"] = np.where(forced, 1e4, np.where(allowed, 0.0, -1.0)).astype(np.float32)
    return c


_NC_CACHE = {}


def run(inputs, S, DEPTH, ncores, debug=False, stop=99):
    key = (S, DEPTH, debug, stop)
    if key not in _NC_CACHE:
        _NC_CACHE[key] = build(S, DEPTH, debug, stop)
    nc = _NC_CACHE[key]
    consts = make_consts(S)
    f32 = np.float32
    w_in = np.asarray(inputs["w_in"], f32)
    shared = dict(consts)
    shared["w_inF"] = np.ascontiguousarray(w_in[:, :, F_COLS])
    shared["w_inT"] = np.ascontiguousarray(w_in[:, :, T_COLS])
    for nm in ("norm_g", "ada_w", "ada_b", "w_out", "nsa_cmp_pos", "nsa_ck_w1", "nsa_ck_w2", "nsa_cv_w1",
               "nsa_cv_w2", "nsa_norm_g", "diff_norm_g", "ssm_conv_w", "ssm_conv_b", "ssm_dt_bias",
               "ssm_a_log", "ssm_d", "ssm_norm_g", "ml_conv_w", "ml_conv_b", "ml_if_b", "ml_norm_g", "final_g"):
        shared[nm] = np.ascontiguousarray(np.asarray(inputs[nm], f32))
    shared["diff_lam"] = np.ascontiguousarray(np.asarray(inputs["diff_lam"], f32).reshape(DEPTH, 128))
    x = np.asarray(inputs["x"], f32)
    c = np.asarray(inputs["c"], f32)
    in_maps = []
    for i in range(ncores):
        m = dict(shared)
        m["x"] = np.ascontiguousarray(x[i])
        m["c"] = np.ascontiguousarray(c[i])
        in_maps.append(m)
    res = run_bass_kernel_spmd(nc, in_maps, core_ids=list(range(ncores)))
    return res


def kernel(**inputs):
    res = run(inputs, 4096, 2, 8)
    return np.stack([np.asarray(r["out"], np.float32) for r in res.results], axis=0)
```

```python
import contextlib
import math
import numpy as np
import ml_dtypes
import concourse.bass as bass
import concourse.mybir as mybir
from concourse.bass_utils import run_bass_kernel_spmd

F32 = mybir.dt.float32
BF16 = mybir.dt.bfloat16
AF = mybir.ActivationFunctionType
ALU = mybir.AluOpType
AX = mybir.AxisListType

D = 1024
NEGB = -30000.0
EPS = 1e-6

F_GROUPS = []
for h in range(4):
    F_GROUPS.append(("aq%d" % h, 0 + 64 * h, 64))
F_GROUPS += [("akc", 256, 64), ("avc", 320, 64), ("aks", 384, 64), ("akw", 512, 64)]
for h in range(4):
    F_GROUPS.append(("bq%d" % h, 908 + 64 * h, 64))
for h in range(4):
    F_GROUPS.append(("bk%d" % h, 1164 + 64 * h, 64))
F_GROUPS += [("cx0", 2188, 128), ("cx1", 2316, 128), ("cB0", 2444, 64), ("cB1", 2508, 64),
             ("cC0", 2572, 64), ("cC1", 2636, 64)]
for h in range(4):
    F_GROUPS.append(("dq%d" % h, 2704 + 64 * h, 64))
for h in range(4):
    F_GROUPS.append(("dk%d" % h, 2960 + 64 * h, 64))
F_ROW = {}
_r = 0
F_COLS = []
for (n_, c0_, w_) in F_GROUPS:
    F_ROW[n_] = (_r, w_)
    F_COLS += list(range(c0_, c0_ + w_))
    _r += w_
NF = _r
T_PARTS = [("vs", 448, 64), ("vw", 576, 64), ("ag", 640, 12), ("az", 652, 256),
           ("bv", 1420, 256), ("bz", 1676, 256), ("cz", 1932, 256), ("dt", 2700, 4),
           ("dv", 3216, 256), ("dif", 3472, 8), ("do", 3480, 256), ("dz", 3736, 256)]
T_COL = {}
_r = 0
T_COLS = []
for (n_, c0_, w_) in T_PARTS:
    T_COL[n_] = (_r, w_)
    T_COLS += list(range(c0_, c0_ + w_))
    _r += w_
NTC = _r
TS_COL = {"ag": (0, 12), "dt": (12, 4), "dif": (16, 8)}


class TT:
    def __init__(self, h, name=""):
        self.h = h
        self.w = {}
        self.r = {}
        self.name = name

    def __getitem__(self, key):
        return V(self, self.h[key])


class V:
    def __init__(self, t, ap):
        self.t = t
        self.ap = ap

    def __getitem__(self, key):
        return V(self.t, self.ap[key])

    def re(self, pat, **kw):
        return V(self.t, self.ap.rearrange(pat, **kw))

    def bc(self, shape):
        return V(self.t, self.ap.to_broadcast(list(shape)))

    def raw(self, fn):
        return V(self.t, fn(self.ap))


class Eng:
    def __init__(self, name, h, sem, key, is_pe=False):
        self.name = name
        self.h = h
        self.sem = sem
        self.key = key
        self.count = 0
        self.seen = {}
        self.is_pe = is_pe
        self.pend = False
        self.dsems = []
        self.dlast = []
        self.dnext = 0


class KB:
    def __init__(self, nc, es):
        self.nc = nc
        self.es = es
        self.sems = {}
        self.eng = {}
        for name, h, pe in (("pe", nc.tensor, True), ("act", nc.scalar, False),
                            ("dve", nc.vector, False), ("pool", nc.gpsimd, False),
                            ("sp", nc.sync, False)):
            s = es.enter_context(nc.semaphore("sem_" + name))
            self.sems[name] = s
            self.eng[name] = Eng(name, h, s, name, pe)
        for q, n in (("sp", 28), ("pool", 10), ("act", 6)):
            E = self.eng[q]
            for i in range(n):
                key = "d_%s_%d" % (q, i)
                s = es.enter_context(nc.semaphore(key))
                self.sems[key] = s
                E.dsems.append(key)
                E.dlast.append(0)
        self.bar = es.enter_context(nc.semaphore("barrier"))
        self.sems["bar"] = self.bar
        self.barcount = 0
        self.uid = 0

    def sb(self, st, shape, dt, name):
        self.uid += 1
        h = st.enter_context(self.nc.sbuf_tensor("%s_%d" % (name, self.uid), list(shape), dt))
        return TT(h, name)

    def ps(self, st, shape, dt, name):
        self.uid += 1
        esz = 4 if dt == F32 else 2
        n = 1
        for d_ in shape[1:]:
            n *= d_
        per_bank = 2048 // esz
        full = ((n + per_bank - 1) // per_bank) * per_bank
        h = st.enter_context(self.nc.psum_tensor("%s_%d" % (name, self.uid), [128, full], dt))
        ap = h[0:shape[0], 0:n]
        if len(shape) == 3:
            ap = ap.rearrange("p (a b) -> p a b", a=shape[1])
        t = TT(None, name)
        t.h = ap
        return t

    def sub(self, v):
        t = TT(None, v.t.name + "_sub")
        t.h = v.ap
        return t

    def _waits(self, E, outs, ins, disjoint=False):
        need = {}

        def add(d, own_ok):
            for k, val in d.items():
                if k == E.key and not own_ok:
                    continue
                if need.get(k, 0) < val:
                    need[k] = val
        for v in ins:
            add(v.t.w, True)
        for v in outs:
            if not disjoint:
                add(v.t.w, True)
                add(v.t.r, True)
        for k, val in need.items():
            if E.is_pe and k == E.key:
                continue
            if E.seen.get(k, 0) >= val:
                continue
            E.h.wait_ge(self.sems[k], val)
            E.seen[k] = val

    def _record(self, ev, outs, ins, disjoint=False):
        k, val = ev
        for v in ins:
            if v.t.r.get(k, 0) < val:
                v.t.r[k] = val
        for v in outs:
            if disjoint:
                v.t.w[k] = max(v.t.w.get(k, 0), val)
            else:
                v.t.w = {k: val}
                v.t.r = {}

    def op(self, eng, fn, outs, ins, inc=True, disjoint=False):
        E = self.eng[eng]
        self._waits(E, outs, ins, disjoint)
        inst = fn(E.h)
        if inc:
            E.count += 1
            inst.then_inc(E.sem, 1)
            ev = (E.key, E.count)
            E.pend = False
        else:
            ev = (E.key, E.count + 1)
            E.pend = True
        self._record(ev, outs, ins, disjoint)

    def dma(self, out, in_, q="sp", disjoint=False):
        E = self.eng[q]
        i = E.dnext % len(E.dsems)
        E.dnext += 1
        key = E.dsems[i]
        if E.dlast[i] and E.seen.get(key, 0) < E.dlast[i]:
            E.h.wait_ge(self.sems[key], E.dlast[i])
            E.seen[key] = E.dlast[i]
        self._waits(E, [out], [in_], disjoint)
        E.h.dma_start(out=out.ap, in_=in_.ap).then_inc(self.sems[key], 16)
        E.dlast[i] += 16
        self._record((key, E.dlast[i]), [out], [in_], disjoint)

    def barrier(self):
        sp = self.eng["sp"]
        assert not self.eng["pe"].pend
        for q in ("sp", "pool", "act"):
            E = self.eng[q]
            for i, key in enumerate(E.dsems):
                if E.dlast[i] and sp.seen.get(key, 0) < E.dlast[i]:
                    sp.h.wait_ge(self.sems[key], E.dlast[i])
                    sp.seen[key] = E.dlast[i]
        for n in ("pe", "act", "dve", "pool"):
            E = self.eng[n]
            if E.count and sp.seen.get(n, 0) < E.count:
                sp.h.wait_ge(E.sem, E.count)
                sp.seen[n] = E.count
        self.barcount += 1
        sp.h.sem_inc(self.bar, 1)
        for n in ("pe", "act", "dve", "pool"):
            E = self.eng[n]
            E.h.wait_ge(self.bar, self.barcount)
        for n, E in self.eng.items():
            for m, E2 in self.eng.items():
                if m != "sp":
                    E.seen[m] = E2.count
            for q in ("sp", "pool", "act"):
                Eq = self.eng[q]
                for i, key in enumerate(Eq.dsems):
                    E.seen[key] = Eq.dlast[i]

    def mm(self, out, lhsT, rhs, start=True, stop=True, inc=True):
        self.op("pe", lambda e: e.matmul(out.ap, lhsT.ap, rhs.ap, start=start, stop=stop),
                [out], [lhsT, rhs], inc=inc)

    def tr(self, out, in_, ident, inc=True):
        self.op("pe", lambda e: e.transpose(out.ap, in_.ap, ident.ap), [out], [in_, ident], inc=inc)

    def act(self, out, in_, func, bias=None, scale=1.0, accum=None):
        ins = [in_]
        outs = [out]
        kw = {}
        if isinstance(bias, V):
            ins.append(bias)
            kw["bias"] = bias.ap
        elif bias is not None:
            kw["bias"] = bias
        if isinstance(scale, V):
            ins.append(scale)
            kw["scale"] = scale.ap
        else:
            kw["scale"] = scale
        if accum is not None:
            outs.append(accum)
            kw["accum_out"] = accum.ap
        self.op("act", lambda e: e.activation(out.ap, in_.ap, func, **kw), outs, ins)

    def ts(self, eng, out, in0, s1, s2, op0, op1=None):
        ins = [in0]
        a1 = s1
        a2 = s2
        if isinstance(s1, V):
            ins.append(s1)
            a1 = s1.ap
        if isinstance(s2, V):
            ins.append(s2)
            a2 = s2.ap
        if op1 is None:
            self.op(eng, lambda e: e.tensor_scalar(out.ap, in0.ap, a1, None, op0), [out], ins)
        else:
            self.op(eng, lambda e: e.tensor_scalar(out.ap, in0.ap, a1, a2, op0, op1), [out], ins)

    def tt(self, eng, out, in0, in1, op):
        self.op(eng, lambda e: e.tensor_tensor(out.ap, in0.ap, in1.ap, op), [out], [in0, in1])

    def stt(self, eng, out, in0, s, in1, op0, op1):
        ins = [in0, in1]
        a = s
        if isinstance(s, V):
            ins.append(s)
            a = s.ap
        self.op(eng, lambda e: e.scalar_tensor_tensor(out.ap, in0.ap, a, in1.ap, op0, op1), [out], ins)

    def cp(self, eng, out, in_):
        if eng == "act":
            self.op("act", lambda e: e.copy(out.ap, in_.ap), [out], [in_])
        else:
            self.op(eng, lambda e: e.tensor_copy(out.ap, in_.ap), [out], [in_])

    def memset(self, eng, out, val):
        self.op(eng, lambda e: e.memset(out.ap, val), [out], [])

    def red(self, out, in_, op):
        self.op("dve", lambda e: e.tensor_reduce(out.ap, in_.ap, AX.X, op), [out], [in_])

    def rsqrt(self, out, in_, scale, post=1.0):
        n = out.ap.shape[0]
        self.act(out, in_, AF.Ln, bias=self.eps_col[0:n, 0:1], scale=scale)
        self.act(out, out, AF.Exp, scale=-0.5)
        if post != 1.0:
            self.ts("dve", out, out, float(post), None, ALU.mult)

    def recip(self, out, in_):
        self.op("dve", lambda e: e.reciprocal(out.ap, in_.ap), [out], [in_])


class Deferred:
    def __init__(self):
        self.q = []

    def push(self, fn, delay):
        e = [delay, fn]
        self.q.append(e)
        return e

    def force(self, e):
        for i, x in enumerate(self.q):
            if x is e:
                del self.q[i]
                e[1]()
                return

    def step(self):
        ready = [e for e in self.q if e[0] <= 0]
        self.q = [e for e in self.q if e[0] > 0]
        for e in self.q:
            e[0] -= 1
        for e in ready:
            e[1]()

    def run_through(self, e):
        while any(x is e for x in self.q):
            x = self.q.pop(0)
            x[1]()

    def flush(self):
        while self.q:
            self.step()


def interleave(gens):
    gens = list(gens)
    while gens:
        for g in list(gens):
            try:
                next(g)
            except StopIteration:
                gens.remove(g)


class Ring:
    def __init__(self, tiles):
        self.tiles = tiles
        self.i = 0

    def next(self):
        t = self.tiles[self.i % len(self.tiles)]
        self.i += 1
        return t


def build(S, DEPTH, debug=False, stop=99):
    NT = S // 128
    NG = S // 512
    NCMP = (S - 32) // 16 + 1
    NCC = (NCMP + 127) // 128
    nc = bass.Bass("TRN2", target_bir_lowering=False)

    def din(name, shape, dt=F32):
        return TT(nc.dram_tensor(name, list(shape), dt, kind="ExternalInput").ap(), name)

    def dscr(name, shape, dt):
        return TT(nc.dram_tensor(name, list(shape), dt, kind="Internal").ap(), name)

    x_in = din("x", [S, D])
    c_in = din("c", [D])
    norm_g = din("norm_g", [DEPTH, D])
    ada_w = din("ada_w", [DEPTH, D, 3 * D])
    ada_b = din("ada_b", [DEPTH, 3 * D])
    w_inF = din("w_inF", [DEPTH, D, NF])
    w_inT = din("w_inT", [DEPTH, D, NTC])
    w_out = din("w_out", [DEPTH, D, D])
    cmp_pos = din("nsa_cmp_pos", [DEPTH, 32, 64])
    ck_w1 = din("nsa_ck_w1", [DEPTH, 2048, 128])
    ck_w2 = din("nsa_ck_w2", [DEPTH, 128, 64])
    cv_w1 = din("nsa_cv_w1", [DEPTH, 2048, 128])
    cv_w2 = din("nsa_cv_w2", [DEPTH, 128, 64])
    nsa_ng = din("nsa_norm_g", [DEPTH, 64])
    diff_lam = din("diff_lam", [DEPTH, 128])
    diff_ng = din("diff_norm_g", [DEPTH, 64])
    ssm_cw = din("ssm_conv_w", [DEPTH, 4, 512])
    ssm_cb = din("ssm_conv_b", [DEPTH, 512])
    ssm_dtb = din("ssm_dt_bias", [DEPTH, 4])
    ssm_alog = din("ssm_a_log", [DEPTH, 4])
    ssm_d = din("ssm_d", [DEPTH, 4])
    ssm_ng = din("ssm_norm_g", [DEPTH, 256])
    ml_cw = din("ml_conv_w", [DEPTH, 4, 512])
    ml_cb = din("ml_conv_b", [DEPTH, 512])
    ml_ifb = din("ml_if_b", [DEPTH, 8])
    ml_ng = din("ml_norm_g", [DEPTH, 64])
    final_g = din("final_g", [D])
    c_identb = din("c_identb", [128, 128], BF16)
    c_identf = din("c_identf", [128, 128])
    c_tri4 = din("c_tri4", [128, 512], BF16)
    c_atri4 = din("c_atri4", [128, 512], BF16)
    c_U = din("c_U", [128, 128])
    c_mb_st = din("c_mb_st", [128, 128])
    c_mb_ts = din("c_mb_ts", [128, 128])
    c_sel127 = din("c_sel127", [128, 128])
    c_E = din("c_E", [64, S], BF16)
    c_c2s = din("c_c2s", [NCC * 128, 65], BF16)
    c_cmask = din("c_cmask", [NCC * 128, S], BF16)
    c_selmul = din("c_selmul", [S, 64])
    c_seladd = din("c_seladd", [S, 64])

    out_d = TT(nc.dram_tensor("out", [S, D], F32, kind="ExternalOutput").ap(), "out")
    xres = dscr("xres", [S, D], F32)
    if debug:
        projF = TT(nc.dram_tensor("projF", [NF, S], BF16, kind="ExternalOutput").ap(), "projF")
        projT = TT(nc.dram_tensor("projT", [S, NTC], BF16, kind="ExternalOutput").ap(), "projT")
        projTs = TT(nc.dram_tensor("projTs", [S, 24], F32, kind="ExternalOutput").ap(), "projTs")
        dbg = TT(nc.dram_tensor("dbg", [128, DEPTH * 24], F32, kind="ExternalOutput").ap(), "dbg")
    else:
        projF = dscr("projF", [NF, S], BF16)
        projT = dscr("projT", [S, NTC], BF16)
        projTs = dscr("projTs", [S, 24], F32)
    if debug:
        mix = TT(nc.dram_tensor("mix", [S, D], BF16, kind="ExternalOutput").ap(), "mix")
    else:
        mix = dscr("mix", [S, D], BF16)

    es = contextlib.ExitStack()
    with es:
        es.enter_context(nc.allow_non_contiguous_dma("small strided parameter loads"))
        es.enter_context(nc.allow_low_precision("bf16 matmul operands, fp32 accumulation"))
        kb = KB(nc, es)
        identb = kb.sb(es, [128, 128], BF16, "identb")
        identf = kb.sb(es, [128, 128], F32, "identf")
        ones_f = kb.sb(es, [128, 128], F32, "onesf")
        kb.dma(identb[:], c_identb[:, :])
        kb.dma(identf[:], c_identf[:, :])
        kb.memset("pool", ones_f[:], 1.0)
        kb.eps_col = kb.sb(es, [128, 1], F32, "epscol")
        kb.memset("pool", kb.eps_col[:], EPS)
        modc = kb.sb(es, [128, DEPTH, 24], F32, "modc")
        Acoef = kb.sb(es, [128, DEPTH, 8], F32, "Acoef")
        gate_row = kb.sb(es, [128, DEPTH, D], F32, "gate_row")
        fin_row = kb.sb(es, [128, D], F32, "fin_row")
        kb.dma(fin_row[:], V(final_g, final_g.h.partition_broadcast(128)))

        with contextlib.ExitStack() as st:
            cact = kb.sb(st, [128, 8], F32, "cact")
            kb.dma(cact[:], V(c_in, c_in.h.rearrange("(k p) -> p k", p=128)))
            csig = kb.sb(st, [128, 8], F32, "csig")
            kb.act(csig[:], cact[:], AF.Sigmoid)
            kb.tt("dve", cact[:], cact[:], csig[:], ALU.mult)
            adab = kb.sb(st, [128, 24], F32, "adab")
            ng = kb.sb(st, [128, 8], F32, "ng")
            pm = kb.ps(st, [128, 24], F32, "pm")
            pg = kb.ps(st, [128, 2, 512], F32, "pg")
            wring = Ring([kb.sb(st, [128, 8, 1024], F32, "adaw%d" % i) for i in range(2)])
            for l in range(DEPTH):
                kb.dma(adab[:], V(ada_b, ada_b.h[l].rearrange("(j p) -> p j", p=128)))
                kb.dma(ng[:], V(norm_g, norm_g.h[l].rearrange("(k p) -> p k", p=128)))
                for blk in range(3):
                    wt = wring.next()
                    for k in range(8):
                        kb.dma(wt[:, k, :], ada_w[l, k * 128:(k + 1) * 128, blk * 1024:(blk + 1) * 1024],
                               q=("sp" if k % 2 == 0 else "pool"))
                    for jj in range(8):
                        j = blk * 8 + jj
                        for k in range(8):
                            kb.mm(pm[:, j:j + 1], wt[:, k, jj * 128:(jj + 1) * 128], cact[:, k:k + 1],
                                  start=(k == 0), stop=(k == 7), inc=(k == 7))
                kb.tt("dve", modc[:, l, :], pm[:], adab[:], ALU.add)
                kb.stt("dve", Acoef[:, l, :], modc[:, l, 8:16], 1.0, ng[:], ALU.add, ALU.mult)
                for j in range(8):
                    kb.mm(pg[:, j // 4, (j % 4) * 128:(j % 4 + 1) * 128],
                          modc[:, l, 16 + j:17 + j].bc([128, 128]), identf[:], inc=(j % 4 == 3))
                kb.cp("act", gate_row[:, l, :], pg[:].re("p a b -> p (a b)"))
        kb.barrier()
        if debug:
            kb.dma(dbg[:, :], modc[:].re("p l j -> p (l j)"))
            kb.barrier()

        for l in range(DEPTH):
            if stop <= 0:
                break
            x_src = x_in if l == 0 else xres
            last = (l == DEPTH - 1)
            phase_inproj(kb, nc, l, S, x_src, modc, Acoef, w_inF, w_inT, projF, projT, projTs, identb)
            kb.barrier()
            if stop <= 1:
                break
            phase_nsa(kb, nc, l, S, NCMP, NCC, projF, projT, projTs, mix, identb, identf,
                      c_tri4, c_atri4, c_E, c_c2s, c_cmask, c_selmul, c_seladd,
                      cmp_pos, ck_w1, ck_w2, cv_w1, cv_w2, nsa_ng)
            kb.barrier()
            if stop <= 2:
                break
            phase_diff(kb, nc, l, S, projF, projT, mix, identf, c_tri4, diff_lam, diff_ng)
            kb.barrier()
            if stop <= 3:
                break
            phase_ssd(kb, nc, l, S, projF, projT, projTs, mix, identb, identf, ones_f, c_U, c_mb_st,
                      ssm_cw, ssm_cb, ssm_dtb, ssm_alog, ssm_d, ssm_ng)
            kb.barrier()
            if stop <= 4:
                break
            phase_mlstm(kb, nc, l, S, projF, projT, projTs, mix, identb, identf, ones_f, c_U, c_mb_st, c_mb_ts,
                        c_sel127, ml_cw, ml_cb, ml_ifb, ml_ng)
            kb.barrier()
            if stop <= 5:
                break
            phase_out(kb, nc, l, S, x_src, (out_d if last else xres), mix, w_out, gate_row, fin_row, identb, last)
            kb.barrier()
    return nc


def bcast_rows(t, sl, n=128):
    return V(t, sl.partition_broadcast(n))


import os
INPROJ_SUB = 9


def phase_inproj(kb, nc, l, S, x_src, modc, Acoef, w_inF, w_inT, projF, projT, projTs, identb):
    NT = S // 128
    NG = S // 512
    SUB = INPROJ_SUB
    with contextlib.ExitStack() as st:
        wF = kb.sb(st, [128, 8, NF], BF16, "wF")
        wT = kb.sb(st, [128, 8, NTC], BF16, "wT")
        stg = Ring([kb.sb(st, [128, 1024], F32, "wstg%d" % i) for i in range(3)])
        ci = 0
        for (src, dst, ncol) in ((w_inF, wF, NF), (w_inT, wT, NTC)):
            for k in range(8):
                for c0 in range(0, ncol, 1024):
                    w = min(1024, ncol - c0)
                    sg = stg.next()
                    kb.dma(sg[:, 0:w], src[l, k * 128:(k + 1) * 128, c0:c0 + w], q=("sp" if ci % 2 == 0 else "pool"))
                    kb.cp(("dve" if ci % 2 == 0 else "act"), dst[:, k, c0:c0 + w], sg[:, 0:w])
                    ci += 1
        xin = Ring([kb.sb(st, [128, 4, D], F32, "xin%d" % i) for i in range(2)])
        hT = Ring([kb.sb(st, [128, 8, 512], BF16, "hT%d" % i) for i in range(2)])
        xn = Ring([kb.sb(st, [128, D], BF16, "xn%d" % i) for i in range(2)])
        junk = kb.sb(st, [128, D], BF16, "junk")
        ss = kb.sb(st, [128, 4], F32, "ss")
        rstd = kb.sb(st, [128, 4], F32, "rstd")
        ptr = Ring([kb.ps(st, [128, 8, 128], BF16, "ptr%d" % i) for i in range(2)])
        pF = Ring([kb.ps(st, [128, 512], F32, "pF%d" % i) for i in range(2)])
        pT = Ring([kb.ps(st, [128, 512], F32, "pT%d" % i) for i in range(2)])
        fstage = Ring([kb.sb(st, [128, 512], BF16, "fst%d" % i) for i in range(3)])
        tstage = Ring([kb.sb(st, [128, NTC], BF16, "tst%d" % i) for i in range(2)])
        tsstage = Ring([kb.sb(st, [128, 24], F32, "tsst%d" % i) for i in range(2)])
        ev = 0

        def load_x(g):
            xi_ = xin.next()
            kb.dma(xi_[:], V(x_src, x_src.h[g * 512:(g + 1) * 512, :].rearrange("(j p) d -> p j d", p=128)), q="pool")
            return xi_
        xi_next = load_x(0)
        for g in range(NG if SUB >= 2 else 0):
            xi = xi_next
            if g + 1 < NG:
                xi_next = load_x(g + 1)
            h = hT.next()
            for j in range(4):
                kb.act(junk[:], xi[:, j, :], AF.Square, accum=ss[:, j:j + 1])
                kb.rsqrt(rstd[:, j:j + 1], ss[:, j:j + 1], 1.0 / D)
                xb = xn.next()
                kb.ts("dve", xb[:], xi[:, j, :], rstd[:, j:j + 1], None, ALU.mult)
                pt = ptr.next()
                for k in range(8):
                    kb.tr(pt[:, k, :], xb[:, k * 128:(k + 1) * 128], identb[:], inc=(k == 7))
                for k in range(8):
                    e = "act" if j % 2 == 0 else "dve"
                    if e == "act":
                        kb.act(h[:, k, j * 128:(j + 1) * 128], pt[:, k, :], AF.Identity,
                               bias=modc[:, l, k:k + 1], scale=Acoef[:, l, k:k + 1])
                    else:
                        kb.ts("dve", h[:, k, j * 128:(j + 1) * 128], pt[:, k, :], Acoef[:, l, k:k + 1],
                              modc[:, l, k:k + 1], ALU.mult, ALU.add)
            for r0 in (range(0, NF, 128) if SUB >= 3 else []):
                w = 128
                p = pF.next()
                for k in range(8):
                    kb.mm(p[0:w, :], wF[:, k, r0:r0 + w], h[:, k, :], start=(k == 0), stop=(k == 7), inc=(k == 7))
                fs = fstage.next()
                kb.cp(("act" if ev % 2 == 0 else "dve"), fs[0:w, :], p[0:w, :])
                ev += 1
                kb.dma(projF[r0:r0 + w, g * 512:(g + 1) * 512], fs[0:w, :], q="sp", disjoint=True)
            for j in range(4 if SUB >= 4 else 0):
                ts_ = tstage.next()
                tss = tsstage.next()
                for c0 in range(0, NTC, 512):
                    w = min(512, NTC - c0)
                    p = pT.next()
                    for k in range(8):
                        kb.mm(p[:, 0:w], h[:, k, j * 128:(j + 1) * 128], wT[:, k, c0:c0 + w],
                              start=(k == 0), stop=(k == 7), inc=(k == 7))
                    e_ = ("act" if ev % 2 == 0 else "dve")
                    kb.cp(e_, ts_[:, c0:c0 + w], p[:, 0:w])
                    ev += 1
                    for nm in (("ag", "dt", "dif") if SUB >= 5 else ()):
                        tc0, tw = T_COL[nm]
                        if c0 <= tc0 < c0 + w:
                            so, _ = TS_COL[nm]
                            kb.cp(e_, tss[:, so:so + tw], p[:, tc0 - c0:tc0 - c0 + tw])
                tok = g * 512 + j * 128
                if SUB >= 6:
                    kb.dma(projT[tok:tok + 128, :], ts_[:], q="sp", disjoint=True)
                if SUB >= 7:
                    kb.dma(projTs[tok:tok + 128, :], tss[:], q="sp", disjoint=True)


def load_T(kb, dst, projT, name, S, sub=None, q="sp"):
    NT = S // 128
    c0, w = T_COL[name]
    if sub is not None:
        c0, w = c0 + sub[0], sub[1]
    step = 8
    for a in range(0, NT, step):
        b = min(NT, a + step)
        kb.dma(dst[:, a:b, :], V(projT, projT.h[a * 128:b * 128, c0:c0 + w].rearrange("(c p) n -> p c n", p=128)), q=q)


def load_Ts(kb, dst, projTs, name, S):
    c0, w = TS_COL[name]
    kb.dma(dst[:], V(projTs, projTs.h[:, c0:c0 + w].rearrange("(c p) n -> p c n", p=128)))


def head_tail(kb, st_bufs, o, ng_row, sz, mix, tok, col0, post_scale, nheads=4, hd=64):
    sq, ssq, rs, yo = st_bufs
    W = nheads * hd
    kb.tt("pool", sq[:, 0:W], o, o, ALU.mult)
    kb.red(ssq[:, 0:nheads], sq[:, 0:W].re("p (h d) -> p h d", h=nheads), ALU.add)
    kb.rsqrt(rs[:, 0:nheads], ssq[:, 0:nheads], 1.0 / hd, post_scale)
    for h in range(nheads):
        kb.ts("dve", sq[:, h * hd:(h + 1) * hd], o[:, h * hd:(h + 1) * hd], rs[:, h:h + 1], None, ALU.mult)
    kb.tt("pool", sq[:, 0:W], sq[:, 0:W], ng_row, ALU.mult)
    kb.tt("dve", yo[:, 0:W], sq[:, 0:W], sz, ALU.mult)
    kb.dma(mix[tok:tok + 128, col0:col0 + W], yo[:, 0:W], q="sp", disjoint=True)


def silu_all(kb, st, z, S, name):
    NT = S // 128
    W = 256
    sg = kb.sb(st, [128, NT, W], BF16, name + "_sg")
    kb.act(sg[:], z[:], AF.Sigmoid)
    kb.tt("pool", z[:], z[:], sg[:], ALU.mult)
    return z


def phase_nsa(kb, nc, l, S, NCMP, NCC, projF, projT, projTs, mix, identb, identf,
              c_tri4, c_atri4, c_E, c_c2s, c_cmask, c_selmul, c_seladd,
              cmp_pos, ck_w1, ck_w2, cv_w1, cv_w2, nsa_ng):
    NT = S // 128
    NCP = NCC * 128
    with contextlib.ExitStack() as st:
        q_all = kb.sb(st, [128, NT, 4, 128], BF16, "q_all")
        qtiles = [kb.sub(q_all[:, i]) for i in range(NT)]
        kcT = kb.sb(st, [64, S], BF16, "kcT")
        vcT = kb.sb(st, [64, S], BF16, "vcT")
        kwT = kb.sb(st, [64, S], BF16, "kwT")
        lsel = kb.sb(st, [128, S], BF16, "lsel")
        vs1 = kb.sb(st, [128, NT, 65], BF16, "vs1")
        vw1 = kb.sb(st, [128, NT, 65], BF16, "vw1")
        gts = kb.sb(st, [128, NT, 12], F32, "gts")
        z = kb.sb(st, [128, NT, 256], BF16, "z")
        tri4 = kb.sb(st, [128, 512], BF16, "tri4")
        atri4 = kb.sb(st, [128, 512], BF16, "atri4")
        c2s = kb.sb(st, [128, NCC, 65], BF16, "c2s")
        cmask = kb.sb(st, [128, NCC, S], BF16, "cmask")
        selmul = kb.sb(st, [128, NT, 64], F32, "selmul")
        seladd = kb.sb(st, [128, NT, 64], F32, "seladd")
        ngrow = kb.sb(st, [128, 4, 64], F32, "ngrow")
        for h in range(4):
            r0, _ = F_ROW["aq%d" % h]
            for i in range(NT):
                pass
            kb.dma(V(q_all, q_all.h[0:64, :, h, :]), V(projF, projF.h[r0:r0 + 64, :].rearrange("d (c t) -> d c t", t=128)),
                   q=("sp" if h % 2 == 0 else "pool"))
        kb.memset("pool", V(q_all, q_all.h[64:128]), 0.0)
        kb.dma(kcT[:], projF[F_ROW["akc"][0]:F_ROW["akc"][0] + 64, :])
        kb.dma(vcT[:], projF[F_ROW["avc"][0]:F_ROW["avc"][0] + 64, :], q="pool")
        kb.dma(kwT[:], projF[F_ROW["akw"][0]:F_ROW["akw"][0] + 64, :])
        kb.dma(lsel[0:64, :], projF[F_ROW["aks"][0]:F_ROW["aks"][0] + 64, :], q="pool")
        kb.dma(lsel[64:128, :], c_E[:, :])
        kb.memset("pool", vs1[:, :, 64:65], 1.0)
        kb.memset("pool", vw1[:, :, 64:65], 1.0)
        load_T(kb, V(vs1, vs1.h[:, :, 0:64]), projT, "vs", S)
        load_T(kb, V(vw1, vw1.h[:, :, 0:64]), projT, "vw", S, q="pool")
        load_Ts(kb, gts, projTs, "ag", S)
        load_T(kb, z, projT, "az", S)
        kb.dma(tri4[:], c_tri4[:, :])
        kb.dma(atri4[:], c_atri4[:, :])
        kb.dma(c2s[:], V(c_c2s, c_c2s.h.rearrange("(c p) n -> p c n", p=128)))
        for cc in range(NCC):
            kb.dma(cmask[:, cc, :], c_cmask[cc * 128:(cc + 1) * 128, :], q=("sp" if cc == 0 else "pool"))
        kb.dma(selmul[:], V(c_selmul, c_selmul.h.rearrange("(c p) n -> p c n", p=128)))
        kb.dma(seladd[:], V(c_seladd, c_seladd.h.rearrange("(c p) n -> p c n", p=128)), q="pool")
        kb.dma(ngrow[:, 0, :], bcast_rows(nsa_ng, nsa_ng.h[l]))
        for h in range(1, 4):
            kb.cp("pool", ngrow[:, h, :], ngrow[:, 0, :])
        kb.act(gts[:], gts[:], AF.Sigmoid)

        kcmpT = kb.sb(st, [64, NCP], BF16, "kcmpT")
        vcmp1 = kb.sb(st, [128, NCC, 65], BF16, "vcmp1")
        kb.memset("pool", vcmp1[:, :, 64:65], 1.0)
        with contextlib.ExitStack() as s2:
            sz = silu_all(kb, s2, z, S, "az")
            posT = kb.sb(s2, [64, 32], F32, "posT")
            posTb = kb.sb(s2, [64, 32], BF16, "posTb")
            kb.dma(posT[:], V(cmp_pos, cmp_pos.h[l].rearrange("l d -> d l")))
            kb.cp("dve", posTb[:], posT[:])
            w1s = kb.sb(s2, [64, 16, 128], F32, "w1s")
            w1b = kb.sb(s2, [64, 32, 128], BF16, "w1b")
            w2s = kb.sb(s2, [128, 64], F32, "w2s")
            w2b = kb.sb(s2, [128, 64], BF16, "w2b")
            hid = kb.sb(s2, [128, NCP], BF16, "hid")
            tpre = kb.sb(s2, [128, NCP], F32, "tpre")
            sgm = kb.sb(s2, [128, NCP], F32, "sgm")
            cst = kb.sb(s2, [128, 1], F32, "cst")
            pc = kb.ps(s2, [128, 1], F32, "pc")
            ph = kb.ps(s2, [128, NCP], F32, "ph")
            po = kb.ps(s2, [128, NCP], F32, "po")
            for which, (w1d, w2d, srcT) in enumerate(((ck_w1, ck_w2, kcT), (cv_w1, cv_w2, vcT))):
                for hf in range(2):
                    kb.dma(w1s[:], V(w1d, w1d.h[l, hf * 1024:(hf + 1) * 1024, :].rearrange("(l d) h -> d l h", d=64)))
                    kb.cp("dve", w1b[:, hf * 16:(hf + 1) * 16, :], w1s[:])
                kb.dma(w2s[:], w2d[l])
                kb.cp("dve", w2b[:], w2s[:])
                for li in range(32):
                    kb.mm(pc[:], w1b[:, li, :], posTb[:, li:li + 1], start=(li == 0), stop=(li == 31), inc=(li == 31))
                kb.cp("dve", cst[:], pc[:])
                for li in range(32):
                    kb.mm(ph[:, 0:NCMP], w1b[:, li, :], V(srcT, srcT.h[:, li:li + 16 * (NCMP - 1) + 1:16]),
                          start=(li == 0), stop=(li == 31), inc=(li == 31))
                kb.memset("pool", hid[:], 0.0)
                kb.ts("dve", tpre[:, 0:NCMP], ph[:, 0:NCMP], cst[:, 0:1], None, ALU.add)
                kb.act(sgm[:, 0:NCMP], tpre[:, 0:NCMP], AF.Sigmoid)
                kb.tt("dve", hid[:, 0:NCMP], tpre[:, 0:NCMP], sgm[:, 0:NCMP], ALU.mult)
                if which == 0:
                    kb.mm(po[0:64, :], w2b[:], hid[:])
                    kb.cp("dve", kcmpT[:], po[0:64, :])
                else:
                    for cc in range(NCC):
                        kb.mm(po[:, cc * 64:(cc + 1) * 64], hid[:, cc * 128:(cc + 1) * 128], w2b[:], inc=(cc == NCC - 1))
                    kb.cp("dve", V(vcmp1, vcmp1.h[:, :, 0:64]), po[:, 0:NCC * 64].re("p (c d) -> p c d", c=NCC))
        kb.barrier()

        psc = Ring([kb.ps(st, [128, 512], F32, "psc%d" % i) for i in range(3)])
        pacc = [kb.ps(st, [65, 512], F32, "pacc%d" % i) for i in range(3)]
        pmisc = kb.ps(st, [128, 512], F32, "pmisc")
        ptl = kb.ps(st, [128, 6, 65], F32, "ptl")
        Pr = Ring([kb.sb(st, [128, 512], BF16, "P%d" % i) for i in range(5)])
        Pc = [kb.sb(st, [128, 512], BF16, "Pc%d" % i) for i in range(NCC)]
        oTs = [kb.sb(st, [65, 3, 512], F32, "oT%d" % i) for i in range(2)]
        rdc = kb.sb(st, [128, 4], F32, "rdc")
        imp = kb.sb(st, [128, 64], F32, "imp")
        imp2 = kb.sb(st, [128, 64], F32, "imp2")
        m8 = kb.sb(st, [128, 16], F32, "m8")
        selpad = kb.sb(st, [128, 128], F32, "selpad")
        kb.memset("pool", selpad[:], 0.0)
        tl = kb.sb(st, [128, 12, 65], F32, "tl")
        rden = kb.sb(st, [128, 12], F32, "rden")
        fco = kb.sb(st, [128, 12], F32, "fco")
        o = kb.sb(st, [128, 256], F32, "o")
        bufs = (kb.sb(st, [128, 256], F32, "sq"), kb.sb(st, [128, 4], F32, "ssq"),
                kb.sb(st, [128, 4], F32, "rs"), kb.sb(st, [128, 256], BF16, "yo"))

        def select1(qi, ncv):
            for h in range(4):
                for cc in range(ncv):
                    kb.mm(pmisc[:, h * 65:(h + 1) * 65], Pc[cc][:, h * 128:(h + 1) * 128], c2s[:, cc, :],
                          start=(cc == 0), stop=(cc == ncv - 1), inc=(h == 3 and cc == ncv - 1))
            pim = pmisc[:, 0:260].re("p (h n) -> p h n", h=4)
            kb.ts("dve", rdc[:], pim[:, :, 64], 1e-30, None, ALU.max)
            kb.recip(rdc[:], rdc[:])
            kb.ts("dve", imp[:], pim[:, 0, 0:64], rdc[:, 0:1], None, ALU.mult)
            for h in range(1, 4):
                kb.stt("dve", imp[:], pim[:, h, 0:64], rdc[:, h:h + 1], imp[:], ALU.mult, ALU.add)
            kb.tt("dve", imp[:], imp[:], selmul[:, qi, :], ALU.mult)
            kb.tt("dve", imp[:], imp[:], seladd[:, qi, :], ALU.add)
            kb.op("dve", lambda e: e.max(out=m8[:, 0:8].ap, in_=imp[:].ap), [m8[:]], [imp[:]])
            kb.op("dve", lambda e: e.match_replace(out=imp2[:].ap, in_to_replace=m8[:, 0:8].ap,
                                                   in_values=imp[:].ap, imm_value=-3.0), [imp2[:]], [m8[:], imp[:]])
            kb.op("dve", lambda e: e.max(out=m8[:, 8:16].ap, in_=imp2[:].ap), [m8[:]], [imp2[:]])
            kb.ts("dve", selpad[:, 64:128], imp[:], m8[:, 15:16], -1.0, ALU.is_ge, ALU.add)

        def select2(qi):
            qt = qtiles[qi]
            kb.tr(pmisc[:, 260:388], selpad[:], identf[:])
            kb.cp("dve", V(qt, qt.h[64:128]), V(pmisc, pmisc.h[64:128, 260:388].unsqueeze(1).to_broadcast([64, 4, 128])))

        def tail(qi):
            q0 = qi * 128
            oT = oTs[qi % 2]
            for half in range(2):
                for i in range(6):
                    idx = half * 6 + i
                    b, h = idx // 4, idx % 4
                    kb.tr(ptl[:, i, :], oT[:, b, h * 128:(h + 1) * 128], identf[0:65, 0:65], inc=(i == 5))
                kb.cp("dve", tl[:, half * 6:(half + 1) * 6, :], ptl[:])
            kb.ts("dve", rden[:], tl[:, :, 64], 1e-30, None, ALU.max)
            kb.recip(rden[:], rden[:])
            kb.tt("dve", fco[:].re("p (b h) -> p b h", b=3), rden[:].re("p (b h) -> p b h", b=3),
                  V(gts, gts.h[:, qi, :].rearrange("p (h b) -> p b h", b=3)), ALU.mult)
            for h in range(4):
                kb.ts("dve", o[:, h * 64:(h + 1) * 64], tl[:, h, 0:64], fco[:, h:h + 1], None, ALU.mult)
                for b in (1, 2):
                    kb.stt("dve", o[:, h * 64:(h + 1) * 64], tl[:, b * 4 + h, 0:64], fco[:, b * 4 + h:b * 4 + h + 1],
                           o[:, h * 64:(h + 1) * 64], ALU.mult, ALU.add)
            head_tail(kb, bufs, o[:], ngrow[:].re("p h d -> p (h d)"), sz[:, qi, :], mix, q0, 0, 1.0)

        dq = Deferred()
        for qi in range(NT):
            qt = qtiles[qi]
            rq = V(qt, qt.h[0:64].rearrange("d h t -> d (h t)"))
            rqs = V(qt, qt.h.rearrange("d h t -> d (h t)"))
            q0 = qi * 128
            ncv = min(NCC, (8 * qi + 6) // 128 + 1)
            wl = [kc for kc in range(qi - 4, qi + 1) if kc >= 0]
            items = [("c", cc) for cc in range(ncv)] + [("w", kc) for kc in wl] + [("s", kc) for kc in range(qi + 1)]
            sel2 = [None]
            pvc = [None]
            for (kind, kc) in items:
                p = psc.next()
                if kind == "c":
                    kb.mm(p[:], kcmpT[:, kc * 128:(kc + 1) * 128], rq)
                    P = Pc[kc]
                    kb.act(P[:], p[:], AF.Exp, scale=0.125)
                    cm = V(cmask, cmask.h[:, kc, q0:q0 + 128].unsqueeze(1).to_broadcast([128, 4, 128]))
                    kb.tt("pool", P[:].re("p (h t) -> p h t", h=4), P[:].re("p (h t) -> p h t", h=4), cm, ALU.mult)
                elif kind == "w":
                    kb.mm(p[:], kwT[:, kc * 128:(kc + 1) * 128], rq)
                    P = Pr.next()
                    kb.act(P[:], p[:], AF.Exp, scale=0.125)
                    if kc == qi:
                        kb.tt("pool", P[:], P[:], tri4[:], ALU.mult)
                    elif kc == qi - 4:
                        kb.tt("pool", P[:], P[:], atri4[:], ALU.mult)
                else:
                    if kc == 0:
                        dq.run_through(pvc[0])
                        dq.force(sel2[0])
                    kb.mm(p[:], lsel[:, kc * 128:(kc + 1) * 128], rqs)
                    P = Pr.next()
                    kb.act(P[:], p[:], AF.Exp, scale=0.125)
                    if kc == qi:
                        kb.tt("pool", P[:], P[:], tri4[:], ALU.mult)

                def pv(kind=kind, kc=kc, P=P, qi=qi, ncv=ncv, wl=wl, sel2=sel2):
                    if kind == "c":
                        kb.mm(pacc[0][:], vcmp1[:, kc, :], P[:], start=(kc == 0), stop=(kc == ncv - 1), inc=True)
                        if kc == ncv - 1:
                            select1(qi, ncv)
                            sel2[0] = dq.push(lambda qi=qi: select2(qi), 3)
                    elif kind == "w":
                        kb.mm(pacc[2][:], vw1[:, kc, :], P[:], start=(kc == wl[0]), stop=(kc == qi), inc=True)
                    else:
                        kb.mm(pacc[1][:], vs1[:, kc, :], P[:], start=(kc == 0), stop=(kc == qi), inc=True)
                        if kc == qi:
                            for b in range(3):
                                kb.cp("dve", oTs[qi % 2][:, b, :], pacc[b][:])
                            dq.push(lambda qi=qi: tail(qi), 2)
                dq.step()
                e_ = dq.push(pv, 1)
                if kind == "c" and kc == ncv - 1:
                    pvc[0] = e_
        dq.flush()


def phase_diff(kb, nc, l, S, projF, projT, mix, identf, c_tri4, diff_lam, diff_ng):
    NT = S // 128
    lambda_init = 0.8 - 0.6 * math.exp(-0.3 * l)
    sc = 32 ** -0.5
    with contextlib.ExitStack() as st:
        qT = kb.sb(st, [64, 4, S], BF16, "qT")
        kT = kb.sb(st, [64, 4, S], BF16, "kT")
        v1 = kb.sb(st, [128, NT, 4, 65], BF16, "v1")
        z = kb.sb(st, [128, NT, 256], BF16, "z")
        tri4 = kb.sb(st, [128, 512], BF16, "tri4")
        ngrow = kb.sb(st, [128, 4, 64], F32, "ngrow")
        lam = kb.sb(st, [128, 128], F32, "lam")
        lp = kb.sb(st, [128, 64], F32, "lp")
        ls = kb.sb(st, [128, 2], F32, "ls")
        nlam = kb.sb(st, [128, 1], F32, "nlam")
        s2 = contextlib.ExitStack()
        vtmp = kb.sb(s2, [128, NT, 256], BF16, "vtmp")
        for h in range(4):
            kb.dma(qT[:, h, :], projF[F_ROW["bq%d" % h][0]:F_ROW["bq%d" % h][0] + 64, :], q="sp")
            kb.dma(kT[:, h, :], projF[F_ROW["bk%d" % h][0]:F_ROW["bk%d" % h][0] + 64, :], q="pool")
        load_T(kb, vtmp, projT, "bv", S)
        load_T(kb, z, projT, "bz", S, q="pool")
        kb.dma(tri4[:], c_tri4[:, :])
        kb.dma(ngrow[:, 0, :], bcast_rows(diff_ng, diff_ng.h[l]))
        kb.dma(lam[:], bcast_rows(diff_lam, diff_lam.h[l]))
        for h in range(1, 4):
            kb.cp("pool", ngrow[:, h, :], ngrow[:, 0, :])
        kb.memset("pool", V(v1, v1.h[:, :, :, 64:65]), 1.0)
        kb.cp("pool", V(v1, v1.h[:, :, :, 0:64]), vtmp[:].re("p c (h d) -> p c h d", h=4))
        lv = lam[:].re("p (a b d) -> p a b d", a=2, b=2)
        kb.tt("dve", lp[:].re("p (a d) -> p a d", a=2), lv[:, :, 0, :], lv[:, :, 1, :], ALU.mult)
        kb.red(ls[:], lp[:].re("p (a d) -> p a d", a=2), ALU.add)
        kb.act(ls[:], ls[:], AF.Exp)
        kb.tt("dve", nlam[:], ls[:, 1:2], ls[:, 0:1], ALU.subtract)
        kb.ts("dve", nlam[:], nlam[:], -lambda_init, None, ALU.add)
        sz = silu_all(kb, s2, z, S, "bz")
        s2.close()
        kb.barrier()

        psc = Ring([kb.ps(st, [128, 512], F32, "psc%d" % i) for i in range(4)])
        _pa = [kb.ps(st, [65, 512], F32, "pacc%d" % hp) for hp in range(2)]
        paccs = [[_pa[0], _pa[0]], [_pa[1], _pa[1]]]
        ptl = Ring([kb.ps(st, [128, 4, 65], F32, "ptl%d" % i) for i in range(2)])
        Pr = Ring([kb.sb(st, [128, 512], BF16, "P%d" % i) for i in range(6)])
        oTs = [kb.sb(st, [65, 2, 512], F32, "oT%d" % i) for i in range(2)]
        tls = [kb.sb(st, [128, 8, 65], F32, "tl%d" % i) for i in range(2)]
        rden = kb.sb(st, [128, 8], F32, "rden")
        o1 = kb.sb(st, [128, 64], F32, "o1")
        o = kb.sb(st, [128, 256], F32, "o")
        bufs = (kb.sb(st, [128, 256], F32, "sq"), kb.sb(st, [128, 4], F32, "ssq"),
                kb.sb(st, [128, 4], F32, "rs"), kb.sb(st, [128, 256], BF16, "yo"))
        qm = Ring([kb.sb(st, [64, 4, 2, 128], BF16, "qm%d" % i) for i in range(2)])
        for t_ in qm.tiles:
            kb.memset("pool", t_[:], 0.0)

        def tail(qi):
            q0 = qi * 128
            oT = oTs[qi % 2]
            tl = tls[qi % 2]
            for half in range(2):
                pt = ptl.next()
                for i in range(4):
                    kb.tr(pt[:, i, :], oT[:, half, i * 128:(i + 1) * 128], identf[0:65, 0:65], inc=(i == 3))
                kb.cp("dve", tl[:, half * 4:(half + 1) * 4, :], pt[:])
            kb.ts("dve", rden[:], tl[:, :, 64], 1e-30, None, ALU.max)
            kb.recip(rden[:], rden[:])
            kb.ts("dve", V(rden, rden.h[:, 1:8:2]), V(rden, rden.h[:, 1:8:2]), nlam[:, 0:1], None, ALU.mult)
            for h in range(4):
                kb.ts("dve", o1[:], tl[:, 2 * h, 0:64], rden[:, 2 * h:2 * h + 1], None, ALU.mult)
                kb.stt("dve", o[:, h * 64:(h + 1) * 64], tl[:, 2 * h + 1, 0:64], rden[:, 2 * h + 1:2 * h + 2],
                       o1[:], ALU.mult, ALU.add)
            head_tail(kb, bufs, o[:], ngrow[:].re("p h d -> p (h d)"), sz[:, qi, :], mix, q0, 256, 1.0 - lambda_init)

        dq = Deferred()
        for qi in range(NT):
            q0 = qi * 128
            qmt = qm.next()
            kb.cp("pool", V(qmt, qmt.h[0:32, :, 0, :]), qT[0:32, :, q0:q0 + 128])
            kb.cp("pool", V(qmt, qmt.h[32:64, :, 1, :]), qT[32:64, :, q0:q0 + 128])
            for hp in range(2):
                for kc in range(qi + 1):
                    p = psc.next()
                    for hh in range(2):
                        h = hp * 2 + hh
                        kb.mm(p[:, hh * 256:(hh + 1) * 256], kT[:, h, kc * 128:(kc + 1) * 128],
                              V(qmt, qmt.h[:, h].rearrange("d c t -> d (c t)")), inc=(hh == 1))
                    P = Pr.next()
                    kb.act(P[:], p[:], AF.Exp, scale=sc)
                    if kc == qi:
                        kb.tt("pool", P[:], P[:], tri4[:], ALU.mult)

                    def pv(qi=qi, hp=hp, kc=kc, P=P):
                        pa = paccs[hp][qi % 2]
                        for hh in range(2):
                            h = hp * 2 + hh
                            kb.mm(pa[:, hh * 256:(hh + 1) * 256], v1[:, kc, h, :], P[:, hh * 256:(hh + 1) * 256],
                                  start=(kc == 0 and hh == 0), stop=(kc == qi and hh == 1), inc=(hh == 1))
                        if kc == qi:
                            kb.cp("act", oTs[qi % 2][:, hp, :], pa[:])
                            if hp == 1:
                                dq.push(lambda qi=qi: tail(qi), 2)
                    dq.step()
                    dq.push(pv, 2)
        dq.flush()


def conv_silu(kb, eng, dst, src, acc, wcol, bcol, S, rows):
    kb.ts(eng, acc[0:rows, :], src, wcol[:, 3:4], bcol, ALU.mult, ALU.add)
    for k in range(3):
        sh = 3 - k
        kb.stt("dve", acc[0:rows, sh:S], src[:, 0:S - sh], wcol[:, k:k + 1], acc[0:rows, sh:S], ALU.mult, ALU.add)
    kb.act(dst, acc[0:rows, :], AF.Silu)


def phase_ssd(kb, nc, l, S, projF, projT, projTs, mix, identb, identf, ones_f, c_U, c_mb_st,
              ssm_cw, ssm_cb, ssm_dtb, ssm_alog, ssm_d, ssm_ng):
    NT = S // 128
    with contextlib.ExitStack() as st:
        U = kb.sb(st, [128, 128], F32, "U")
        mbst = kb.sb(st, [128, 128], F32, "mbst")
        kb.dma(U[:], c_U[:, :])
        kb.dma(mbst[:], c_mb_st[:, :])
        xT = kb.sb(st, [128, 2, S], BF16, "xT")
        BT = kb.sb(st, [64, 2, S], BF16, "BT")
        CT = kb.sb(st, [64, 2, S], BF16, "CT")
        xB = kb.sb(st, [128, NT, 384], BF16, "xB")
        z = kb.sb(st, [128, NT, 256], BF16, "z")
        dtr = kb.sb(st, [128, NT, 4], F32, "dtr")
        with contextlib.ExitStack() as s2:
            raw = Ring([kb.sb(s2, [128, S], BF16, "raw%d" % i) for i in range(2)])
            acc = Ring([kb.sb(s2, [128, S], F32, "acc%d" % i) for i in range(2)])
            wc = kb.sb(s2, [128, 6, 4], F32, "wc")
            bc_ = kb.sb(s2, [128, 6], F32, "bc")
            specs = [("cx0", 0, 128, xT, 0), ("cx1", 128, 128, xT, 1), ("cB0", 256, 64, BT, 0), ("cB1", 320, 64, BT, 1),
                     ("cC0", 384, 64, CT, 0), ("cC1", 448, 64, CT, 1)]
            for i, (nm, ch0, rows, dst, di) in enumerate(specs):
                kb.dma(wc[0:rows, i, :], V(ssm_cw, ssm_cw.h[l, :, ch0:ch0 + rows].rearrange("k c -> c k")))
                kb.dma(bc_[0:rows, i:i + 1], V(ssm_cb, ssm_cb.h[l, ch0:ch0 + rows].rearrange("(c o) -> c o", o=1)))
            for i, (nm, ch0, rows, dst, di) in enumerate(specs):
                r = raw.next()
                a = acc.next()
                r0 = F_ROW[nm][0]
                kb.dma(r[0:rows, :], projF[r0:r0 + rows, :], q=("sp" if i % 2 == 0 else "pool"))
                conv_silu(kb, ("dve" if i % 2 == 0 else "pool"), dst[0:rows, di, :], r[0:rows, :], a,
                          wc[0:rows, i, :], bc_[0:rows, i:i + 1], S, rows)
            load_T(kb, z, projT, "cz", S)
            sz = silu_all(kb, s2, z, S, "cz")
        kb.barrier()
        load_Ts(kb, dtr, projTs, "dt", S)
        dtb = kb.sb(st, [128, 4], F32, "dtb")
        aneg = kb.sb(st, [128, 4], F32, "aneg")
        dsk = kb.sb(st, [128, 4], F32, "dsk")
        ngrow = kb.sb(st, [128, 256], F32, "ngrow")
        kb.dma(dtb[:], bcast_rows(ssm_dtb, ssm_dtb.h[l]))
        kb.dma(aneg[:], bcast_rows(ssm_alog, ssm_alog.h[l]))
        kb.dma(dsk[:], bcast_rows(ssm_d, ssm_d.h[l]))
        kb.dma(ngrow[:], bcast_rows(ssm_ng, ssm_ng.h[l]))
        kb.act(aneg[:], aneg[:], AF.Exp)
        kb.ts("dve", aneg[:], aneg[:], -1.0, None, ALU.mult)
        dt = kb.sb(st, [128, NT, 4], F32, "dt")
        adt = kb.sb(st, [128, NT, 4], F32, "adt")
        kb.tt("dve", dt[:], dtr[:], V(dtb, dtb.h[:, :].unsqueeze(1).to_broadcast([128, NT, 4])), ALU.add)
        kb.act(dt[:], dt[:], AF.Exp)
        kb.act(dt[:], dt[:], AF.Ln, bias=1.0)
        kb.tt("dve", adt[:], dt[:], V(aneg, aneg.h[:, :].unsqueeze(1).to_broadcast([128, NT, 4])), ALU.mult)
        acs = kb.sb(st, [128, NT, 4], F32, "acs")
        alast = kb.sb(st, [128, NT, 4], F32, "alast")
        ea = kb.sb(st, [128, NT, 4], F32, "ea")
        de = kb.sb(st, [128, NT, 4], F32, "de")
        cd = kb.sb(st, [128, NT, 4], F32, "cd")
        with contextlib.ExitStack() as s2:
            pa = kb.ps(s2, [128, NT * 4], F32, "pa")
            pb = kb.ps(s2, [128, NT * 4], F32, "pb")
            kb.mm(pa[:], U[:], adt[:].re("p c h -> p (c h)"))
            kb.mm(pb[:], ones_f[:], adt[:].re("p c h -> p (c h)"))
            kb.cp("dve", acs[:].re("p c h -> p (c h)"), pa[:])
            kb.cp("dve", alast[:].re("p c h -> p (c h)"), pb[:])
        kb.barrier()
        kb.act(ea[:], acs[:], AF.Exp)
        kb.act(cd[:], alast[:], AF.Exp)
        kb.tt("dve", de[:], alast[:], acs[:], ALU.subtract)
        kb.act(de[:], de[:], AF.Exp)
        with contextlib.ExitStack() as s2:
            ptx = Ring([kb.ps(s2, [128, 384], BF16, "ptx%d" % i) for i in range(2)])
            for c in range(NT):
                pt = ptx.next()
                kb.tr(pt[:, 0:128], xT[:, 0, c * 128:(c + 1) * 128], identb[:], inc=False)
                kb.tr(pt[:, 128:256], xT[:, 1, c * 128:(c + 1) * 128], identb[:], inc=False)
                kb.tr(pt[:, 256:320], BT[:, 0, c * 128:(c + 1) * 128], identb[0:64, 0:64], inc=False)
                kb.tr(pt[:, 320:384], BT[:, 1, c * 128:(c + 1) * 128], identb[0:64, 0:64], inc=True)
                kb.cp(("act" if c % 2 == 0 else "dve"), xB[:, c, :], pt[:])
        kb.barrier()
        pR = kb.ps(st, [128, 4, 128], F32, "pR")
        pS = kb.ps(st, [128, 2, 128], F32, "pS")
        pY = kb.ps(st, [128, 256], F32, "pY")
        pO = kb.ps(st, [128, 256], F32, "pO")
        pN = kb.ps(st, [64, 4, 64], F32, "pN")
        arg = kb.sb(st, [128, 4, 128], F32, "arg")
        dec = kb.sb(st, [128, 4, 128], F32, "dec")
        GT = kb.sb(st, [128, 4, 128], BF16, "GT")
        xdt = kb.sb(st, [128, 4, 64], BF16, "xdt")
        xdw = kb.sb(st, [128, 4, 64], BF16, "xdw")
        stf = kb.sb(st, [64, 4, 64], F32, "stf")
        stb = kb.sb(st, [64, 4, 64], BF16, "stb")
        yd = kb.sb(st, [128, 256], F32, "yd")
        y = kb.sb(st, [128, 256], F32, "y")
        kb.memset("pool", stf[:], 0.0)
        kb.memset("pool", stb[:], 0.0)
        bufs = (kb.sb(st, [128, 256], F32, "sq"), kb.sb(st, [128, 4], F32, "ssq"),
                kb.sb(st, [128, 4], F32, "rs"), kb.sb(st, [128, 256], BF16, "yo"))
        xdts = [xdt, kb.sb(st, [128, 4, 64], BF16, "xdt_r1")]
        stbs = [stb] + [kb.sb(st, [64, 4, 64], BF16, "stb_r%d" % i) for i in range(2)]

        def stream_state(c):
            xc = V(xB, xB.h[:, c, 0:256].rearrange("p (h d) -> p h d", h=4))
            xd = xdts[c % 2]
            kb.tt("pool", xd[:], xc, V(dt, dt.h[:, c, :].unsqueeze(2).to_broadcast([128, 4, 64])), ALU.mult)
            kb.tt("pool", xdw[:], xd[:], V(de, de.h[:, c, :].unsqueeze(2).to_broadcast([128, 4, 64])), ALU.mult)
            yield
            for h in range(4):
                kb.mm(pN[:, h, :], xB[:, c, 256 + 64 * (h // 2):256 + 64 * (h // 2) + 64], xdw[:, h, :], inc=(h == 3))
            yield
            for h in range(4):
                kb.stt("dve", stf[:, h, :], stf[:, h, :], cd[0:64, c, h:h + 1], pN[:, h, :], ALU.mult, ALU.add)
                if h % 2 == 1:
                    yield
            kb.cp("act", stbs[(c + 1) % 3][:], stf[:])
            yield

        def stream_out(c):
            t0 = c * 128
            xc = V(xB, xB.h[:, c, 0:256].rearrange("p (h d) -> p h d", h=4))
            xd = xdts[c % 2]
            sb_ = stbs[c % 3]
            for h in range(4):
                kb.mm(pR[:, h, :], adt[:, c, h:h + 1].bc([128, 128]), U[:], inc=(h == 3))
            for g in range(2):
                kb.mm(pS[:, g, :], BT[:, g, t0:t0 + 128], CT[:, g, t0:t0 + 128], inc=(g == 1))
            yield
            for h in range(4):
                kb.stt("dve", arg[:, h, :], pR[:, h, :], acs[:, c, h:h + 1], mbst[:], ALU.subtract, ALU.add)
                if h % 2 == 1:
                    yield
            kb.act(dec[:], arg[:], AF.Exp)
            for h in range(4):
                kb.mm(pO[:, h * 64:(h + 1) * 64], CT[:, h // 2, t0:t0 + 128], sb_[:, h, :], inc=(h == 3))
            yield
            for h in range(4):
                kb.tt("dve", GT[:, h, :], pS[:, h // 2, :], dec[:, h, :], ALU.mult)
                if h % 2 == 1:
                    yield
            for h in range(4):
                kb.mm(pY[:, h * 64:(h + 1) * 64], GT[:, h, :], xd[:, h, :], inc=(h == 3))
            yield
            kb.cp("act", yd[:], pY[:])
            yield
            for h in range(4):
                hs = slice(h * 64, (h + 1) * 64)
                kb.stt("dve", y[:, hs], pO[:, hs], ea[:, c, h:h + 1], yd[:, hs], ALU.mult, ALU.add)
                kb.stt("dve", y[:, hs], xc[:, h, :], dsk[:, h:h + 1], y[:, hs], ALU.mult, ALU.add)
                if h % 2 == 1:
                    yield
            kb.tt("pool", y[:], y[:], sz[:, c, :], ALU.mult)
            yield
            head_tail(kb, bufs, y[:], ngrow[:], ones_f[:, 0:1].bc([128, 256]), mix, t0, 512, 1.0, nheads=2, hd=128)
            yield

        interleave([stream_state(0)])
        for c in range(NT):
            gens = [stream_out(c)]
            if c + 1 < NT:
                gens.insert(0, stream_state(c + 1))
            interleave(gens)


def phase_mlstm(kb, nc, l, S, projF, projT, projTs, mix, identb, identf, ones_f, c_U, c_mb_st, c_mb_ts,
                c_sel127, ml_cw, ml_cb, ml_ifb, ml_ng):
    NT = S // 128
    with contextlib.ExitStack() as st:
        U = kb.sb(st, [128, 128], F32, "U")
        mbst = kb.sb(st, [128, 4, 128], F32, "mbst")
        mbts = kb.sb(st, [128, 4, 128], F32, "mbts")
        sel127 = kb.sb(st, [128, 128], F32, "sel127")
        kb.dma(U[:], c_U[:, :])
        kb.dma(sel127[:], c_sel127[:, :])
        for h in range(4):
            kb.dma(mbst[:, h, :], c_mb_st[:, :])
            kb.dma(mbts[:, h, :], c_mb_ts[:, :], q="pool")
        qT = kb.sb(st, [64, 4, S], BF16, "qT")
        kT = kb.sb(st, [64, 4, S], BF16, "kT")
        kTl = kb.sb(st, [128, NT, 4, 64], BF16, "kTl")
        v1 = kb.sb(st, [128, NT, 4, 65], BF16, "v1")
        z = kb.sb(st, [128, NT, 256], BF16, "z")
        og = kb.sb(st, [128, NT, 256], BF16, "og")
        ifr = kb.sb(st, [128, NT, 8], F32, "ifr")
        with contextlib.ExitStack() as s2:
            raw = Ring([kb.sb(s2, [64, S], BF16, "raw%d" % i) for i in range(2)])
            acc = Ring([kb.sb(s2, [64, S], F32, "acc%d" % i) for i in range(2)])
            wc = kb.sb(s2, [64, 8, 4], F32, "wc")
            bc_ = kb.sb(s2, [64, 8], F32, "bc")
            for i in range(8):
                ch0 = i * 64
                kb.dma(wc[:, i, :], V(ml_cw, ml_cw.h[l, :, ch0:ch0 + 64].rearrange("k c -> c k")))
                kb.dma(bc_[:, i:i + 1], V(ml_cb, ml_cb.h[l, ch0:ch0 + 64].rearrange("(c o) -> c o", o=1)))
            for i in range(8):
                nm = ("dq%d" % i) if i < 4 else ("dk%d" % (i - 4))
                dst = qT if i < 4 else kT
                r = raw.next()
                a = acc.next()
                r0 = F_ROW[nm][0]
                kb.dma(r[:], projF[r0:r0 + 64, :], q=("sp" if i % 2 == 0 else "pool"))
                conv_silu(kb, ("dve" if i % 2 == 0 else "pool"), dst[:, i % 4, :], r[:], a, wc[:, i, :], bc_[:, i:i + 1], S, 64)
        kb.barrier()
        with contextlib.ExitStack() as s2:
            vtmp = kb.sb(s2, [128, NT, 256], BF16, "vtmp")
            load_T(kb, vtmp, projT, "dv", S)
            load_T(kb, z, projT, "dz", S, q="pool")
            load_T(kb, og, projT, "do", S)
            kb.memset("pool", V(v1, v1.h[:, :, :, 64:65]), 1.0)
            kb.cp("pool", V(v1, v1.h[:, :, :, 0:64]), vtmp[:].re("p c (h d) -> p c h d", h=4))
            sz = silu_all(kb, s2, z, S, "dz")
            kb.act(og[:], og[:], AF.Sigmoid)
        kb.barrier()
        load_Ts(kb, ifr, projTs, "dif", S)
        ifb = kb.sb(st, [128, 8], F32, "ifb")
        ngrow = kb.sb(st, [128, 4, 64], F32, "ngrow")
        kb.dma(ifb[:], bcast_rows(ml_ifb, ml_ifb.h[l]))
        kb.dma(ngrow[:, 0, :], bcast_rows(ml_ng, ml_ng.h[l]))
        for h in range(1, 4):
            kb.cp("pool", ngrow[:, h, :], ngrow[:, 0, :])
        kb.tt("dve", ifr[:], ifr[:], V(ifb, ifb.h[:, :].unsqueeze(1).to_broadcast([128, NT, 8])), ALU.add)
        ig = V(ifr, ifr.h[:, :, 0:4])
        lf = kb.sb(st, [128, NT, 4], F32, "lf")
        kb.act(lf[:], V(ifr, ifr.h[:, :, 4:8]), AF.Exp, scale=-1.0)
        kb.act(lf[:], lf[:], AF.Ln, bias=1.0)
        kb.ts("dve", lf[:], lf[:], -1.0, None, ALU.mult)
        b = kb.sb(st, [128, NT, 4], F32, "b")
        blast = kb.sb(st, [128, NT, 4], F32, "blast")
        u = kb.sb(st, [128, NT, 4], F32, "u")
        with contextlib.ExitStack() as s2:
            pa = kb.ps(s2, [128, NT * 4], F32, "pa")
            pb = kb.ps(s2, [128, NT * 4], F32, "pb")
            kb.mm(pa[:], U[:], lf[:].re("p c h -> p (c h)"))
            kb.mm(pb[:], ones_f[:], lf[:].re("p c h -> p (c h)"))
            kb.cp("dve", b[:].re("p c h -> p (c h)"), pa[:])
            kb.cp("dve", blast[:].re("p c h -> p (c h)"), pb[:])
        kb.barrier()
        kb.tt("dve", u[:], ig, b[:], ALU.subtract)
        with contextlib.ExitStack() as s2:
            ptk = Ring([kb.ps(s2, [128, 4, 64], BF16, "ptk%d" % i) for i in range(2)])
            for c in range(NT):
                pt = ptk.next()
                for h in range(4):
                    kb.tr(pt[:, h, :], kT[:, h, c * 128:(c + 1) * 128], identb[0:64, 0:64], inc=(h == 3))
                kb.cp(("act" if c % 2 == 0 else "dve"), kTl[:, c, :, :], pt[:])
        kb.barrier()
        pM = kb.ps(st, [128, 4, 128], F32, "pM")
        pW = kb.ps(st, [128, 4, 128], F32, "pW")
        pSC = kb.ps(st, [128, 4, 128], F32, "pSC")
        pND = kb.ps(st, [128, 4, 65], F32, "pND")
        pIN = kb.ps(st, [128, 4, 65], F32, "pIN")
        pL = kb.ps(st, [64, 4, 65], F32, "pL")
        pU = kb.ps(st, [128, 4], F32, "pU")
        cmx = kb.sb(st, [128, 4], F32, "cmx")
        umax = kb.sb(st, [128, 4], F32, "umax")
        mprev = kb.sb(st, [128, 4], F32, "mprev")
        tmp = kb.sb(st, [128, 4], F32, "tmp")
        ntmp = kb.sb(st, [128, 4], F32, "ntmp")
        mt = kb.sb(st, [128, 4], F32, "mt")
        emt = kb.sb(st, [128, 4], F32, "emt")
        wint = kb.sb(st, [128, 4], F32, "wint")
        wend = kb.sb(st, [128, 4], F32, "wend")
        mm_ = kb.sb(st, [128, 4], F32, "mm")
        aprev = kb.sb(st, [128, 4], F32, "aprev")
        aloc = kb.sb(st, [128, 4], F32, "aloc")
        wT = kb.sb(st, [128, 4, 128], F32, "wT")
        sqk = kb.sb(st, [128, 4, 128], BF16, "sqk")
        nds = kb.sb(st, [128, 4, 65], F32, "nds")
        nd = kb.sb(st, [128, 4, 65], F32, "nd")
        dn = kb.sb(st, [128, 4], F32, "dn")
        hh_ = kb.sb(st, [128, 256], F32, "hh")
        kw = kb.sb(st, [128, 4, 64], BF16, "kw")
        cnf = kb.sb(st, [64, 4, 65], F32, "cnf")
        cnb = kb.sb(st, [64, 4, 65], BF16, "cnb")
        ltmp = kb.sb(st, [64, 4, 65], F32, "ltmp")
        kb.memset("pool", cnf[:], 0.0)
        kb.memset("pool", cnb[:], 0.0)
        kb.memset("pool", mprev[:], 0.0)
        bufs = (kb.sb(st, [128, 256], F32, "sq"), kb.sb(st, [128, 4], F32, "ssq"),
                kb.sb(st, [128, 4], F32, "rs"), kb.sb(st, [128, 256], BF16, "yo"))
        tmps = [kb.sb(st, [128, 4], F32, "tmp_r%d" % i) for i in range(2)]
        ntmps = [kb.sb(st, [128, 4], F32, "ntmp_r%d" % i) for i in range(2)]
        emts = [kb.sb(st, [128, 4], F32, "emt_r%d" % i) for i in range(2)]
        wints = [kb.sb(st, [128, 4], F32, "wint_r%d" % i) for i in range(2)]
        cnbs = [cnb] + [kb.sb(st, [64, 4, 65], BF16, "cnb_r%d" % i) for i in range(2)]

        def stream_state(c):
            tmp, ntmp, emt, wint = tmps[c % 2], ntmps[c % 2], emts[c % 2], wints[c % 2]
            for h in range(4):
                kb.mm(pM[:, h, :], u[:, c, h:h + 1].bc([128, 128]), identf[:], start=(h == 0), stop=False, inc=False)
            kb.mm(pM[:].re("p h s -> p (h s)"), identf[:], mbts[:].re("p h s -> p (h s)"), start=False, stop=True)
            yield
            kb.red(cmx[:], pM[:], ALU.max)
            kb.mm(pU[:], sel127[:], cmx[:])
            yield
            kb.cp("dve", umax[:], pU[:])
            kb.tt("dve", tmp[:], cmx[:], mprev[:], ALU.max)
            kb.ts("dve", ntmp[:], tmp[:], -1.0, None, ALU.mult)
            yield
            kb.tt("dve", mt[:], tmp[:], b[:, c, :], ALU.add)
            kb.act(emt[:], mt[:], AF.Exp, scale=-1.0)
            kb.tt("dve", wint[:], mprev[:], tmp[:], ALU.subtract)
            kb.act(wint[:], wint[:], AF.Exp)
            yield
            kb.tt("dve", wend[:], u[:, c, :], umax[:], ALU.subtract)
            kb.act(wend[:], wend[:], AF.Exp)
            kb.ts("dve", wend[:], wend[:], 0.125, None, ALU.mult)
            yield
            kb.tt("dve", mm_[:], mprev[:], umax[:], ALU.max)
            kb.tt("dve", aprev[:], mprev[:], mm_[:], ALU.subtract)
            kb.act(aprev[:], aprev[:], AF.Exp)
            kb.tt("dve", aloc[:], umax[:], mm_[:], ALU.subtract)
            kb.act(aloc[:], aloc[:], AF.Exp)
            yield
            kb.tt("pool", kw[:], kTl[:, c, :, :], V(wend, wend.h[:, :].unsqueeze(2).to_broadcast([128, 4, 64])), ALU.mult)
            for h in range(4):
                kb.mm(pL[:, h, :], kw[:, h, :], v1[:, c, h, :], inc=(h == 3))
            yield
            for h in range(4):
                kb.ts("dve", ltmp[:, h, :], pL[:, h, :], aloc[0:64, h:h + 1], None, ALU.mult)
                kb.stt("dve", cnf[:, h, :], cnf[:, h, :], aprev[0:64, h:h + 1], ltmp[:, h, :], ALU.mult, ALU.add)
                if h % 2 == 1:
                    yield
            kb.cp("act", cnbs[(c + 1) % 3][:], cnf[:])
            kb.tt("dve", mprev[:], mm_[:], blast[:, c, :], ALU.add)
            yield

        def stream_out(c):
            t0 = c * 128
            ntmp, emt, wint = ntmps[c % 2], emts[c % 2], wints[c % 2]
            cn = cnbs[c % 3]
            for h in range(4):
                kb.mm(pW[:, h, :], ntmp[:, h:h + 1].bc([128, 128]), identf[:], start=(h == 0), stop=False, inc=False)
            kb.mm(pW[:].re("p h s -> p (h s)"), identf[:], mbst[:].re("p h s -> p (h s)"), start=False, stop=True)
            yield
            for h in range(4):
                kb.mm(pSC[:, h, :], kT[:, h, t0:t0 + 128], qT[:, h, t0:t0 + 128], inc=(h == 3))
            yield
            for h in range(4):
                kb.act(wT[:, h, :], pW[:, h, :], AF.Exp, bias=u[:, c, h:h + 1])
                if h % 2 == 1:
                    yield
            kb.stt("dve", sqk[:], pSC[:], 0.125, wT[:], ALU.mult, ALU.mult)
            yield
            for h in range(4):
                kb.mm(pND[:, h, :], sqk[:, h, :], v1[:, c, h, :], inc=(h == 3))
            for h in range(4):
                kb.mm(pIN[:, h, :], qT[:, h, t0:t0 + 128], cn[:, h, :], inc=(h == 3))
            yield
            kb.cp("act", nds[:], pND[:])
            yield
            for h in range(4):
                kb.stt("dve", nd[:, h, :], pIN[:, h, :], wint[:, h:h + 1], nds[:, h, :], ALU.mult, ALU.add)
                if h % 2 == 1:
                    yield
            kb.ts("dve", dn[:], nd[:, :, 64], -1.0, None, ALU.mult)
            kb.tt("dve", dn[:], dn[:], nd[:, :, 64], ALU.max)
            kb.tt("dve", dn[:], dn[:], emt[:], ALU.max)
            kb.recip(dn[:], dn[:])
            yield
            for h in range(4):
                kb.ts("dve", hh_[:, h * 64:(h + 1) * 64], nd[:, h, 0:64], dn[:, h:h + 1], None, ALU.mult)
            kb.tt("pool", hh_[:], hh_[:], og[:, c, :], ALU.mult)
            yield
            head_tail(kb, bufs, hh_[:], ngrow[:].re("p h d -> p (h d)"), sz[:, c, :], mix, t0, 768, 1.0)
            yield

        interleave([stream_state(0)])
        for c in range(NT):
            gens = [stream_out(c)]
            if c + 1 < NT:
                gens.insert(0, stream_state(c + 1))
            interleave(gens)


def phase_out(kb, nc, l, S, x_src, x_dst, mix, w_out, gate_row, fin_row, identb, last):
    NT = S // 128
    with contextlib.ExitStack() as st:
        wo = kb.sb(st, [128, 8, D], BF16, "wo")
        stg = Ring([kb.sb(st, [128, D], F32, "wstg%d" % i) for i in range(2)])
        for k in range(8):
            sg = stg.next()
            kb.dma(sg[:], w_out[l, k * 128:(k + 1) * 128, :], q=("sp" if k % 2 == 0 else "pool"))
            kb.cp(("dve" if k % 2 == 0 else "act"), wo[:, k, :], sg[:])
        mixr = Ring([kb.sb(st, [128, D], BF16, "mixt%d" % i) for i in range(2)])
        xr = Ring([kb.sb(st, [128, D], F32, "xt%d" % i) for i in range(2)])
        mT = Ring([kb.sb(st, [128, 8, 128], BF16, "mT%d" % i) for i in range(2)])
        yr = Ring([kb.sb(st, [128, D], F32, "y%d" % i) for i in range(2)])
        junk = kb.sb(st, [128, D], BF16, "junk")
        ss = kb.sb(st, [128, 1], F32, "ss")
        ptr = Ring([kb.ps(st, [128, 8, 128], BF16, "ptr%d" % i) for i in range(2)])
        py = Ring([kb.ps(st, [128, 512], F32, "py%d" % i) for i in range(4)])
        def load_t(t):
            m_ = mixr.next()
            x_ = xr.next()
            kb.dma(m_[:], mix[t * 128:(t + 1) * 128, :], q="pool")
            kb.dma(x_[:], x_src[t * 128:(t + 1) * 128, :], q="pool")
            return m_, x_
        nxt = load_t(0)
        for t in range(NT):
            t0 = t * 128
            mt_, xt = nxt
            if t + 1 < NT:
                nxt = load_t(t + 1)
            pt = ptr.next()
            for k in range(8):
                kb.tr(pt[:, k, :], mt_[:, k * 128:(k + 1) * 128], identb[:], inc=(k == 7))
            m = mT.next()
            kb.cp("act", m[:], pt[:])
            y = yr.next()
            for half in range(2):
                p = py.next()
                for k in range(8):
                    kb.mm(p[:], m[:, k, :], wo[:, k, half * 512:(half + 1) * 512], start=(k == 0), stop=(k == 7), inc=(k == 7))
                hs = slice(half * 512, (half + 1) * 512)
                kb.tt("dve", y[:, hs], p[:], gate_row[:, l, hs], ALU.mult)
                kb.tt("pool", y[:, hs], y[:, hs], xt[:, hs], ALU.add)
            if last:
                kb.act(junk[:], y[:], AF.Square, accum=ss[:])
                kb.rsqrt(ss[:], ss[:], 1.0 / D)
                kb.stt("dve", y[:], y[:], ss[:, 0:1], fin_row[:], ALU.mult, ALU.mult)
            kb.dma(x_dst[t0:t0 + 128, :], y[:], q="sp", disjoint=True)


def make_consts(S):
    NCMP = (S - 32) // 16 + 1
    NCC = (NCMP + 127) // 128
    bf = ml_dtypes.bfloat16
    k = np.arange(128)
    tri = (k[:, None] <= k[None, :]).astype(np.float32)
    c = {}
    c["c_identb"] = np.eye(128, dtype=np.float32).astype(bf)
    c["c_identf"] = np.eye(128, dtype=np.float32)
    c["c_tri4"] = np.tile(tri, (1, 4)).astype(bf)
    c["c_atri4"] = np.tile(1.0 - tri, (1, 4)).astype(bf)
    c["c_U"] = tri.copy()
    c["c_mb_st"] = ((1.0 - tri) * NEGB).astype(np.float32)
    c["c_mb_ts"] = np.ascontiguousarray(c["c_mb_st"].T)
    s127 = np.zeros((128, 128), np.float32)
    s127[127, :] = 1.0
    c["c_sel127"] = s127
    E = np.zeros((64, S), np.float32)
    keys = np.arange(S)
    E[keys // 64 % 64, keys] = 30000.0 * (keys // 64 < 64)
    c["c_E"] = E.astype(bf)
    n_sel = S // 64
    cmp_idx = np.arange(NCMP)[:, None] * 16 + np.arange(32)[None, :]
    sel_start = np.arange(n_sel) * 64
    overlap = np.clip(np.minimum(cmp_idx[:, -1:] + 1, sel_start[None, :] + 64)
                      - np.maximum(cmp_idx[:, :1], sel_start[None, :]), 0, None)
    c2s = np.zeros((NCC * 128, 65), np.float32)
    c2s[:NCMP, :n_sel] = overlap / 32.0
    c2s[:NCMP, 64] = 1.0
    c["c_c2s"] = c2s.astype(bf)
    cm = np.zeros((NCC * 128, S), np.float32)
    cm[:NCMP] = (cmp_idx[:, -1][:, None] <= keys[None, :]).astype(np.float32)
    c["c_cmask"] = cm.astype(bf)
    t = np.arange(S)
    cur = t // 64
    sid = np.arange(64)
    forced = (sid[None, :] == cur[:, None]) | (sid[None, :] == 0)
    allowed = (sid[None, :] <= cur[:, None]) & (sid[None, :] < n_sel)
    c["c_selmul"] = (allowed & ~forced).astype(np.float32)
    c["c_seladd"] = np.where(forced, 1e4, np.where(allowed, 0.0, -1.0)).astype(np.float32)
    return c


_NC_CACHE = {}


def run(inputs, S, DEPTH, ncores, debug=False, stop=99):
    key = (S, DEPTH, debug, stop)
    if key not in _NC_CACHE:
        _NC_CACHE[key] = build(S, DEPTH, debug, stop)
    nc = _NC_CACHE[key]
    consts = make_consts(S)
    f32 = np.float32
    w_in = np.asarray(inputs["w_in"], f32)
    shared = dict(consts)
    shared["w_inF"] = np.ascontiguousarray(w_in[:, :, F_COLS])
    shared["w_inT"] = np.ascontiguousarray(w_in[:, :, T_COLS])
    for nm in ("norm_g", "ada_w", "ada_b", "w_out", "nsa_cmp_pos", "nsa_ck_w1", "nsa_ck_w2", "nsa_cv_w1",
               "nsa_cv_w2", "nsa_norm_g", "diff_norm_g", "ssm_conv_w", "ssm_conv_b", "ssm_dt_bias",
               "ssm_a_log", "ssm_d", "ssm_norm_g", "ml_conv_w", "ml_conv_b", "ml_if_b", "ml_norm_g", "final_g"):
        shared[nm] = np.ascontiguousarray(np.asarray(inputs[nm], f32))
    shared["diff_lam"] = np.ascontiguousarray(np.asarray(inputs["diff_lam"], f32).reshape(DEPTH, 128))
    x = np.asarray(inputs["x"], f32)
    c = np.asarray(inputs["c"], f32)
    in_maps = []
    for i in range(ncores):
        m = dict(shared)
        m["x"] = np.ascontiguousarray(x[i])
        m["c"] = np.ascontiguousarray(c[i])
        in_maps.append(m)
    res = run_bass_kernel_spmd(nc, in_maps, core_ids=list(range(ncores)))
    return res


def kernel(**inputs):
    res = run(inputs, 4096, 2, 8)
    return np.stack([np.asarray(r["out"], np.float32) for r in res.results], axis=0)
```

```python
import contextlib
import math
import numpy as np
import ml_dtypes
import concourse.bass as bass
import concourse.mybir as mybir
from concourse.bass_utils import run_bass_kernel_spmd

F32 = mybir.dt.float32
BF16 = mybir.dt.bfloat16
AF = mybir.ActivationFunctionType
ALU = mybir.AluOpType
AX = mybir.AxisListType

D = 1024
NEGB = -30000.0
EPS = 1e-6

F_GROUPS = []
for h in range(4):
    F_GROUPS.append(("aq%d" % h, 0 + 64 * h, 64))
F_GROUPS += [("akc", 256, 64), ("avc", 320, 64), ("aks", 384, 64), ("akw", 512, 64)]
for h in range(4):
    F_GROUPS.append(("bq%d" % h, 908 + 64 * h, 64))
for h in range(4):
    F_GROUPS.append(("bk%d" % h, 1164 + 64 * h, 64))
F_GROUPS += [("cx0", 2188, 128), ("cx1", 2316, 128), ("cB0", 2444, 64), ("cB1", 2508, 64),
             ("cC0", 2572, 64), ("cC1", 2636, 64)]
for h in range(4):
    F_GROUPS.append(("dq%d" % h, 2704 + 64 * h, 64))
for h in range(4):
    F_GROUPS.append(("dk%d" % h, 2960 + 64 * h, 64))
F_ROW = {}
_r = 0
F_COLS = []
for (n_, c0_, w_) in F_GROUPS:
    F_ROW[n_] = (_r, w_)
    F_COLS += list(range(c0_, c0_ + w_))
    _r += w_
NF = _r
T_PARTS = [("vs", 448, 64), ("vw", 576, 64), ("ag", 640, 12), ("az", 652, 256),
           ("bv", 1420, 256), ("bz", 1676, 256), ("cz", 1932, 256), ("dt", 2700, 4),
           ("dv", 3216, 256), ("dif", 3472, 8), ("do", 3480, 256), ("dz", 3736, 256)]
T_COL = {}
_r = 0
T_COLS = []
for (n_, c0_, w_) in T_PARTS:
    T_COL[n_] = (_r, w_)
    T_COLS += list(range(c0_, c0_ + w_))
    _r += w_
NTC = _r
TS_COL = {"ag": (0, 12), "dt": (12, 4), "dif": (16, 8)}


class TT:
    def __init__(self, h, name=""):
        self.h = h
        self.w = {}
        self.r = {}
        self.name = name

    def __getitem__(self, key):
        return V(self, self.h[key])


class V:
    def __init__(self, t, ap):
        self.t = t
        self.ap = ap

    def __getitem__(self, key):
        return V(self.t, self.ap[key])

    def re(self, pat, **kw):
        return V(self.t, self.ap.rearrange(pat, **kw))

    def bc(self, shape):
        return V(self.t, self.ap.to_broadcast(list(shape)))

    def raw(self, fn):
        return V(self.t, fn(self.ap))


class Eng:
    def __init__(self, name, h, sem, key, is_pe=False):
        self.name = name
        self.h = h
        self.sem = sem
        self.key = key
        self.count = 0
        self.seen = {}
        self.is_pe = is_pe
        self.pend = False
        self.dsems = []
        self.dlast = []
        self.dnext = 0


class KB:
    def __init__(self, nc, es):
        self.nc = nc
        self.es = es
        self.sems = {}
        self.eng = {}
        for name, h, pe in (("pe", nc.tensor, True), ("act", nc.scalar, False),
                            ("dve", nc.vector, False), ("pool", nc.gpsimd, False),
                            ("sp", nc.sync, False)):
            s = es.enter_context(nc.semaphore("sem_" + name))
            self.sems[name] = s
            self.eng[name] = Eng(name, h, s, name, pe)
        for q, n in (("sp", 28), ("pool", 10), ("act", 6)):
            E = self.eng[q]
            for i in range(n):
                key = "d_%s_%d" % (q, i)
                s = es.enter_context(nc.semaphore(key))
                self.sems[key] = s
                E.dsems.append(key)
                E.dlast.append(0)
        self.bar = es.enter_context(nc.semaphore("barrier"))
        self.sems["bar"] = self.bar
        self.barcount = 0
        self.uid = 0

    def sb(self, st, shape, dt, name):
        self.uid += 1
        h = st.enter_context(self.nc.sbuf_tensor("%s_%d" % (name, self.uid), list(shape), dt))
        return TT(h, name)

    def ps(self, st, shape, dt, name):
        self.uid += 1
        esz = 4 if dt == F32 else 2
        n = 1
        for d_ in shape[1:]:
            n *= d_
        per_bank = 2048 // esz
        full = ((n + per_bank - 1) // per_bank) * per_bank
        h = st.enter_context(self.nc.psum_tensor("%s_%d" % (name, self.uid), [128, full], dt))
        ap = h[0:shape[0], 0:n]
        if len(shape) == 3:
            ap = ap.rearrange("p (a b) -> p a b", a=shape[1])
        t = TT(None, name)
        t.h = ap
        return t

    def sub(self, v):
        t = TT(None, v.t.name + "_sub")
        t.h = v.ap
        return t

    def _waits(self, E, outs, ins, disjoint=False):
        need = {}

        def add(d, own_ok):
            for k, val in d.items():
                if k == E.key and not own_ok:
                    continue
                if need.get(k, 0) < val:
                    need[k] = val
        for v in ins:
            add(v.t.w, True)
        for v in outs:
            if not disjoint:
                add(v.t.w, True)
                add(v.t.r, True)
        for k, val in need.items():
            if E.is_pe and k == E.key:
                continue
            if E.seen.get(k, 0) >= val:
                continue
            E.h.wait_ge(self.sems[k], val)
            E.seen[k] = val

    def _record(self, ev, outs, ins, disjoint=False):
        k, val = ev
        for v in ins:
            if v.t.r.get(k, 0) < val:
                v.t.r[k] = val
        for v in outs:
            if disjoint:
                v.t.w[k] = max(v.t.w.get(k, 0), val)
            else:
                v.t.w = {k: val}
                v.t.r = {}

    def op(self, eng, fn, outs, ins, inc=True, disjoint=False):
        E = self.eng[eng]
        self._waits(E, outs, ins, disjoint)
        inst = fn(E.h)
        if inc:
            E.count += 1
            inst.then_inc(E.sem, 1)
            ev = (E.key, E.count)
            E.pend = False
        else:
            ev = (E.key, E.count + 1)
            E.pend = True
        self._record(ev, outs, ins, disjoint)

    def dma(self, out, in_, q="sp", disjoint=False):
        E = self.eng[q]
        i = E.dnext % len(E.dsems)
        E.dnext += 1
        key = E.dsems[i]
        if E.dlast[i] and E.seen.get(key, 0) < E.dlast[i]:
            E.h.wait_ge(self.sems[key], E.dlast[i])
            E.seen[key] = E.dlast[i]
        self._waits(E, [out], [in_], disjoint)
        E.h.dma_start(out=out.ap, in_=in_.ap).then_inc(self.sems[key], 16)
        E.dlast[i] += 16
        self._record((key, E.dlast[i]), [out], [in_], disjoint)

    def barrier(self):
        sp = self.eng["sp"]
        assert not self.eng["pe"].pend
        for q in ("sp", "pool", "act"):
            E = self.eng[q]
            for i, key in enumerate(E.dsems):
                if E.dlast[i] and sp.seen.get(key, 0) < E.dlast[i]:
                    sp.h.wait_ge(self.sems[key], E.dlast[i])
                    sp.seen[key] = E.dlast[i]
        for n in ("pe", "act", "dve", "pool"):
            E = self.eng[n]
            if E.count and sp.seen.get(n, 0) < E.count:
                sp.h.wait_ge(E.sem, E.count)
                sp.seen[n] = E.count
        self.barcount += 1
        sp.h.sem_inc(self.bar, 1)
        for n in ("pe", "act", "dve", "pool"):
            E = self.eng[n]
            E.h.wait_ge(self.bar, self.barcount)
        for n, E in self.eng.items():
            for m, E2 in self.eng.items():
                if m != "sp":
                    E.seen[m] = E2.count
            for q in ("sp", "pool", "act"):
                Eq = self.eng[q]
                for i, key in enumerate(Eq.dsems):
                    E.seen[key] = Eq.dlast[i]

    def mm(self, out, lhsT, rhs, start=True, stop=True, inc=True):
        self.op("pe", lambda e: e.matmul(out.ap, lhsT.ap, rhs.ap, start=start, stop=stop),
                [out], [lhsT, rhs], inc=inc)

    def tr(self, out, in_, ident, inc=True):
        self.op("pe", lambda e: e.transpose(out.ap, in_.ap, ident.ap), [out], [in_, ident], inc=inc)

    def act(self, out, in_, func, bias=None, scale=1.0, accum=None):
        ins = [in_]
        outs = [out]
        kw = {}
        if isinstance(bias, V):
            ins.append(bias)
            kw["bias"] = bias.ap
        elif bias is not None:
            kw["bias"] = bias
        if isinstance(scale, V):
            ins.append(scale)
            kw["scale"] = scale.ap
        else:
            kw["scale"] = scale
        if accum is not None:
            outs.append(accum)
            kw["accum_out"] = accum.ap
        self.op("act", lambda e: e.activation(out.ap, in_.ap, func, **kw), outs, ins)

    def ts(self, eng, out, in0, s1, s2, op0, op1=None):
        ins = [in0]
        a1 = s1
        a2 = s2
        if isinstance(s1, V):
            ins.append(s1)
            a1 = s1.ap
        if isinstance(s2, V):
            ins.append(s2)
            a2 = s2.ap
        if op1 is None:
            self.op(eng, lambda e: e.tensor_scalar(out.ap, in0.ap, a1, None, op0), [out], ins)
        else:
            self.op(eng, lambda e: e.tensor_scalar(out.ap, in0.ap, a1, a2, op0, op1), [out], ins)

    def tt(self, eng, out, in0, in1, op):
        self.op(eng, lambda e: e.tensor_tensor(out.ap, in0.ap, in1.ap, op), [out], [in0, in1])

    def stt(self, eng, out, in0, s, in1, op0, op1):
        ins = [in0, in1]
        a = s
        if isinstance(s, V):
            ins.append(s)
            a = s.ap
        self.op(eng, lambda e: e.scalar_tensor_tensor(out.ap, in0.ap, a, in1.ap, op0, op1), [out], ins)

    def cp(self, eng, out, in_):
        if eng == "act":
            self.op("act", lambda e: e.copy(out.ap, in_.ap), [out], [in_])
        else:
            self.op(eng, lambda e: e.tensor_copy(out.ap, in_.ap), [out], [in_])

    def memset(self, eng, out, val):
        self.op(eng, lambda e: e.memset(out.ap, val), [out], [])

    def red(self, out, in_, op):
        self.op("dve", lambda e: e.tensor_reduce(out.ap, in_.ap, AX.X, op), [out], [in_])

    def rsqrt(self, out, in_, scale, post=1.0):
        n = out.ap.shape[0]
        self.act(out, in_, AF.Ln, bias=self.eps_col[0:n, 0:1], scale=scale)
        self.act(out, out, AF.Exp, scale=-0.5)
        if post != 1.0:
            self.ts("dve", out, out, float(post), None, ALU.mult)

    def recip(self, out, in_):
        self.op("dve", lambda e: e.reciprocal(out.ap, in_.ap), [out], [in_])


class Deferred:
    def __init__(self):
        self.q = []

    def push(self, fn, delay):
        e = [delay, fn]
        self.q.append(e)
        return e

    def force(self, e):
        for i, x in enumerate(self.q):
            if x is e:
                del self.q[i]
                e[1]()
                return

    def step(self):
        ready = [e for e in self.q if e[0] <= 0]
        self.q = [e for e in self.q if e[0] > 0]
        for e in self.q:
            e[0] -= 1
        for e in ready:
            e[1]()

    def run_through(self, e):
        while any(x is e for x in self.q):
            x = self.q.pop(0)
            x[1]()

    def flush(self):
        while self.q:
            self.step()


def interleave(gens):
    gens = list(gens)
    while gens:
        for g in list(gens):
            try:
                next(g)
            except StopIteration:
                gens.remove(g)


class Ring:
    def __init__(self, tiles):
        self.tiles = tiles
        self.i = 0

    def next(self):
        t = self.tiles[self.i % len(self.tiles)]
        self.i += 1
        return t


def build(S, DEPTH, debug=False, stop=99):
    NT = S // 128
    NG = S // 512
    NCMP = (S - 32) // 16 + 1
    NCC = (NCMP + 127) // 128
    nc = bass.Bass("TRN2", target_bir_lowering=False)

    def din(name, shape, dt=F32):
        return TT(nc.dram_tensor(name, list(shape), dt, kind="ExternalInput").ap(), name)

    def dscr(name, shape, dt):
        return TT(nc.dram_tensor(name, list(shape), dt, kind="Internal").ap(), name)

    x_in = din("x", [S, D])
    c_in = din("c", [D])
    norm_g = din("norm_g", [DEPTH, D])
    ada_w = din("ada_w", [DEPTH, D, 3 * D])
    ada_b = din("ada_b", [DEPTH, 3 * D])
    w_inF = din("w_inF", [DEPTH, D, NF])
    w_inT = din("w_inT", [DEPTH, D, NTC])
    w_out = din("w_out", [DEPTH, D, D])
    cmp_pos = din("nsa_cmp_pos", [DEPTH, 32, 64])
    ck_w1 = din("nsa_ck_w1", [DEPTH, 2048, 128])
    ck_w2 = din("nsa_ck_w2", [DEPTH, 128, 64])
    cv_w1 = din("nsa_cv_w1", [DEPTH, 2048, 128])
    cv_w2 = din("nsa_cv_w2", [DEPTH, 128, 64])
    nsa_ng = din("nsa_norm_g", [DEPTH, 64])
    diff_lam = din("diff_lam", [DEPTH, 128])
    diff_ng = din("diff_norm_g", [DEPTH, 64])
    ssm_cw = din("ssm_conv_w", [DEPTH, 4, 512])
    ssm_cb = din("ssm_conv_b", [DEPTH, 512])
    ssm_dtb = din("ssm_dt_bias", [DEPTH, 4])
    ssm_alog = din("ssm_a_log", [DEPTH, 4])
    ssm_d = din("ssm_d", [DEPTH, 4])
    ssm_ng = din("ssm_norm_g", [DEPTH, 256])
    ml_cw = din("ml_conv_w", [DEPTH, 4, 512])
    ml_cb = din("ml_conv_b", [DEPTH, 512])
    ml_ifb = din("ml_if_b", [DEPTH, 8])
    ml_ng = din("ml_norm_g", [DEPTH, 64])
    final_g = din("final_g", [D])
    c_identb = din("c_identb", [128, 128], BF16)
    c_identf = din("c_identf", [128, 128])
    c_tri4 = din("c_tri4", [128, 512], BF16)
    c_atri4 = din("c_atri4", [128, 512], BF16)
    c_U = din("c_U", [128, 128])
    c_mb_st = din("c_mb_st", [128, 128])
    c_mb_ts = din("c_mb_ts", [128, 128])
    c_sel127 = din("c_sel127", [128, 128])
    c_E = din("c_E", [64, S], BF16)
    c_c2s = din("c_c2s", [NCC * 128, 65], BF16)
    c_cmask = din("c_cmask", [NCC * 128, S], BF16)
    c_selmul = din("c_selmul", [S, 64])
    c_seladd = din("c_seladd", [S, 64])

    out_d = TT(nc.dram_tensor("out", [S, D], F32, kind="ExternalOutput").ap(), "out")
    xres = dscr("xres", [S, D], F32)
    if debug:
        projF = TT(nc.dram_tensor("projF", [NF, S], BF16, kind="ExternalOutput").ap(), "projF")
        projT = TT(nc.dram_tensor("projT", [S, NTC], BF16, kind="ExternalOutput").ap(), "projT")
        projTs = TT(nc.dram_tensor("projTs", [S, 24], F32, kind="ExternalOutput").ap(), "projTs")
        dbg = TT(nc.dram_tensor("dbg", [128, DEPTH * 24], F32, kind="ExternalOutput").ap(), "dbg")
    else:
        projF = dscr("projF", [NF, S], BF16)
        projT = dscr("projT", [S, NTC], BF16)
        projTs = dscr("projTs", [S, 24], F32)
    if debug:
        mix = TT(nc.dram_tensor("mix", [S, D], BF16, kind="ExternalOutput").ap(), "mix")
    else:
        mix = dscr("mix", [S, D], BF16)

    es = contextlib.ExitStack()
    with es:
        es.enter_context(nc.allow_non_contiguous_dma("small strided parameter loads"))
        es.enter_context(nc.allow_low_precision("bf16 matmul operands, fp32 accumulation"))
        kb = KB(nc, es)
        identb = kb.sb(es, [128, 128], BF16, "identb")
        identf = kb.sb(es, [128, 128], F32, "identf")
        ones_f = kb.sb(es, [128, 128], F32, "onesf")
        kb.dma(identb[:], c_identb[:, :])
        kb.dma(identf[:], c_identf[:, :])
        kb.memset("pool", ones_f[:], 1.0)
        kb.eps_col = kb.sb(es, [128, 1], F32, "epscol")
        kb.memset("pool", kb.eps_col[:], EPS)
        modc = kb.sb(es, [128, DEPTH, 24], F32, "modc")
        Acoef = kb.sb(es, [128, DEPTH, 8], F32, "Acoef")
        gate_row = kb.sb(es, [128, DEPTH, D], F32, "gate_row")
        fin_row = kb.sb(es, [128, D], F32, "fin_row")
        kb.dma(fin_row[:], V(final_g, final_g.h.partition_broadcast(128)))

        with contextlib.ExitStack() as st:
            cact = kb.sb(st, [128, 8], F32, "cact")
            kb.dma(cact[:], V(c_in, c_in.h.rearrange("(k p) -> p k", p=128)))
            csig = kb.sb(st, [128, 8], F32, "csig")
            kb.act(csig[:], cact[:], AF.Sigmoid)
            kb.tt("dve", cact[:], cact[:], csig[:], ALU.mult)
            adab = kb.sb(st, [128, 24], F32, "adab")
            ng = kb.sb(st, [128, 8], F32, "ng")
            pm = kb.ps(st, [128, 24], F32, "pm")
            pg = kb.ps(st, [128, 2, 512], F32, "pg")
            wring = Ring([kb.sb(st, [128, 8, 1024], F32, "adaw%d" % i) for i in range(2)])
            for l in range(DEPTH):
                kb.dma(adab[:], V(ada_b, ada_b.h[l].rearrange("(j p) -> p j", p=128)))
                kb.dma(ng[:], V(norm_g, norm_g.h[l].rearrange("(k p) -> p k", p=128)))
                for blk in range(3):
                    wt = wring.next()
                    for k in range(8):
                        kb.dma(wt[:, k, :], ada_w[l, k * 128:(k + 1) * 128, blk * 1024:(blk + 1) * 1024],
                               q=("sp" if k % 2 == 0 else "pool"))
                    for jj in range(8):
                        j = blk * 8 + jj
                        for k in range(8):
                            kb.mm(pm[:, j:j + 1], wt[:, k, jj * 128:(jj + 1) * 128], cact[:, k:k + 1],
                                  start=(k == 0), stop=(k == 7), inc=(k == 7))
                kb.tt("dve", modc[:, l, :], pm[:], adab[:], ALU.add)
                kb.stt("dve", Acoef[:, l, :], modc[:, l, 8:16], 1.0, ng[:], ALU.add, ALU.mult)
                for j in range(8):
                    kb.mm(pg[:, j // 4, (j % 4) * 128:(j % 4 + 1) * 128],
                          modc[:, l, 16 + j:17 + j].bc([128, 128]), identf[:], inc=(j % 4 == 3))
                kb.cp("act", gate_row[:, l, :], pg[:].re("p a b -> p (a b)"))
        kb.barrier()
        if debug:
            kb.dma(dbg[:, :], modc[:].re("p l j -> p (l j)"))
            kb.barrier()

        for l in range(DEPTH):
            if stop <= 0:
                break
            x_src = x_in if l == 0 else xres
            last = (l == DEPTH - 1)
            phase_inproj(kb, nc, l, S, x_src, modc, Acoef, w_inF, w_inT, projF, projT, projTs, identb)
            kb.barrier()
            if stop <= 1:
                break
            phase_nsa(kb, nc, l, S, NCMP, NCC, projF, projT, projTs, mix, identb, identf,
                      c_tri4, c_atri4, c_E, c_c2s, c_cmask, c_selmul, c_seladd,
                      cmp_pos, ck_w1, ck_w2, cv_w1, cv_w2, nsa_ng)
            kb.barrier()
            if stop <= 2:
                break
            phase_diff(kb, nc, l, S, projF, projT, mix, identf, c_tri4, diff_lam, diff_ng)
            kb.barrier()
            if stop <= 3:
                break
            phase_ssd(kb, nc, l, S, projF, projT, projTs, mix, identb, identf, ones_f, c_U, c_mb_st,
                      ssm_cw, ssm_cb, ssm_dtb, ssm_alog, ssm_d, ssm_ng)
            kb.barrier()
            if stop <= 4:
                break
            phase_mlstm(kb, nc, l, S, projF, projT, projTs, mix, identb, identf, ones_f, c_U, c_mb_st, c_mb_ts,
                        c_sel127, ml_cw, ml_cb, ml_ifb, ml_ng)
            kb.barrier()
            if stop <= 5:
                break
            phase_out(kb, nc, l, S, x_src, (out_d if last else xres), mix, w_out, gate_row, fin_row, identb, last)
            kb.barrier()
    return nc


def bcast_rows(t, sl, n=128):
    return V(t, sl.partition_broadcast(n))


import os
INPROJ_SUB = 9


def phase_inproj(kb, nc, l, S, x_src, modc, Acoef, w_inF, w_inT, projF, projT, projTs, identb):
    NT = S // 128
    NG = S // 512
    SUB = INPROJ_SUB
    with contextlib.ExitStack() as st:
        wF = kb.sb(st, [128, 8, NF], BF16, "wF")
        wT = kb.sb(st, [128, 8, NTC], BF16, "wT")
        stg = Ring([kb.sb(st, [128, 1024], F32, "wstg%d" % i) for i in range(3)])
        ci = 0
        for (src, dst, ncol) in ((w_inF, wF, NF), (w_inT, wT, NTC)):
            for k in range(8):
                for c0 in range(0, ncol, 1024):
                    w = min(1024, ncol - c0)
                    sg = stg.next()
                    kb.dma(sg[:, 0:w], src[l, k * 128:(k + 1) * 128, c0:c0 + w], q=("sp" if ci % 2 == 0 else "pool"))
                    kb.cp(("dve" if ci % 2 == 0 else "act"), dst[:, k, c0:c0 + w], sg[:, 0:w])
                    ci += 1
        xin = Ring([kb.sb(st, [128, 4, D], F32, "xin%d" % i) for i in range(2)])
        hT = Ring([kb.sb(st, [128, 8, 512], BF16, "hT%d" % i) for i in range(2)])
        xn = Ring([kb.sb(st, [128, D], BF16, "xn%d" % i) for i in range(2)])
        junk = kb.sb(st, [128, D], BF16, "junk")
        ss = kb.sb(st, [128, 4], F32, "ss")
        rstd = kb.sb(st, [128, 4], F32, "rstd")
        ptr = Ring([kb.ps(st, [128, 8, 128], BF16, "ptr%d" % i) for i in range(2)])
        pF = Ring([kb.ps(st, [128, 512], F32, "pF%d" % i) for i in range(2)])
        pT = Ring([kb.ps(st, [128, 512], F32, "pT%d" % i) for i in range(2)])
        fstage = Ring([kb.sb(st, [128, 512], BF16, "fst%d" % i) for i in range(3)])
        tstage = Ring([kb.sb(st, [128, NTC], BF16, "tst%d" % i) for i in range(2)])
        tsstage = Ring([kb.sb(st, [128, 24], F32, "tsst%d" % i) for i in range(2)])
        ev = 0

        def load_x(g):
            xi_ = xin.next()
            kb.dma(xi_[:], V(x_src, x_src.h[g * 512:(g + 1) * 512, :].rearrange("(j p) d -> p j d", p=128)), q="pool")
            return xi_
        xi_next = load_x(0)
        for g in range(NG if SUB >= 2 else 0):
            xi = xi_next
            if g + 1 < NG:
                xi_next = load_x(g + 1)
            h = hT.next()
            for j in range(4):
                kb.act(junk[:], xi[:, j, :], AF.Square, accum=ss[:, j:j + 1])
                kb.rsqrt(rstd[:, j:j + 1], ss[:, j:j + 1], 1.0 / D)
                xb = xn.next()
                kb.ts("dve", xb[:], xi[:, j, :], rstd[:, j:j + 1], None, ALU.mult)
                pt = ptr.next()
                for k in range(8):
                    kb.tr(pt[:, k, :], xb[:, k * 128:(k + 1) * 128], identb[:], inc=(k == 7))
                for k in range(8):
                    e = "act" if j % 2 == 0 else "dve"
                    if e == "act":
                        kb.act(h[:, k, j * 128:(j + 1) * 128], pt[:, k, :], AF.Identity,
                               bias=modc[:, l, k:k + 1], scale=Acoef[:, l, k:k + 1])
                    else:
                        kb.ts("dve", h[:, k, j * 128:(j + 1) * 128], pt[:, k, :], Acoef[:, l, k:k + 1],
                              modc[:, l, k:k + 1], ALU.mult, ALU.add)
            for r0 in (range(0, NF, 128) if SUB >= 3 else []):
                w = 128
                p = pF.next()
                for k in range(8):
                    kb.mm(p[0:w, :], wF[:, k, r0:r0 + w], h[:, k, :], start=(k == 0), stop=(k == 7), inc=(k == 7))
                fs = fstage.next()
                kb.cp(("act" if ev % 2 == 0 else "dve"), fs[0:w, :], p[0:w, :])
                ev += 1
                kb.dma(projF[r0:r0 + w, g * 512:(g + 1) * 512], fs[0:w, :], q="sp", disjoint=True)
            for j in range(4 if SUB >= 4 else 0):
                ts_ = tstage.next()
                tss = tsstage.next()
                for c0 in range(0, NTC, 512):
                    w = min(512, NTC - c0)
                    p = pT.next()
                    for k in range(8):
                        kb.mm(p[:, 0:w], h[:, k, j * 128:(j + 1) * 128], wT[:, k, c0:c0 + w],
                              start=(k == 0), stop=(k == 7), inc=(k == 7))
                    e_ = ("act" if ev % 2 == 0 else "dve")
                    kb.cp(e_, ts_[:, c0:c0 + w], p[:, 0:w])
                    ev += 1
                    for nm in (("ag", "dt", "dif") if SUB >= 5 else ()):
                        tc0, tw = T_COL[nm]
                        if c0 <= tc0 < c0 + w:
                            so, _ = TS_COL[nm]
                            kb.cp(e_, tss[:, so:so + tw], p[:, tc0 - c0:tc0 - c0 + tw])
                tok = g * 512 + j * 128
                if SUB >= 6:
                    kb.dma(projT[tok:tok + 128, :], ts_[:], q="sp", disjoint=True)
                if SUB >= 7:
                    kb.dma(projTs[tok:tok + 128, :], tss[:], q="sp", disjoint=True)


def load_T(kb, dst, projT, name, S, sub=None, q="sp"):
    NT = S // 128
    c0, w = T_COL[name]
    if sub is not None:
        c0, w = c0 + sub[0], sub[1]
    step = 8
    for a in range(0, NT, step):
        b = min(NT, a + step)
        kb.dma(dst[:, a:b, :], V(projT, projT.h[a * 128:b * 128, c0:c0 + w].rearrange("(c p) n -> p c n", p=128)), q=q)


def load_Ts(kb, dst, projTs, name, S):
    c0, w = TS_COL[name]
    kb.dma(dst[:], V(projTs, projTs.h[:, c0:c0 + w].rearrange("(c p) n -> p c n", p=128)))


def head_tail(kb, st_bufs, o, ng_row, sz, mix, tok, col0, post_scale, nheads=4, hd=64):
    sq, ssq, rs, yo = st_bufs
    W = nheads * hd
    kb.tt("pool", sq[:, 0:W], o, o, ALU.mult)
    kb.red(ssq[:, 0:nheads], sq[:, 0:W].re("p (h d) -> p h d", h=nheads), ALU.add)
    kb.rsqrt(rs[:, 0:nheads], ssq[:, 0:nheads], 1.0 / hd, post_scale)
    for h in range(nheads):
        kb.ts("dve", sq[:, h * hd:(h + 1) * hd], o[:, h * hd:(h + 1) * hd], rs[:, h:h + 1], None, ALU.mult)
    kb.tt("pool", sq[:, 0:W], sq[:, 0:W], ng_row, ALU.mult)
    kb.tt("dve", yo[:, 0:W], sq[:, 0:W], sz, ALU.mult)
    kb.dma(mix[tok:tok + 128, col0:col0 + W], yo[:, 0:W], q="sp", disjoint=True)


def silu_all(kb, st, z, S, name):
    NT = S // 128
    W = 256
    sg = kb.sb(st, [128, NT, W], BF16, name + "_sg")
    kb.act(sg[:], z[:], AF.Sigmoid)
    kb.tt("pool", z[:], z[:], sg[:], ALU.mult)
    return z


def phase_nsa(kb, nc, l, S, NCMP, NCC, projF, projT, projTs, mix, identb, identf,
              c_tri4, c_atri4, c_E, c_c2s, c_cmask, c_selmul, c_seladd,
              cmp_pos, ck_w1, ck_w2, cv_w1, cv_w2, nsa_ng):
    NT = S // 128
    NCP = NCC * 128
    with contextlib.ExitStack() as st:
        q_all = kb.sb(st, [128, NT, 4, 128], BF16, "q_all")
        qtiles = [kb.sub(q_all[:, i]) for i in range(NT)]
        kcT = kb.sb(st, [64, S], BF16, "kcT")
        vcT = kb.sb(st, [64, S], BF16, "vcT")
        kwT = kb.sb(st, [64, S], BF16, "kwT")
        lsel = kb.sb(st, [128, S], BF16, "lsel")
        vs1 = kb.sb(st, [128, NT, 65], BF16, "vs1")
        vw1 = kb.sb(st, [128, NT, 65], BF16, "vw1")
        gts = kb.sb(st, [128, NT, 12], F32, "gts")
        z = kb.sb(st, [128, NT, 256], BF16, "z")
        tri4 = kb.sb(st, [128, 512], BF16, "tri4")
        atri4 = kb.sb(st, [128, 512], BF16, "atri4")
        c2s = kb.sb(st, [128, NCC, 65], BF16, "c2s")
        cmask = kb.sb(st, [128, NCC, S], BF16, "cmask")
        selmul = kb.sb(st, [128, NT, 64], F32, "selmul")
        seladd = kb.sb(st, [128, NT, 64], F32, "seladd")
        ngrow = kb.sb(st, [128, 4, 64], F32, "ngrow")
        for h in range(4):
            r0, _ = F_ROW["aq%d" % h]
            for i in range(NT):
                pass
            kb.dma(V(q_all, q_all.h[0:64, :, h, :]), V(projF, projF.h[r0:r0 + 64, :].rearrange("d (c t) -> d c t", t=128)),
                   q=("sp" if h % 2 == 0 else "pool"))
        kb.memset("pool", V(q_all, q_all.h[64:128]), 0.0)
        kb.dma(kcT[:], projF[F_ROW["akc"][0]:F_ROW["akc"][0] + 64, :])
        kb.dma(vcT[:], projF[F_ROW["avc"][0]:F_ROW["avc"][0] + 64, :], q="pool")
        kb.dma(kwT[:], projF[F_ROW["akw"][0]:F_ROW["akw"][0] + 64, :])
        kb.dma(lsel[0:64, :], projF[F_ROW["aks"][0]:F_ROW["aks"][0] + 64, :], q="pool")
        kb.dma(lsel[64:128, :], c_E[:, :])
        kb.memset("pool", vs1[:, :, 64:65], 1.0)
        kb.memset("pool", vw1[:, :, 64:65], 1.0)
        load_T(kb, V(vs1, vs1.h[:, :, 0:64]), projT, "vs", S)
        load_T(kb, V(vw1, vw1.h[:, :, 0:64]), projT, "vw", S, q="pool")
        load_Ts(kb, gts, projTs, "ag", S)
        load_T(kb, z, projT, "az", S)
        kb.dma(tri4[:], c_tri4[:, :])
        kb.dma(atri4[:], c_atri4[:, :])
        kb.dma(c2s[:], V(c_c2s, c_c2s.h.rearrange("(c p) n -> p c n", p=128)))
        for cc in range(NCC):
            kb.dma(cmask[:, cc, :], c_cmask[cc * 128:(cc + 1) * 128, :], q=("sp" if cc == 0 else "pool"))
        kb.dma(selmul[:], V(c_selmul, c_selmul.h.rearrange("(c p) n -> p c n", p=128)))
        kb.dma(seladd[:], V(c_seladd, c_seladd.h.rearrange("(c p) n -> p c n", p=128)), q="pool")
        kb.dma(ngrow[:, 0, :], bcast_rows(nsa_ng, nsa_ng.h[l]))
        for h in range(1, 4):
            kb.cp("pool", ngrow[:, h, :], ngrow[:, 0, :])
        kb.act(gts[:], gts[:], AF.Sigmoid)

        kcmpT = kb.sb(st, [64, NCP], BF16, "kcmpT")
        vcmp1 = kb.sb(st, [128, NCC, 65], BF16, "vcmp1")
        kb.memset("pool", vcmp1[:, :, 64:65], 1.0)
        with contextlib.ExitStack() as s2:
            sz = silu_all(kb, s2, z, S, "az")
            posT = kb.sb(s2, [64, 32], F32, "posT")
            posTb = kb.sb(s2, [64, 32], BF16, "posTb")
            kb.dma(posT[:], V(cmp_pos, cmp_pos.h[l].rearrange("l d -> d l")))
            kb.cp("dve", posTb[:], posT[:])
            w1s = kb.sb(s2, [64, 16, 128], F32, "w1s")
            w1b = kb.sb(s2, [64, 32, 128], BF16, "w1b")
            w2s = kb.sb(s2, [128, 64], F32, "w2s")
            w2b = kb.sb(s2, [128, 64], BF16, "w2b")
            hid = kb.sb(s2, [128, NCP], BF16, "hid")
            tpre = kb.sb(s2, [128, NCP], F32, "tpre")
            sgm = kb.sb(s2, [128, NCP], F32, "sgm")
            cst = kb.sb(s2, [128, 1], F32, "cst")
            pc = kb.ps(s2, [128, 1], F32, "pc")
            ph = kb.ps(s2, [128, NCP], F32, "ph")
            po = kb.ps(s2, [128, NCP], F32, "po")
            for which, (w1d, w2d, srcT) in enumerate(((ck_w1, ck_w2, kcT), (cv_w1, cv_w2, vcT))):
                for hf in range(2):
                    kb.dma(w1s[:], V(w1d, w1d.h[l, hf * 1024:(hf + 1) * 1024, :].rearrange("(l d) h -> d l h", d=64)))
                    kb.cp("dve", w1b[:, hf * 16:(hf + 1) * 16, :], w1s[:])
                kb.dma(w2s[:], w2d[l])
                kb.cp("dve", w2b[:], w2s[:])
                for li in range(32):
                    kb.mm(pc[:], w1b[:, li, :], posTb[:, li:li + 1], start=(li == 0), stop=(li == 31), inc=(li == 31))
                kb.cp("dve", cst[:], pc[:])
                for li in range(32):
                    kb.mm(ph[:, 0:NCMP], w1b[:, li, :], V(srcT, srcT.h[:, li:li + 16 * (NCMP - 1) + 1:16]),
                          start=(li == 0), stop=(li == 31), inc=(li == 31))
                kb.memset("pool", hid[:], 0.0)
                kb.ts("dve", tpre[:, 0:NCMP], ph[:, 0:NCMP], cst[:, 0:1], None, ALU.add)
                kb.act(sgm[:, 0:NCMP], tpre[:, 0:NCMP], AF.Sigmoid)
                kb.tt("dve", hid[:, 0:NCMP], tpre[:, 0:NCMP], sgm[:, 0:NCMP], ALU.mult)
                if which == 0:
                    kb.mm(po[0:64, :], w2b[:], hid[:])
                    kb.cp("dve", kcmpT[:], po[0:64, :])
                else:
                    for cc in range(NCC):
                        kb.mm(po[:, cc * 64:(cc + 1) * 64], hid[:, cc * 128:(cc + 1) * 128], w2b[:], inc=(cc == NCC - 1))
                    kb.cp("dve", V(vcmp1, vcmp1.h[:, :, 0:64]), po[:, 0:NCC * 64].re("p (c d) -> p c d", c=NCC))
        kb.barrier()

        psc = Ring([kb.ps(st, [128, 512], F32, "psc%d" % i) for i in range(3)])
        pacc = [kb.ps(st, [65, 512], F32, "pacc%d" % i) for i in range(3)]
        pmisc = kb.ps(st, [128, 512], F32, "pmisc")
        ptl = kb.ps(st, [128, 6, 65], F32, "ptl")
        Pr = Ring([kb.sb(st, [128, 512], BF16, "P%d" % i) for i in range(5)])
        Pc = [kb.sb(st, [128, 512], BF16, "Pc%d" % i) for i in range(NCC)]
        oTs = [kb.sb(st, [65, 3, 512], F32, "oT%d" % i) for i in range(2)]
        rdc = kb.sb(st, [128, 4], F32, "rdc")
        imp = kb.sb(st, [128, 64], F32, "imp")
        imp2 = kb.sb(st, [128, 64], F32, "imp2")
        m8 = kb.sb(st, [128, 16], F32, "m8")
        selpad = kb.sb(st, [128, 128], F32, "selpad")
        kb.memset("pool", selpad[:], 0.0)
        tl = kb.sb(st, [128, 12, 65], F32, "tl")
        rden = kb.sb(st, [128, 12], F32, "rden")
        fco = kb.sb(st, [128, 12], F32, "fco")
        o = kb.sb(st, [128, 256], F32, "o")
        bufs = (kb.sb(st, [128, 256], F32, "sq"), kb.sb(st, [128, 4], F32, "ssq"),
                kb.sb(st, [128, 4], F32, "rs"), kb.sb(st, [128, 256], BF16, "yo"))

        def select1(qi, ncv):
            for h in range(4):
                for cc in range(ncv):
                    kb.mm(pmisc[:, h * 65:(h + 1) * 65], Pc[cc][:, h * 128:(h + 1) * 128], c2s[:, cc, :],
                          start=(cc == 0), stop=(cc == ncv - 1), inc=(h == 3 and cc == ncv - 1))
            pim = pmisc[:, 0:260].re("p (h n) -> p h n", h=4)
            kb.ts("dve", rdc[:], pim[:, :, 64], 1e-30, None, ALU.max)
            kb.recip(rdc[:], rdc[:])
            kb.ts("dve", imp[:], pim[:, 0, 0:64], rdc[:, 0:1], None, ALU.mult)
            for h in range(1, 4):
                kb.stt("dve", imp[:], pim[:, h, 0:64], rdc[:, h:h + 1], imp[:], ALU.mult, ALU.add)
            kb.tt("dve", imp[:], imp[:], selmul[:, qi, :], ALU.mult)
            kb.tt("dve", imp[:], imp[:], seladd[:, qi, :], ALU.add)
            kb.op("dve", lambda e: e.max(out=m8[:, 0:8].ap, in_=imp[:].ap), [m8[:]], [imp[:]])
            kb.op("dve", lambda e: e.match_replace(out=imp2[:].ap, in_to_replace=m8[:, 0:8].ap,
                                                   in_values=imp[:].ap, imm_value=-3.0), [imp2[:]], [m8[:], imp[:]])
            kb.op("dve", lambda e: e.max(out=m8[:, 8:16].ap, in_=imp2[:].ap), [m8[:]], [imp2[:]])
            kb.ts("dve", selpad[:, 64:128], imp[:], m8[:, 15:16], -1.0, ALU.is_ge, ALU.add)

        def select2(qi):
            qt = qtiles[qi]
            kb.tr(pmisc[:, 260:388], selpad[:], identf[:])
            kb.cp("dve", V(qt, qt.h[64:128]), V(pmisc, pmisc.h[64:128, 260:388].unsqueeze(1).to_broadcast([64, 4, 128])))

        def tail(qi):
            q0 = qi * 128
            oT = oTs[qi % 2]
            for half in range(2):
                for i in range(6):
                    idx = half * 6 + i
                    b, h = idx // 4, idx % 4
                    kb.tr(ptl[:, i, :], oT[:, b, h * 128:(h + 1) * 128], identf[0:65, 0:65], inc=(i == 5))
                kb.cp("dve", tl[:, half * 6:(half + 1) * 6, :], ptl[:])
            kb.ts("dve", rden[:], tl[:, :, 64], 1e-30, None, ALU.max)
            kb.recip(rden[:], rden[:])
            kb.tt("dve", fco[:].re("p (b h) -> p b h", b=3), rden[:].re("p (b h) -> p b h", b=3),
                  V(gts, gts.h[:, qi, :].rearrange("p (h b) -> p b h", b=3)), ALU.mult)
            for h in range(4):
                kb.ts("dve", o[:, h * 64:(h + 1) * 64], tl[:, h, 0:64], fco[:, h:h + 1], None, ALU.mult)
                for b in (1, 2):
                    kb.stt("dve", o[:, h * 64:(h + 1) * 64], tl[:, b * 4 + h, 0:64], fco[:, b * 4 + h:b * 4 + h + 1],
                           o[:, h * 64:(h + 1) * 64], ALU.mult, ALU.add)
            head_tail(kb, bufs, o[:], ngrow[:].re("p h d -> p (h d)"), sz[:, qi, :], mix, q0, 0, 1.0)

        dq = Deferred()
        for qi in range(NT):
            qt = qtiles[qi]
            rq = V(qt, qt.h[0:64].rearrange("d h t -> d (h t)"))
            rqs = V(qt, qt.h.rearrange("d h t -> d (h t)"))
            q0 = qi * 128
            ncv = min(NCC, (8 * qi + 6) // 128 + 1)
            wl = [kc for kc in range(qi - 4, qi + 1) if kc >= 0]
            items = [("c", cc) for cc in range(ncv)] + [("w", kc) for kc in wl] + [("s", kc) for kc in range(qi + 1)]
            sel2 = [None]
            pvc = [None]
            for (kind, kc) in items:
                p = psc.next()
                if kind == "c":
                    kb.mm(p[:], kcmpT[:, kc * 128:(kc + 1) * 128], rq)
                    P = Pc[kc]
                    kb.act(P[:], p[:], AF.Exp, scale=0.125)
                    cm = V(cmask, cmask.h[:, kc, q0:q0 + 128].unsqueeze(1).to_broadcast([128, 4, 128]))
                    kb.tt("pool", P[:].re("p (h t) -> p h t", h=4), P[:].re("p (h t) -> p h t", h=4), cm, ALU.mult)
                elif kind == "w":
                    kb.mm(p[:], kwT[:, kc * 128:(kc + 1) * 128], rq)
                    P = Pr.next()
                    kb.act(P[:], p[:], AF.Exp, scale=0.125)
                    if kc == qi:
                        kb.tt("pool", P[:], P[:], tri4[:], ALU.mult)
                    elif kc == qi - 4:
                        kb.tt("pool", P[:], P[:], atri4[:], ALU.mult)
                else:
                    if kc == 0:
                        dq.run_through(pvc[0])
                        dq.force(sel2[0])
                    kb.mm(p[:], lsel[:, kc * 128:(kc + 1) * 128], rqs)
                    P = Pr.next()
                    kb.act(P[:], p[:], AF.Exp, scale=0.125)
                    if kc == qi:
                        kb.tt("pool", P[:], P[:], tri4[:], ALU.mult)

                def pv(kind=kind, kc=kc, P=P, qi=qi, ncv=ncv, wl=wl, sel2=sel2):
                    if kind == "c":
                        kb.mm(pacc[0][:], vcmp1[:, kc, :], P[:], start=(kc == 0), stop=(kc == ncv - 1), inc=True)
                        if kc == ncv - 1:
                            select1(qi, ncv)
                            sel2[0] = dq.push(lambda qi=qi: select2(qi), 3)
                    elif kind == "w":
                        kb.mm(pacc[2][:], vw1[:, kc, :], P[:], start=(kc == wl[0]), stop=(kc == qi), inc=True)
                    else:
                        kb.mm(pacc[1][:], vs1[:, kc, :], P[:], start=(kc == 0), stop=(kc == qi), inc=True)
                        if kc == qi:
                            for b in range(3):
                                kb.cp("dve", oTs[qi % 2][:, b, :], pacc[b][:])
                            dq.push(lambda qi=qi: tail(qi), 2)
                dq.step()
                e_ = dq.push(pv, 1)
                if kind == "c" and kc == ncv - 1:
                    pvc[0] = e_
        dq.flush()


def phase_diff(kb, nc, l, S, projF, projT, mix, identf, c_tri4, diff_lam, diff_ng):
    NT = S // 128
    lambda_init = 0.8 - 0.6 * math.exp(-0.3 * l)
    sc = 32 ** -0.5
    with contextlib.ExitStack() as st:
        qT = kb.sb(st, [64, 4, S], BF16, "qT")
        kT = kb.sb(st, [64, 4, S], BF16, "kT")
        v1 = kb.sb(st, [128, NT, 4, 65], BF16, "v1")
        z = kb.sb(st, [128, NT, 256], BF16, "z")
        tri4 = kb.sb(st, [128, 512], BF16, "tri4")
        ngrow = kb.sb(st, [128, 4, 64], F32, "ngrow")
        lam = kb.sb(st, [128, 128], F32, "lam")
        lp = kb.sb(st, [128, 64], F32, "lp")
        ls = kb.sb(st, [128, 2], F32, "ls")
        nlam = kb.sb(st, [128, 1], F32, "nlam")
        s2 = contextlib.ExitStack()
        vtmp = kb.sb(s2, [128, NT, 256], BF16, "vtmp")
        for h in range(4):
            kb.dma(qT[:, h, :], projF[F_ROW["bq%d" % h][0]:F_ROW["bq%d" % h][0] + 64, :], q="sp")
            kb.dma(kT[:, h, :], projF[F_ROW["bk%d" % h][0]:F_ROW["bk%d" % h][0] + 64, :], q="pool")
        load_T(kb, vtmp, projT, "bv", S)
        load_T(kb, z, projT, "bz", S, q="pool")
        kb.dma(tri4[:], c_tri4[:, :])
        kb.dma(ngrow[:, 0, :], bcast_rows(diff_ng, diff_ng.h[l]))
        kb.dma(lam[:], bcast_rows(diff_lam, diff_lam.h[l]))
        for h in range(1, 4):
            kb.cp("pool", ngrow[:, h, :], ngrow[:, 0, :])
        kb.memset("pool", V(v1, v1.h[:, :, :, 64:65]), 1.0)
        kb.cp("pool", V(v1, v1.h[:, :, :, 0:64]), vtmp[:].re("p c (h d) -> p c h d", h=4))
        lv = lam[:].re("p (a b d) -> p a b d", a=2, b=2)
        kb.tt("dve", lp[:].re("p (a d) -> p a d", a=2), lv[:, :, 0, :], lv[:, :, 1, :], ALU.mult)
        kb.red(ls[:], lp[:].re("p (a d) -> p a d", a=2), ALU.add)
        kb.act(ls[:], ls[:], AF.Exp)
        kb.tt("dve", nlam[:], ls[:, 1:2], ls[:, 0:1], ALU.subtract)
        kb.ts("dve", nlam[:], nlam[:], -lambda_init, None, ALU.add)
        sz = silu_all(kb, s2, z, S, "bz")
        s2.close()
        kb.barrier()

        psc = Ring([kb.ps(st, [128, 2, 512], F32, "psc%d" % i) for i in range(2)])
        _pa = [kb.ps(st, [65, 512], F32, "pacc%d" % hp) for hp in range(2)]
        paccs = [[_pa[0], _pa[0]], [_pa[1], _pa[1]]]
        ptl = Ring([kb.ps(st, [128, 4, 65], F32, "ptl%d" % i) for i in range(2)])
        Pr = Ring([kb.sb(st, [128, 2, 512], BF16, "P%d" % i) for i in range(4)])
        oTs = [kb.sb(st, [65, 2, 512], F32, "oT%d" % i) for i in range(2)]
        tls = [kb.sb(st, [128, 8, 65], F32, "tl%d" % i) for i in range(2)]
        rden = kb.sb(st, [128, 8], F32, "rden")
        o1 = kb.sb(st, [128, 64], F32, "o1")
        o = kb.sb(st, [128, 256], F32, "o")
        bufs = (kb.sb(st, [128, 256], F32, "sq"), kb.sb(st, [128, 4], F32, "ssq"),
                kb.sb(st, [128, 4], F32, "rs"), kb.sb(st, [128, 256], BF16, "yo"))
        qm = Ring([kb.sb(st, [64, 4, 2, 128], BF16, "qm%d" % i) for i in range(2)])
        for t_ in qm.tiles:
            kb.memset("pool", t_[:], 0.0)

        def tail(qi):
            q0 = qi * 128
            oT = oTs[qi % 2]
            tl = tls[qi % 2]
            for half in range(2):
                pt = ptl.next()
                for i in range(4):
                    kb.tr(pt[:, i, :], oT[:, half, i * 128:(i + 1) * 128], identf[0:65, 0:65], inc=(i == 3))
                kb.cp("dve", tl[:, half * 4:(half + 1) * 4, :], pt[:])
            kb.ts("dve", rden[:], tl[:, :, 64], 1e-30, None, ALU.max)
            kb.recip(rden[:], rden[:])
            kb.ts("dve", V(rden, rden.h[:, 1:8:2]), V(rden, rden.h[:, 1:8:2]), nlam[:, 0:1], None, ALU.mult)
            for h in range(4):
                kb.ts("dve", o1[:], tl[:, 2 * h, 0:64], rden[:, 2 * h:2 * h + 1], None, ALU.mult)
                kb.stt("dve", o[:, h * 64:(h + 1) * 64], tl[:, 2 * h + 1, 0:64], rden[:, 2 * h + 1:2 * h + 2],
                       o1[:], ALU.mult, ALU.add)
            head_tail(kb, bufs, o[:], ngrow[:].re("p h d -> p (h d)"), sz[:, qi, :], mix, q0, 256, 1.0 - lambda_init)

        dq = Deferred()
        for qi in range(NT):
            q0 = qi * 128
            qmt = qm.next()
            kb.cp("pool", V(qmt, qmt.h[0:32, :, 0, :]), qT[0:32, :, q0:q0 + 128])
            kb.cp("pool", V(qmt, qmt.h[32:64, :, 1, :]), qT[32:64, :, q0:q0 + 128])
            for hp in range(2):
                for k0 in range(0, qi + 1, 2):
                    kcs = [kc for kc in (k0, k0 + 1) if kc <= qi]
                    nk = len(kcs)
                    p = psc.next()
                    for j, kc in enumerate(kcs):
                        for hh in range(2):
                            h = hp * 2 + hh
                            kb.mm(p[:, j, hh * 256:(hh + 1) * 256], kT[:, h, kc * 128:(kc + 1) * 128],
                                  V(qmt, qmt.h[:, h].rearrange("d c t -> d (c t)")), inc=(hh == 1 and j == nk - 1))
                    P = Pr.next()
                    kb.act(P[:, 0:nk, :], p[:, 0:nk, :], AF.Exp, scale=sc)
                    if kcs[-1] == qi:
                        kb.tt("pool", P[:, nk - 1, :], P[:, nk - 1, :], tri4[:], ALU.mult)

                    def pv(qi=qi, hp=hp, kcs=kcs, P=P):
                        pa = paccs[hp][qi % 2]
                        for j, kc in enumerate(kcs):
                            for hh in range(2):
                                h = hp * 2 + hh
                                kb.mm(pa[:, hh * 256:(hh + 1) * 256], v1[:, kc, h, :], P[:, j, hh * 256:(hh + 1) * 256],
                                      start=(kc == 0 and hh == 0), stop=(kc == qi and hh == 1),
                                      inc=(hh == 1 and j == len(kcs) - 1))
                        if kcs[-1] == qi:
                            kb.cp("act", oTs[qi % 2][:, hp, :], pa[:])
                            if hp == 1:
                                dq.push(lambda qi=qi: tail(qi), 2)
                    dq.step()
                    dq.push(pv, 0)
        dq.flush()


def conv_silu(kb, eng, dst, src, acc, wcol, bcol, S, rows):
    kb.ts(eng, acc[0:rows, :], src, wcol[:, 3:4], bcol, ALU.mult, ALU.add)
    for k in range(3):
        sh = 3 - k
        kb.stt("dve", acc[0:rows, sh:S], src[:, 0:S - sh], wcol[:, k:k + 1], acc[0:rows, sh:S], ALU.mult, ALU.add)
    kb.act(dst, acc[0:rows, :], AF.Silu)


def phase_ssd(kb, nc, l, S, projF, projT, projTs, mix, identb, identf, ones_f, c_U, c_mb_st,
              ssm_cw, ssm_cb, ssm_dtb, ssm_alog, ssm_d, ssm_ng):
    NT = S // 128
    with contextlib.ExitStack() as st:
        U = kb.sb(st, [128, 128], F32, "U")
        mbst = kb.sb(st, [128, 128], F32, "mbst")
        kb.dma(U[:], c_U[:, :])
        kb.dma(mbst[:], c_mb_st[:, :])
        xT = kb.sb(st, [128, 2, S], BF16, "xT")
        BT = kb.sb(st, [64, 2, S], BF16, "BT")
        CT = kb.sb(st, [64, 2, S], BF16, "CT")
        xB = kb.sb(st, [128, NT, 384], BF16, "xB")
        z = kb.sb(st, [128, NT, 256], BF16, "z")
        dtr = kb.sb(st, [128, NT, 4], F32, "dtr")
        with contextlib.ExitStack() as s2:
            raw = Ring([kb.sb(s2, [128, S], BF16, "raw%d" % i) for i in range(2)])
            acc = Ring([kb.sb(s2, [128, S], F32, "acc%d" % i) for i in range(2)])
            wc = kb.sb(s2, [128, 6, 4], F32, "wc")
            bc_ = kb.sb(s2, [128, 6], F32, "bc")
            specs = [("cx0", 0, 128, xT, 0), ("cx1", 128, 128, xT, 1), ("cB0", 256, 64, BT, 0), ("cB1", 320, 64, BT, 1),
                     ("cC0", 384, 64, CT, 0), ("cC1", 448, 64, CT, 1)]
            for i, (nm, ch0, rows, dst, di) in enumerate(specs):
                kb.dma(wc[0:rows, i, :], V(ssm_cw, ssm_cw.h[l, :, ch0:ch0 + rows].rearrange("k c -> c k")))
                kb.dma(bc_[0:rows, i:i + 1], V(ssm_cb, ssm_cb.h[l, ch0:ch0 + rows].rearrange("(c o) -> c o", o=1)))
            for i, (nm, ch0, rows, dst, di) in enumerate(specs):
                r = raw.next()
                a = acc.next()
                r0 = F_ROW[nm][0]
                kb.dma(r[0:rows, :], projF[r0:r0 + rows, :], q=("sp" if i % 2 == 0 else "pool"))
                conv_silu(kb, ("dve" if i % 2 == 0 else "pool"), dst[0:rows, di, :], r[0:rows, :], a,
                          wc[0:rows, i, :], bc_[0:rows, i:i + 1], S, rows)
            load_T(kb, z, projT, "cz", S)
            sz = silu_all(kb, s2, z, S, "cz")
        kb.barrier()
        load_Ts(kb, dtr, projTs, "dt", S)
        dtb = kb.sb(st, [128, 4], F32, "dtb")
        aneg = kb.sb(st, [128, 4], F32, "aneg")
        dsk = kb.sb(st, [128, 4], F32, "dsk")
        ngrow = kb.sb(st, [128, 256], F32, "ngrow")
        kb.dma(dtb[:], bcast_rows(ssm_dtb, ssm_dtb.h[l]))
        kb.dma(aneg[:], bcast_rows(ssm_alog, ssm_alog.h[l]))
        kb.dma(dsk[:], bcast_rows(ssm_d, ssm_d.h[l]))
        kb.dma(ngrow[:], bcast_rows(ssm_ng, ssm_ng.h[l]))
        kb.act(aneg[:], aneg[:], AF.Exp)
        kb.ts("dve", aneg[:], aneg[:], -1.0, None, ALU.mult)
        dt = kb.sb(st, [128, NT, 4], F32, "dt")
        adt = kb.sb(st, [128, NT, 4], F32, "adt")
        kb.tt("dve", dt[:], dtr[:], V(dtb, dtb.h[:, :].unsqueeze(1).to_broadcast([128, NT, 4])), ALU.add)
        kb.act(dt[:], dt[:], AF.Exp)
        kb.act(dt[:], dt[:], AF.Ln, bias=1.0)
        kb.tt("dve", adt[:], dt[:], V(aneg, aneg.h[:, :].unsqueeze(1).to_broadcast([128, NT, 4])), ALU.mult)
        acs = kb.sb(st, [128, NT, 4], F32, "acs")
        alast = kb.sb(st, [128, NT, 4], F32, "alast")
        ea = kb.sb(st, [128, NT, 4], F32, "ea")
        de = kb.sb(st, [128, NT, 4], F32, "de")
        cd = kb.sb(st, [128, NT, 4], F32, "cd")
        with contextlib.ExitStack() as s2:
            pa = kb.ps(s2, [128, NT * 4], F32, "pa")
            pb = kb.ps(s2, [128, NT * 4], F32, "pb")
            kb.mm(pa[:], U[:], adt[:].re("p c h -> p (c h)"))
            kb.mm(pb[:], ones_f[:], adt[:].re("p c h -> p (c h)"))
            kb.cp("dve", acs[:].re("p c h -> p (c h)"), pa[:])
            kb.cp("dve", alast[:].re("p c h -> p (c h)"), pb[:])
        kb.barrier()
        kb.act(ea[:], acs[:], AF.Exp)
        kb.act(cd[:], alast[:], AF.Exp)
        kb.tt("dve", de[:], alast[:], acs[:], ALU.subtract)
        kb.act(de[:], de[:], AF.Exp)
        with contextlib.ExitStack() as s2:
            ptx = Ring([kb.ps(s2, [128, 384], BF16, "ptx%d" % i) for i in range(2)])
            for c in range(NT):
                pt = ptx.next()
                kb.tr(pt[:, 0:128], xT[:, 0, c * 128:(c + 1) * 128], identb[:], inc=False)
                kb.tr(pt[:, 128:256], xT[:, 1, c * 128:(c + 1) * 128], identb[:], inc=False)
                kb.tr(pt[:, 256:320], BT[:, 0, c * 128:(c + 1) * 128], identb[0:64, 0:64], inc=False)
                kb.tr(pt[:, 320:384], BT[:, 1, c * 128:(c + 1) * 128], identb[0:64, 0:64], inc=True)
                kb.cp(("act" if c % 2 == 0 else "dve"), xB[:, c, :], pt[:])
        kb.barrier()
        pR = kb.ps(st, [128, 4, 128], F32, "pR")
        pS = kb.ps(st, [128, 2, 128], F32, "pS")
        pY = kb.ps(st, [128, 256], F32, "pY")
        pO = kb.ps(st, [128, 256], F32, "pO")
        pN = kb.ps(st, [64, 4, 64], F32, "pN")
        arg = kb.sb(st, [128, 4, 128], F32, "arg")
        dec = kb.sb(st, [128, 4, 128], F32, "dec")
        GT = kb.sb(st, [128, 4, 128], BF16, "GT")
        xdt = kb.sb(st, [128, 4, 64], BF16, "xdt")
        xdw = kb.sb(st, [128, 4, 64], BF16, "xdw")
        stf = kb.sb(st, [64, 4, 64], F32, "stf")
        stb = kb.sb(st, [64, 4, 64], BF16, "stb")
        yd = kb.sb(st, [128, 256], F32, "yd")
        y = kb.sb(st, [128, 256], F32, "y")
        kb.memset("pool", stf[:], 0.0)
        kb.memset("pool", stb[:], 0.0)
        bufs = (kb.sb(st, [128, 256], F32, "sq"), kb.sb(st, [128, 4], F32, "ssq"),
                kb.sb(st, [128, 4], F32, "rs"), kb.sb(st, [128, 256], BF16, "yo"))
        xdts = [xdt, kb.sb(st, [128, 4, 64], BF16, "xdt_r1")]
        stbs = [stb] + [kb.sb(st, [64, 4, 64], BF16, "stb_r%d" % i) for i in range(2)]

        def stream_state(c):
            xc = V(xB, xB.h[:, c, 0:256].rearrange("p (h d) -> p h d", h=4))
            xd = xdts[c % 2]
            kb.tt("pool", xd[:], xc, V(dt, dt.h[:, c, :].unsqueeze(2).to_broadcast([128, 4, 64])), ALU.mult)
            kb.tt("pool", xdw[:], xd[:], V(de, de.h[:, c, :].unsqueeze(2).to_broadcast([128, 4, 64])), ALU.mult)
            yield
            for h in range(4):
                kb.mm(pN[:, h, :], xB[:, c, 256 + 64 * (h // 2):256 + 64 * (h // 2) + 64], xdw[:, h, :], inc=(h == 3))
            yield
            for h in range(4):
                kb.stt("dve", stf[:, h, :], stf[:, h, :], cd[0:64, c, h:h + 1], pN[:, h, :], ALU.mult, ALU.add)
                if h % 2 == 1:
                    yield
            kb.cp("act", stbs[(c + 1) % 3][:], stf[:])
            yield

        def stream_out(c):
            t0 = c * 128
            xc = V(xB, xB.h[:, c, 0:256].rearrange("p (h d) -> p h d", h=4))
            xd = xdts[c % 2]
            sb_ = stbs[c % 3]
            for h in range(4):
                kb.mm(pR[:, h, :], adt[:, c, h:h + 1].bc([128, 128]), U[:], inc=(h == 3))
            for g in range(2):
                kb.mm(pS[:, g, :], BT[:, g, t0:t0 + 128], CT[:, g, t0:t0 + 128], inc=(g == 1))
            yield
            for h in range(4):
                kb.stt("dve", arg[:, h, :], pR[:, h, :], acs[:, c, h:h + 1], mbst[:], ALU.subtract, ALU.add)
                if h % 2 == 1:
                    yield
            kb.act(dec[:], arg[:], AF.Exp)
            for h in range(4):
                kb.mm(pO[:, h * 64:(h + 1) * 64], CT[:, h // 2, t0:t0 + 128], sb_[:, h, :], inc=(h == 3))
            yield
            for h in range(4):
                kb.tt("dve", GT[:, h, :], pS[:, h // 2, :], dec[:, h, :], ALU.mult)
                if h % 2 == 1:
                    yield
            for h in range(4):
                kb.mm(pY[:, h * 64:(h + 1) * 64], GT[:, h, :], xd[:, h, :], inc=(h == 3))
            yield
            kb.cp("act", yd[:], pY[:])
            yield
            for h in range(4):
                hs = slice(h * 64, (h + 1) * 64)
                kb.stt("dve", y[:, hs], pO[:, hs], ea[:, c, h:h + 1], yd[:, hs], ALU.mult, ALU.add)
                kb.stt("dve", y[:, hs], xc[:, h, :], dsk[:, h:h + 1], y[:, hs], ALU.mult, ALU.add)
                if h % 2 == 1:
                    yield
            kb.tt("pool", y[:], y[:], sz[:, c, :], ALU.mult)
            yield
            head_tail(kb, bufs, y[:], ngrow[:], ones_f[:, 0:1].bc([128, 256]), mix, t0, 512, 1.0, nheads=2, hd=128)
            yield

        interleave([stream_state(0)])
        for c in range(NT):
            gens = [stream_out(c)]
            if c + 1 < NT:
                gens.insert(0, stream_state(c + 1))
            interleave(gens)


def phase_mlstm(kb, nc, l, S, projF, projT, projTs, mix, identb, identf, ones_f, c_U, c_mb_st, c_mb_ts,
                c_sel127, ml_cw, ml_cb, ml_ifb, ml_ng):
    NT = S // 128
    with contextlib.ExitStack() as st:
        U = kb.sb(st, [128, 128], F32, "U")
        mbst = kb.sb(st, [128, 4, 128], F32, "mbst")
        mbts = kb.sb(st, [128, 4, 128], F32, "mbts")
        sel127 = kb.sb(st, [128, 128], F32, "sel127")
        kb.dma(U[:], c_U[:, :])
        kb.dma(sel127[:], c_sel127[:, :])
        for h in range(4):
            kb.dma(mbst[:, h, :], c_mb_st[:, :])
            kb.dma(mbts[:, h, :], c_mb_ts[:, :], q="pool")
        qT = kb.sb(st, [64, 4, S], BF16, "qT")
        kT = kb.sb(st, [64, 4, S], BF16, "kT")
        kTl = kb.sb(st, [128, NT, 4, 64], BF16, "kTl")
        v1 = kb.sb(st, [128, NT, 4, 65], BF16, "v1")
        z = kb.sb(st, [128, NT, 256], BF16, "z")
        og = kb.sb(st, [128, NT, 256], BF16, "og")
        ifr = kb.sb(st, [128, NT, 8], F32, "ifr")
        with contextlib.ExitStack() as s2:
            raw = Ring([kb.sb(s2, [64, S], BF16, "raw%d" % i) for i in range(2)])
            acc = Ring([kb.sb(s2, [64, S], F32, "acc%d" % i) for i in range(2)])
            wc = kb.sb(s2, [64, 8, 4], F32, "wc")
            bc_ = kb.sb(s2, [64, 8], F32, "bc")
            for i in range(8):
                ch0 = i * 64
                kb.dma(wc[:, i, :], V(ml_cw, ml_cw.h[l, :, ch0:ch0 + 64].rearrange("k c -> c k")))
                kb.dma(bc_[:, i:i + 1], V(ml_cb, ml_cb.h[l, ch0:ch0 + 64].rearrange("(c o) -> c o", o=1)))
            for i in range(8):
                nm = ("dq%d" % i) if i < 4 else ("dk%d" % (i - 4))
                dst = qT if i < 4 else kT
                r = raw.next()
                a = acc.next()
                r0 = F_ROW[nm][0]
                kb.dma(r[:], projF[r0:r0 + 64, :], q=("sp" if i % 2 == 0 else "pool"))
                conv_silu(kb, ("dve" if i % 2 == 0 else "pool"), dst[:, i % 4, :], r[:], a, wc[:, i, :], bc_[:, i:i + 1], S, 64)
        kb.barrier()
        with contextlib.ExitStack() as s2:
            vtmp = kb.sb(s2, [128, NT, 256], BF16, "vtmp")
            load_T(kb, vtmp, projT, "dv", S)
            load_T(kb, z, projT, "dz", S, q="pool")
            load_T(kb, og, projT, "do", S)
            kb.memset("pool", V(v1, v1.h[:, :, :, 64:65]), 1.0)
            kb.cp("pool", V(v1, v1.h[:, :, :, 0:64]), vtmp[:].re("p c (h d) -> p c h d", h=4))
            sz = silu_all(kb, s2, z, S, "dz")
            kb.act(og[:], og[:], AF.Sigmoid)
        kb.barrier()
        load_Ts(kb, ifr, projTs, "dif", S)
        ifb = kb.sb(st, [128, 8], F32, "ifb")
        ngrow = kb.sb(st, [128, 4, 64], F32, "ngrow")
        kb.dma(ifb[:], bcast_rows(ml_ifb, ml_ifb.h[l]))
        kb.dma(ngrow[:, 0, :], bcast_rows(ml_ng, ml_ng.h[l]))
        for h in range(1, 4):
            kb.cp("pool", ngrow[:, h, :], ngrow[:, 0, :])
        kb.tt("dve", ifr[:], ifr[:], V(ifb, ifb.h[:, :].unsqueeze(1).to_broadcast([128, NT, 8])), ALU.add)
        ig = V(ifr, ifr.h[:, :, 0:4])
        lf = kb.sb(st, [128, NT, 4], F32, "lf")
        kb.act(lf[:], V(ifr, ifr.h[:, :, 4:8]), AF.Exp, scale=-1.0)
        kb.act(lf[:], lf[:], AF.Ln, bias=1.0)
        kb.ts("dve", lf[:], lf[:], -1.0, None, ALU.mult)
        b = kb.sb(st, [128, NT, 4], F32, "b")
        blast = kb.sb(st, [128, NT, 4], F32, "blast")
        u = kb.sb(st, [128, NT, 4], F32, "u")
        with contextlib.ExitStack() as s2:
            pa = kb.ps(s2, [128, NT * 4], F32, "pa")
            pb = kb.ps(s2, [128, NT * 4], F32, "pb")
            kb.mm(pa[:], U[:], lf[:].re("p c h -> p (c h)"))
            kb.mm(pb[:], ones_f[:], lf[:].re("p c h -> p (c h)"))
            kb.cp("dve", b[:].re("p c h -> p (c h)"), pa[:])
            kb.cp("dve", blast[:].re("p c h -> p (c h)"), pb[:])
        kb.barrier()
        kb.tt("dve", u[:], ig, b[:], ALU.subtract)
        with contextlib.ExitStack() as s2:
            ptk = Ring([kb.ps(s2, [128, 4, 64], BF16, "ptk%d" % i) for i in range(2)])
            for c in range(NT):
                pt = ptk.next()
                for h in range(4):
                    kb.tr(pt[:, h, :], kT[:, h, c * 128:(c + 1) * 128], identb[0:64, 0:64], inc=(h == 3))
                kb.cp(("act" if c % 2 == 0 else "dve"), kTl[:, c, :, :], pt[:])
        kb.barrier()
        pM = kb.ps(st, [128, 4, 128], F32, "pM")
        pW = kb.ps(st, [128, 4, 128], F32, "pW")
        pSC = kb.ps(st, [128, 4, 128], F32, "pSC")
        pND = kb.ps(st, [128, 4, 65], F32, "pND")
        pIN = kb.ps(st, [128, 4, 65], F32, "pIN")
        pL = kb.ps(st, [64, 4, 65], F32, "pL")
        pU = kb.ps(st, [128, 4], F32, "pU")
        cmx = kb.sb(st, [128, 4], F32, "cmx")
        umax = kb.sb(st, [128, 4], F32, "umax")
        mprev = kb.sb(st, [128, 4], F32, "mprev")
        tmp = kb.sb(st, [128, 4], F32, "tmp")
        ntmp = kb.sb(st, [128, 4], F32, "ntmp")
        mt = kb.sb(st, [128, 4], F32, "mt")
        emt = kb.sb(st, [128, 4], F32, "emt")
        wint = kb.sb(st, [128, 4], F32, "wint")
        wend = kb.sb(st, [128, 4], F32, "wend")
        mm_ = kb.sb(st, [128, 4], F32, "mm")
        aprev = kb.sb(st, [128, 4], F32, "aprev")
        aloc = kb.sb(st, [128, 4], F32, "aloc")
        wT = kb.sb(st, [128, 4, 128], F32, "wT")
        sqk = kb.sb(st, [128, 4, 128], BF16, "sqk")
        nds = kb.sb(st, [128, 4, 65], F32, "nds")
        nd = kb.sb(st, [128, 4, 65], F32, "nd")
        dn = kb.sb(st, [128, 4], F32, "dn")
        hh_ = kb.sb(st, [128, 256], F32, "hh")
        kw = kb.sb(st, [128, 4, 64], BF16, "kw")
        cnf = kb.sb(st, [64, 4, 65], F32, "cnf")
        cnb = kb.sb(st, [64, 4, 65], BF16, "cnb")
        ltmp = kb.sb(st, [64, 4, 65], F32, "ltmp")
        kb.memset("pool", cnf[:], 0.0)
        kb.memset("pool", cnb[:], 0.0)
        kb.memset("pool", mprev[:], 0.0)
        bufs = (kb.sb(st, [128, 256], F32, "sq"), kb.sb(st, [128, 4], F32, "ssq"),
                kb.sb(st, [128, 4], F32, "rs"), kb.sb(st, [128, 256], BF16, "yo"))
        tmps = [kb.sb(st, [128, 4], F32, "tmp_r%d" % i) for i in range(2)]
        ntmps = [kb.sb(st, [128, 4], F32, "ntmp_r%d" % i) for i in range(2)]
        emts = [kb.sb(st, [128, 4], F32, "emt_r%d" % i) for i in range(2)]
        wints = [kb.sb(st, [128, 4], F32, "wint_r%d" % i) for i in range(2)]
        cnbs = [cnb] + [kb.sb(st, [64, 4, 65], BF16, "cnb_r%d" % i) for i in range(2)]

        def stream_state(c):
            tmp, ntmp, emt, wint = tmps[c % 2], ntmps[c % 2], emts[c % 2], wints[c % 2]
            for h in range(4):
                kb.mm(pM[:, h, :], u[:, c, h:h + 1].bc([128, 128]), identf[:], start=(h == 0), stop=False, inc=False)
            kb.mm(pM[:].re("p h s -> p (h s)"), identf[:], mbts[:].re("p h s -> p (h s)"), start=False, stop=True)
            yield
            kb.red(cmx[:], pM[:], ALU.max)
            kb.mm(pU[:], sel127[:], cmx[:])
            yield
            kb.cp("dve", umax[:], pU[:])
            kb.tt("dve", tmp[:], cmx[:], mprev[:], ALU.max)
            kb.ts("dve", ntmp[:], tmp[:], -1.0, None, ALU.mult)
            yield
            kb.tt("dve", mt[:], tmp[:], b[:, c, :], ALU.add)
            kb.act(emt[:], mt[:], AF.Exp, scale=-1.0)
            kb.tt("dve", wint[:], mprev[:], tmp[:], ALU.subtract)
            kb.act(wint[:], wint[:], AF.Exp)
            yield
            kb.tt("dve", wend[:], u[:, c, :], umax[:], ALU.subtract)
            kb.act(wend[:], wend[:], AF.Exp)
            kb.ts("dve", wend[:], wend[:], 0.125, None, ALU.mult)
            yield
            kb.tt("dve", mm_[:], mprev[:], umax[:], ALU.max)
            kb.tt("dve", aprev[:], mprev[:], mm_[:], ALU.subtract)
            kb.act(aprev[:], aprev[:], AF.Exp)
            kb.tt("dve", aloc[:], umax[:], mm_[:], ALU.subtract)
            kb.act(aloc[:], aloc[:], AF.Exp)
            yield
            kb.tt("pool", kw[:], kTl[:, c, :, :], V(wend, wend.h[:, :].unsqueeze(2).to_broadcast([128, 4, 64])), ALU.mult)
            for h in range(4):
                kb.mm(pL[:, h, :], kw[:, h, :], v1[:, c, h, :], inc=(h == 3))
            yield
            for h in range(4):
                kb.ts("dve", ltmp[:, h, :], pL[:, h, :], aloc[0:64, h:h + 1], None, ALU.mult)
                kb.stt("dve", cnf[:, h, :], cnf[:, h, :], aprev[0:64, h:h + 1], ltmp[:, h, :], ALU.mult, ALU.add)
                if h % 2 == 1:
                    yield
            kb.cp("act", cnbs[(c + 1) % 3][:], cnf[:])
            kb.tt("dve", mprev[:], mm_[:], blast[:, c, :], ALU.add)
            yield

        def stream_out(c):
            t0 = c * 128
            ntmp, emt, wint = ntmps[c % 2], emts[c % 2], wints[c % 2]
            cn = cnbs[c % 3]
            for h in range(4):
                kb.mm(pW[:, h, :], ntmp[:, h:h + 1].bc([128, 128]), identf[:], start=(h == 0), stop=False, inc=False)
            kb.mm(pW[:].re("p h s -> p (h s)"), identf[:], mbst[:].re("p h s -> p (h s)"), start=False, stop=True)
            yield
            for h in range(4):
                kb.mm(pSC[:, h, :], kT[:, h, t0:t0 + 128], qT[:, h, t0:t0 + 128], inc=(h == 3))
            yield
            for h in range(4):
                kb.act(wT[:, h, :], pW[:, h, :], AF.Exp, bias=u[:, c, h:h + 1])
                if h % 2 == 1:
                    yield
            kb.stt("dve", sqk[:], pSC[:], 0.125, wT[:], ALU.mult, ALU.mult)
            yield
            for h in range(4):
                kb.mm(pND[:, h, :], sqk[:, h, :], v1[:, c, h, :], inc=(h == 3))
            for h in range(4):
                kb.mm(pIN[:, h, :], qT[:, h, t0:t0 + 128], cn[:, h, :], inc=(h == 3))
            yield
            kb.cp("act", nds[:], pND[:])
            yield
            for h in range(4):
                kb.stt("dve", nd[:, h, :], pIN[:, h, :], wint[:, h:h + 1], nds[:, h, :], ALU.mult, ALU.add)
                if h % 2 == 1:
                    yield
            kb.ts("dve", dn[:], nd[:, :, 64], -1.0, None, ALU.mult)
            kb.tt("dve", dn[:], dn[:], nd[:, :, 64], ALU.max)
            kb.tt("dve", dn[:], dn[:], emt[:], ALU.max)
            kb.recip(dn[:], dn[:])
            yield
            for h in range(4):
                kb.ts("dve", hh_[:, h * 64:(h + 1) * 64], nd[:, h, 0:64], dn[:, h:h + 1], None, ALU.mult)
            kb.tt("pool", hh_[:], hh_[:], og[:, c, :], ALU.mult)
            yield
            head_tail(kb, bufs, hh_[:], ngrow[:].re("p h d -> p (h d)"), sz[:, c, :], mix, t0, 768, 1.0)
            yield

        interleave([stream_state(0)])
        for c in range(NT):
            gens = [stream_out(c)]
            if c + 1 < NT:
                gens.insert(0, stream_state(c + 1))
            interleave(gens)


def phase_out(kb, nc, l, S, x_src, x_dst, mix, w_out, gate_row, fin_row, identb, last):
    NT = S // 128
    with contextlib.ExitStack() as st:
        wo = kb.sb(st, [128, 8, D], BF16, "wo")
        stg = Ring([kb.sb(st, [128, D], F32, "wstg%d" % i) for i in range(2)])
        for k in range(8):
            sg = stg.next()
            kb.dma(sg[:], w_out[l, k * 128:(k + 1) * 128, :], q=("sp" if k % 2 == 0 else "pool"))
            kb.cp(("dve" if k % 2 == 0 else "act"), wo[:, k, :], sg[:])
        mixr = Ring([kb.sb(st, [128, D], BF16, "mixt%d" % i) for i in range(2)])
        xr = Ring([kb.sb(st, [128, D], F32, "xt%d" % i) for i in range(2)])
        mT = Ring([kb.sb(st, [128, 8, 128], BF16, "mT%d" % i) for i in range(2)])
        yr = Ring([kb.sb(st, [128, D], F32, "y%d" % i) for i in range(2)])
        junk = kb.sb(st, [128, D], BF16, "junk")
        ss = kb.sb(st, [128, 1], F32, "ss")
        ptr = Ring([kb.ps(st, [128, 8, 128], BF16, "ptr%d" % i) for i in range(2)])
        py = Ring([kb.ps(st, [128, 512], F32, "py%d" % i) for i in range(4)])
        def load_t(t):
            m_ = mixr.next()
            x_ = xr.next()
            kb.dma(m_[:], mix[t * 128:(t + 1) * 128, :], q="pool")
            kb.dma(x_[:], x_src[t * 128:(t + 1) * 128, :], q="pool")
            return m_, x_
        nxt = load_t(0)
        for t in range(NT):
            t0 = t * 128
            mt_, xt = nxt
            if t + 1 < NT:
                nxt = load_t(t + 1)
            pt = ptr.next()
            for k in range(8):
                kb.tr(pt[:, k, :], mt_[:, k * 128:(k + 1) * 128], identb[:], inc=(k == 7))
            m = mT.next()
            kb.cp("act", m[:], pt[:])
            y = yr.next()
            for half in range(2):
                p = py.next()
                for k in range(8):
                    kb.mm(p[:], m[:, k, :], wo[:, k, half * 512:(half + 1) * 512], start=(k == 0), stop=(k == 7), inc=(k == 7))
                hs = slice(half * 512, (half + 1) * 512)
                kb.tt("dve", y[:, hs], p[:], gate_row[:, l, hs], ALU.mult)
                kb.tt("pool", y[:, hs], y[:, hs], xt[:, hs], ALU.add)
            if last:
                kb.act(junk[:], y[:], AF.Square, accum=ss[:])
                kb.rsqrt(ss[:], ss[:], 1.0 / D)
                kb.stt("dve", y[:], y[:], ss[:, 0:1], fin_row[:], ALU.mult, ALU.mult)
            kb.dma(x_dst[t0:t0 + 128, :], y[:], q="sp", disjoint=True)


def make_consts(S):
    NCMP = (S - 32) // 16 + 1
    NCC = (NCMP + 127) // 128
    bf = ml_dtypes.bfloat16
    k = np.arange(128)
    tri = (k[:, None] <= k[None, :]).astype(np.float32)
    c = {}
    c["c_identb"] = np.eye(128, dtype=np.float32).astype(bf)
    c["c_identf"] = np.eye(128, dtype=np.float32)
    c["c_tri4"] = np.tile(tri, (1, 4)).astype(bf)
    c["c_atri4"] = np.tile(1.0 - tri, (1, 4)).astype(bf)
    c["c_U"] = tri.copy()
    c["c_mb_st"] = ((1.0 - tri) * NEGB).astype(np.float32)
    c["c_mb_ts"] = np.ascontiguousarray(c["c_mb_st"].T)
    s127 = np.zeros((128, 128), np.float32)
    s127[127, :] = 1.0
    c["c_sel127"] = s127
    E = np.zeros((64, S), np.float32)
    keys = np.arange(S)
    E[keys // 64 % 64, keys] = 30000.0 * (keys // 64 < 64)
    c["c_E"] = E.astype(bf)
    n_sel = S // 64
    cmp_idx = np.arange(NCMP)[:, None] * 16 + np.arange(32)[None, :]
    sel_start = np.arange(n_sel) * 64
    overlap = np.clip(np.minimum(cmp_idx[:, -1:] + 1, sel_start[None, :] + 64)
                      - np.maximum(cmp_idx[:, :1], sel_start[None, :]), 0, None)
    c2s = np.zeros((NCC * 128, 65), np.float32)
    c2s[:NCMP, :n_sel] = overlap / 32.0
    c2s[:NCMP, 64] = 1.0
    c["c_c2s"] = c2s.astype(bf)
    cm = np.zeros((NCC * 128, S), np.float32)
    cm[:NCMP] = (cmp_idx[:, -1][:, None] <= keys[None, :]).astype(np.float32)
    c["c_cmask"] = cm.astype(bf)
    t = np.arange(S)
    cur = t // 64
    sid = np.arange(64)
    forced = (sid[None, :] == cur[:, None]) | (sid[None, :] == 0)
    allowed = (sid[None, :] <= cur[:, None]) & (sid[None, :] < n_sel)
    c["c_selmul"] = (allowed & ~forced).astype(np.float32)
    c["c_seladd"] = np.where(forced, 1e4, np.where(allowed, 0.0, -1.0)).astype(np.float32)
    return c


_NC_CACHE = {}


def run(inputs, S, DEPTH, ncores, debug=False, stop=99):
    key = (S, DEPTH, debug, stop)
    if key not in _NC_CACHE:
        _NC_CACHE[key] = build(S, DEPTH, debug, stop)
    nc = _NC_CACHE[key]
    consts = make_consts(S)
    f32 = np.float32
    w_in = np.asarray(inputs["w_in"], f32)
    shared = dict(consts)
    shared["w_inF"] = np.ascontiguousarray(w_in[:, :, F_COLS])
    shared["w_inT"] = np.ascontiguousarray(w_in[:, :, T_COLS])
    for nm in ("norm_g", "ada_w", "ada_b", "w_out", "nsa_cmp_pos", "nsa_ck_w1", "nsa_ck_w2", "nsa_cv_w1",
               "nsa_cv_w2", "nsa_norm_g", "diff_norm_g", "ssm_conv_w", "ssm_conv_b", "ssm_dt_bias",
               "ssm_a_log", "ssm_d", "ssm_norm_g", "ml_conv_w", "ml_conv_b", "ml_if_b", "ml_norm_g", "final_g"):
        shared[nm] = np.ascontiguousarray(np.asarray(inputs[nm], f32))
    shared["diff_lam"] = np.ascontiguousarray(np.asarray(inputs["diff_lam"], f32).reshape(DEPTH, 128))
    x = np.asarray(inputs["x"], f32)
    c = np.asarray(inputs["c"], f32)
    in_maps = []
    for i in range(ncores):
        m = dict(shared)
        m["x"] = np.ascontiguousarray(x[i])
        m["c"] = np.ascontiguousarray(c[i])
        in_maps.append(m)
    res = run_bass_kernel_spmd(nc, in_maps, core_ids=list(range(ncores)))
    return res


def kernel(**inputs):
    res = run(inputs, 4096, 2, 8)
    return np.stack([np.asarray(r["out"], np.float32) for r in res.results], axis=0)
```
